# Optimizing a Trainium2 kernel written in Bass

```python
import math
import jax, jax.numpy as jnp
from jax import lax
import numpy as np

D_MODEL = 2048
BATCH = 4
SEQ = 4096
DEPTH = 1

GRID_W = 64
CTX_LEN = 256
D_MIX = D_MODEL
GLA_HEADS = 4
GLA_DK = (D_MIX // 4) // GLA_HEADS
GLA_DV = (D_MIX // 2) // GLA_HEADS
GLA_RANK = 16
GLA_TAU = 16.0
GLA_CHUNK = 64
DIFF_HEADS = 8
DIFF_DH = 64
DIFF_DV = 2 * DIFF_DH
ROPE_BASE = 10000.0
Q_BLOCK = 128
N_EXPERTS = 16
EC_CAPACITY_FACTOR = 2
D_FF_EXPERT = D_MODEL
ALPHA = (2.0 * DEPTH) ** 0.25
BETA = (8.0 * DEPTH) ** -0.25
LN_EPS = 1e-5
RMS_EPS = 1e-6

IN_SIZES = (GLA_HEADS * GLA_DK, GLA_HEADS * GLA_DK, GLA_HEADS * GLA_DV, GLA_HEADS * GLA_DV, 2 * GLA_RANK,
            DIFF_HEADS * 2 * DIFF_DH, DIFF_HEADS * 2 * DIFF_DH, DIFF_HEADS * DIFF_DV)
IN_SPLITS = tuple(int(s) for s in np.cumsum(IN_SIZES)[:-1])
D_IN = int(sum(IN_SIZES))

kernel_name = 'hybrid_gla_diffattn_ec_moe_diffusion_block'


def layer_norm(x, g, b):
    xf = x.astype(jnp.float32)
    mu = jnp.mean(xf, axis=-1, keepdims=True)
    var = jnp.mean(jnp.square(xf - mu), axis=-1, keepdims=True)
    return ((xf - mu) * lax.rsqrt(var + LN_EPS) * g + b).astype(x.dtype)


def head_rms_norm(x, g):
    xf = x.astype(jnp.float32)
    return (xf * lax.rsqrt(jnp.mean(xf * xf, axis=-1, keepdims=True) + RMS_EPS) * g).astype(x.dtype)


def axial_rope_tables(rows, dh):
    row = jnp.repeat(jnp.arange(rows, dtype=jnp.float32), GRID_W)
    col = jnp.tile(jnp.arange(GRID_W, dtype=jnp.float32), rows)
    n_freq = dh // 4
    inv = ROPE_BASE ** (-jnp.arange(n_freq, dtype=jnp.float32) / n_freq)
    ang = jnp.concatenate([row[:, None] * inv, col[:, None] * inv], axis=-1)
    return jnp.cos(ang), jnp.sin(ang)


def apply_rope(x, cos, sin):
    half = x.shape[-1] // 2
    xf = x.astype(jnp.float32)
    x1, x2 = xf[..., :half], xf[..., half:]
    cs = cos[None, :, None, None, :]
    sn = sin[None, :, None, None, :]
    return jnp.concatenate([x1 * cs - x2 * sn, x1 * sn + x2 * cs], axis=-1).astype(x.dtype)


def flip(a):
    return a[:, :, ::-1]


def gla_inputs(gq, gk, gv, glr, w_gate2, b_gate):
    B, N, _ = gq.shape
    heads = lambda a: a.reshape(B, N, GLA_HEADS, -1).transpose(0, 2, 1, 3).astype(jnp.float32)
    q = heads(gq) * (GLA_DK ** -0.5)
    k = heads(gk)
    v = heads(gv)
    logg = [jax.nn.log_sigmoid(heads(glr[..., d * GLA_RANK:(d + 1) * GLA_RANK] @ w_gate2[d] + b_gate[d])) / GLA_TAU
            for d in range(2)]
    return q, k, v, logg[0], logg[1]


def gla_chunk_scan(q, k, v, logg, s0):
    B, H, N, dk = q.shape
    dv = v.shape[-1]
    nc = N // GLA_CHUNK
    split = lambda a: a.reshape(B, H, nc, GLA_CHUNK, a.shape[-1]).transpose(2, 0, 1, 3, 4)
    mask = jnp.tril(jnp.ones((GLA_CHUNK, GLA_CHUNK), dtype=bool))

    def step(s, inp):
        qb, kb, vb, gb = inp
        bcum = jnp.cumsum(gb, axis=-2)
        btot = bcum[..., -1:, :]
        q_in = qb * jnp.exp(bcum)
        k_in = kb * jnp.exp(-bcum)
        k_out = kb * jnp.exp(btot - bcum)
        att = jnp.where(mask, jnp.einsum('bhik,bhjk->bhij', q_in, k_in), 0.0)
        o = jnp.einsum('bhij,bhjv->bhiv', att, vb) + jnp.einsum('bhik,bhkv->bhiv', q_in, s)
        s_new = jnp.exp(btot)[..., 0, :, None] * s + jnp.einsum('bhjk,bhjv->bhkv', k_out, vb)
        return s_new, o

    s_fin, oc = lax.scan(step, s0, (split(q), split(k), split(v), split(logg)))
    return oc.transpose(1, 2, 0, 3, 4).reshape(B, H, N, dv), s_fin


def gla_output(o, r, g):
    B, H, N, dv = o.shape
    o = head_rms_norm(o, g).transpose(0, 2, 1, 3).reshape(B, N, H * dv)
    return o.astype(r.dtype) * jax.nn.silu(r)


def diff_heads(dq, dk, dv):
    B, N, _ = dq.shape
    return (dq.reshape(B, N, DIFF_HEADS, 2, DIFF_DH), dk.reshape(B, N, DIFF_HEADS, 2, DIFF_DH),
            dv.reshape(B, N, DIFF_HEADS, DIFF_DV))


def diff_attend(q, k, v, lam):
    s = jnp.einsum('bqhmd,bkhmd->bhmqk', q, k).astype(jnp.float32) * (DIFF_DH ** -0.5)
    p = jax.nn.softmax(s, axis=-1)
    a = p[:, :, 0] - lam * p[:, :, 1]
    return jnp.einsum('bhqk,bkhv->bqhv', a.astype(v.dtype), v)


def diff_output(o, g, lam_init):
    B, N, H, dv = o.shape
    return (head_rms_norm(o, g) * (1.0 - lam_init)).reshape(B, N, H * dv)


def hybrid_mixer(u_ctx, u_lat, cos, sin, w_in, w_gate2, b_gate, gla_norm_g, diff_lambda, diff_norm_g, w_o,
                 lam_init, need_ctx_out):
    gq_c, gk_c, gv_c, gr_c, glr_c, dq_c, dk_c, dv_c = jnp.split(u_ctx @ w_in, IN_SPLITS, axis=-1)
    gq_l, gk_l, gv_l, gr_l, glr_l, dq_l, dk_l, dv_l = jnp.split(u_lat @ w_in, IN_SPLITS, axis=-1)
    B, n, _ = u_lat.shape

    qc, kc, vc, gfc, gbc = gla_inputs(gq_c, gk_c, gv_c, glr_c, w_gate2, b_gate)
    ql, kl, vl, gfl, gbl = gla_inputs(gq_l, gk_l, gv_l, glr_l, w_gate2, b_gate)
    s0 = jnp.zeros((B, GLA_HEADS, GLA_DK, GLA_DV), jnp.float32)
    o_cf, s_cf = gla_chunk_scan(qc, kc, vc, gfc, s0)
    o_cb, s_cb = gla_chunk_scan(flip(qc), flip(kc), flip(vc), flip(gbc), s0)
    o_lf, _ = gla_chunk_scan(ql, kl, vl, gfl, s_cf)
    o_lb, _ = gla_chunk_scan(flip(ql), flip(kl), flip(vl), flip(gbl), s_cb)
    gla_lat = gla_output(o_lf + flip(o_lb), gr_l, gla_norm_g)

    lq1, lk1, lq2, lk2 = diff_lambda.astype(jnp.float32)
    lam = jnp.exp(jnp.sum(lq1 * lk1)) - jnp.exp(jnp.sum(lq2 * lk2)) + lam_init
    qc_d, kc_d, vc_d = diff_heads(dq_c, dk_c, dv_c)
    ql_d, kl_d, vl_d = diff_heads(dq_l, dk_l, dv_l)
    ql_d = apply_rope(ql_d, cos, sin)
    kl_d = apply_rope(kl_d, cos, sin)
    k_all = jnp.concatenate([kc_d, kl_d], axis=1)
    v_all = jnp.concatenate([vc_d, vl_d], axis=1)
    nblk = n // Q_BLOCK
    qb = ql_d.reshape(B, nblk, Q_BLOCK, DIFF_HEADS, 2, DIFF_DH).swapaxes(0, 1)
    ob = lax.map(lambda qblk: diff_attend(qblk, k_all, v_all, lam), qb)
    diff_lat = diff_output(ob.swapaxes(0, 1).reshape(B, n, DIFF_HEADS, DIFF_DV), diff_norm_g, lam_init)

    out_lat = jnp.concatenate([gla_lat, diff_lat], axis=-1) @ w_o
    if not need_ctx_out:
        return None, out_lat
    gla_ctx = gla_output(o_cf + flip(o_cb), gr_c, gla_norm_g)
    diff_ctx = diff_output(diff_attend(qc_d, kc_d, vc_d, lam), diff_norm_g, lam_init)
    out_ctx = jnp.concatenate([gla_ctx, diff_ctx], axis=-1) @ w_o
    return out_ctx, out_lat


def expert_choice_ffn(h, w_router, w1, w3, w2):
    B, N, D = h.shape
    cap = EC_CAPACITY_FACTOR * N // N_EXPERTS
    aff = jax.nn.softmax((h @ w_router).astype(jnp.float32), axis=-1)
    gate, idx = lax.top_k(aff.swapaxes(1, 2), cap)
    xs = jax.vmap(lambda hb, ib: hb[ib])(h, idx)
    hid = jax.nn.silu(jnp.einsum('becd,edf->becf', xs, w1)) * jnp.einsum('becd,edf->becf', xs, w3)
    y = jnp.einsum('becf,efd->becd', hid, w2) * gate[..., None].astype(h.dtype)
    flat = (idx + (jnp.arange(B, dtype=idx.dtype) * N)[:, None, None]).reshape(-1)
    out = jax.ops.segment_sum(y.reshape(-1, D), flat, num_segments=B * N)
    return out.reshape(B, N, D)


def setup_inputs(seed: int = 0) -> dict:
    key = jax.random.key(seed)
    ks = jax.random.split(key, 24)
    nrm = lambda k, shape, s: jax.random.normal(k, shape, jnp.float32) * s
    col_scale = np.ones((D_IN,), np.float32)
    col_scale[IN_SPLITS[1]:IN_SPLITS[2]] = BETA
    col_scale[IN_SPLITS[6]:] = BETA
    return {
        'x': nrm(ks[0], (BATCH, SEQ, D_MODEL), 1.0),
        'c': nrm(ks[1], (BATCH, D_MODEL), 1.0),
        'ctx': nrm(ks[2], (BATCH, CTX_LEN, D_MODEL), 1.0),
        'c_ctx': nrm(ks[3], (D_MODEL,), 1.0),
        'w_ada': nrm(ks[4], (DEPTH, D_MODEL, 6 * D_MODEL), 0.5 * D_MODEL ** -0.5),
        'b_ada': nrm(ks[5], (DEPTH, 6 * D_MODEL), 0.01),
        'w_in': nrm(ks[6], (DEPTH, D_MODEL, D_IN), D_MODEL ** -0.5) * jnp.asarray(col_scale),
        'w_gate2': nrm(ks[7], (DEPTH, 2, GLA_RANK, GLA_HEADS * GLA_DK), GLA_RANK ** -0.5),
        'b_gate': nrm(ks[8], (DEPTH, 2, GLA_HEADS * GLA_DK), 0.1),
        'gla_norm_g': 1.0 + nrm(ks[9], (DEPTH, GLA_DV), 0.02),
        'diff_lambda': nrm(ks[10], (DEPTH, 4, DIFF_DH), 0.1),
        'diff_norm_g': 1.0 + nrm(ks[11], (DEPTH, DIFF_DV), 0.02),
        'w_o': nrm(ks[12], (DEPTH, D_MIX, D_MODEL), BETA * D_MIX ** -0.5),
        'ln1_g': 1.0 + nrm(ks[13], (DEPTH, D_MODEL), 0.02),
        'ln1_b': nrm(ks[14], (DEPTH, D_MODEL), 0.01),
        'w_router': nrm(ks[15], (DEPTH, D_MODEL, N_EXPERTS), D_MODEL ** -0.5),
        'w1': nrm(ks[16], (DEPTH, N_EXPERTS, D_MODEL, D_FF_EXPERT), D_MODEL ** -0.5),
        'w3': nrm(ks[17], (DEPTH, N_EXPERTS, D_MODEL, D_FF_EXPERT), D_MODEL ** -0.5),
        'w2': nrm(ks[18], (DEPTH, N_EXPERTS, D_FF_EXPERT, D_MODEL), BETA * D_FF_EXPERT ** -0.5),
        'ln2_g': 1.0 + nrm(ks[19], (DEPTH, D_MODEL), 0.02),
        'ln2_b': nrm(ks[20], (DEPTH, D_MODEL), 0.01),
    }


def reference(x, c, ctx, c_ctx, w_ada, b_ada, w_in, w_gate2, b_gate, gla_norm_g, diff_lambda, diff_norm_g,
              w_o, ln1_g, ln1_b, w_router, w1, w3, w2, ln2_g, ln2_b):
    n_lat = x.shape[1]
    rows = n_lat // GRID_W
    cos, sin = axial_rope_tables(rows, DIFF_DH)
    for l in range(DEPTH):
        last = l == DEPTH - 1
        lam_init = 0.8 - 0.6 * math.exp(-0.3 * l)
        mod_l = jax.nn.silu(c) @ w_ada[l] + b_ada[l]
        mod_c = jax.nn.silu(c_ctx) @ w_ada[l] + b_ada[l]
        sh1, sc1, g1, sh2, sc2, g2 = jnp.split(mod_l[:, None, :], 6, axis=-1)
        csh1, csc1, cg1, csh2, csc2, cg2 = jnp.split(mod_c, 6, axis=-1)
        o_ctx, o_lat = hybrid_mixer(ctx * (1.0 + csc1) + csh1, x * (1.0 + sc1) + sh1, cos, sin, w_in[l],
                                    w_gate2[l], b_gate[l], gla_norm_g[l], diff_lambda[l], diff_norm_g[l],
                                    w_o[l], lam_init, not last)
        x = layer_norm(ALPHA * x + g1 * o_lat, ln1_g[l], ln1_b[l])
        f_lat = expert_choice_ffn(x * (1.0 + sc2) + sh2, w_router[l], w1[l], w3[l], w2[l])
        x = layer_norm(ALPHA * x + g2 * f_lat, ln2_g[l], ln2_b[l])
        if not last:
            ctx = layer_norm(ALPHA * ctx + cg1 * o_ctx, ln1_g[l], ln1_b[l])
            f_ctx = expert_choice_ffn(ctx * (1.0 + csc2) + csh2, w_router[l], w1[l], w3[l], w2[l])
            ctx = layer_norm(ALPHA * ctx + cg2 * f_ctx, ln2_g[l], ln2_b[l])
    return x
```

```python
import math
from contextlib import ExitStack

import numpy as np
import concourse.bass as bass
import concourse.mybir as mybir
from concourse.bass_utils import run_bass_kernel_spmd

F32 = mybir.dt.float32
F32R = mybir.dt.float32r
BF16 = mybir.dt.bfloat16
I32 = mybir.dt.int32
AF = mybir.ActivationFunctionType
ALU = mybir.AluOpType
AX = mybir.AxisListType

D = 2048
NLAT = 4096
NCTX = 256
NTOK = NLAT + NCTX
DIN = 6176
NE = 16
CAP = 512
ALPHA = 2.0 ** 0.25
LAM_INIT = 0.2
NDSEM = 80

STOP_AFTER = "Z"
DEBUG = False
DEBUG_NAMES = ()
B_KINDS = None
ROPE_STAGE = 9
C_HEADS = 8


class Buf:
    def __init__(self, t=None, waw=True):
        self.t = t
        self.w = {}
        self.r = {}
        self.waw = waw


class K:
    def __init__(self, nc, es):
        self.nc = nc
        self.es = es
        self.E = {"pe": nc.tensor, "act": nc.scalar, "dve": nc.vector, "pool": nc.gpsimd, "sp": nc.sync}
        self.sem = {e: es.enter_context(nc.semaphore("s_" + e)) for e in self.E}
        self.seq = {e: 0 for e in self.E}
        self.known = {e: {} for e in self.E}
        self.dsem = [es.enter_context(nc.semaphore("d%d" % i)) for i in range(NDSEM)]
        self.duse = [0] * NDSEM
        self.dcount = 0
        self.dcount_sw = 0
        self.scopes = []
        self.n = 0

    def push(self):
        self.scopes.append(ExitStack())

    def pop(self):
        self.barrier()
        self.scopes.pop().close()

    def sb(self, name, shape, dtype):
        st = self.scopes[-1] if self.scopes else self.es
        return Buf(st.enter_context(self.nc.sbuf_tensor(name, list(shape), dtype)))

    def ps(self, name, shape, dtype):
        st = self.scopes[-1] if self.scopes else self.es
        return Buf(st.enter_context(self.nc.psum_tensor(name, list(shape), dtype)))

    def _semobj(self, key):
        return self.sem[key[1]] if key[0] == "e" else self.dsem[key[1]]

    def _wait(self, eng, key, val):
        if self.known[eng].get(key, 0) >= val:
            return
        self.E[eng].wait_ge(self._semobj(key), val)
        self.known[eng][key] = val

    def _deps(self, eng, reads, writes):
        for b in reads:
            for kk, v in b.w.items():
                if eng == "pe" and kk == ("e", "pe"):
                    continue
                self._wait(eng, kk, v)
        for b in writes:
            if b.waw:
                for kk, v in b.w.items():
                    if eng == "pe" and kk == ("e", "pe"):
                        continue
                    self._wait(eng, kk, v)
            for kk, v in b.r.items():
                if eng == "pe" and kk == ("e", "pe"):
                    continue
                self._wait(eng, kk, v)

    def _commit(self, key, val, reads, writes):
        for b in reads:
            b.r[key] = max(b.r.get(key, 0), val)
        for b in writes:
            if b.waw:
                b.w = {key: val}
                b.r = {}
            else:
                b.w[key] = max(b.w.get(key, 0), val)

    def op(self, eng, fn, reads=(), writes=()):
        self._deps(eng, reads, writes)
        inst = fn(self.E[eng])
        self.seq[eng] += 1
        inst.then_inc(self.sem[eng], 1)
        self._commit(("e", eng), self.seq[eng], reads, writes)
        self.n += 1

    def _dslot(self, q):
        half = NDSEM // 2
        if q == "pool":
            i = half + self.dcount_sw % half
            self.dcount_sw += 1
        else:
            i = self.dcount % half
            self.dcount += 1
        if self.duse[i] > 0:
            self._wait(q, ("d", i), self.duse[i] * 16)
        return i

    def dma(self, q, out, in_, reads=(), writes=(), **kw):
        self._deps(q, reads, writes)
        i = self._dslot(q)
        inst = self.E[q].dma_start(out=out, in_=in_, **kw)
        self.duse[i] += 1
        inst.then_inc(self.dsem[i], 16)
        self._commit(("d", i), self.duse[i] * 16, reads, writes)
        self.n += 1

    def idma(self, fn, reads=(), writes=()):
        q = "pool"
        self._deps(q, reads, writes)
        i = self._dslot(q)
        inst = fn(self.E[q])
        self.duse[i] += 1
        inst.then_inc(self.dsem[i], 16)
        self._commit(("d", i), self.duse[i] * 16, reads, writes)
        self.n += 1

    def barrier(self, engines=None):
        for e in (engines or list(self.E)):
            for f in self.E:
                if self.seq[f] > 0 and not (e == f == "pe"):
                    self._wait(e, ("e", f), self.seq[f])
            for i in range(NDSEM):
                if self.duse[i] > 0:
                    self._wait(e, ("d", i), self.duse[i] * 16)


def build_program():
    nc = bass.Bass("TRN2", target_bir_lowering=False)

    used_inputs = []

    class Lazy:
        def __init__(self, name, shape, dt=F32):
            self.name, self.shape, self.dt, self._ap = name, list(shape), dt, None

        @property
        def ap(self):
            if self._ap is None:
                self._ap = nc.dram_tensor(self.name, self.shape, self.dt, kind="ExternalInput").ap()
                used_inputs.append(self.name)
            return self._ap

    def din(name, shape, dt=F32):
        return Lazy(name, shape, dt)

    def dscr(name, shape, dt=F32):
        kind = "ExternalOutput" if (DEBUG and name in DEBUG_NAMES) else "Internal"
        return nc.dram_tensor(name, list(shape), dt, kind=kind).ap()

    x = din("x", [NLAT, D])
    ctx = din("ctx", [NCTX, D])
    cvec = din("cvec", [2, D])
    w_ada = din("w_ada", [D, 6 * D])
    b_ada = din("b_ada", [1, 6 * D])
    w_in = din("w_in", [D, DIN])
    w_gate2 = din("w_gate2", [2, 16, 512])
    b_gate = din("b_gate", [128, 2, 4])
    gla_norm_g = din("gla_norm_g", [1, 256])
    diff_lambda = din("diff_lambda", [1, 256])
    diff_norm_g = din("diff_norm_g", [1, 128])
    w_o = din("w_o", [D, D])
    ln1_g = din("ln1_g", [1, D])
    ln1_b = din("ln1_b", [1, D])
    w_router = din("w_router", [D, NE])
    w1 = din("w1", [NE, D, D])
    w3 = din("w3", [NE, D, D])
    w2 = din("w2", [NE, D, D])
    ln2_g = din("ln2_g", [1, D])
    ln2_b = din("ln2_b", [1, D])
    c_identf = din("c_identf", [128, 128])
    c_identb = din("c_identb", [128, 128], BF16)
    c_perm = din("c_perm", [128, 128], BF16)
    c_ropec = din("c_ropec", [128, NLAT])
    c_ropes = din("c_ropes", [128, NLAT])
    c_reset = din("c_reset", [128, 512])
    c_maskf = din("c_maskf", [128, 128], BF16)
    c_maskb = din("c_maskb", [128, 128], BF16)
    c_iota = din("c_iota", [128, 512])
    c_tokv = din("c_tokv", [128, 32, 2])
    out = nc.dram_tensor("out", [NLAT, D], F32, kind="ExternalOutput").ap()
    nc.used_inputs = used_inputs

    MOD = dscr("MOD", [2, 6 * D])
    AT = dscr("AT", [2, 4, 128, NTOK], BF16)
    BT = dscr("BT", [2, 4, 128, NTOK], BF16)
    ETOT = dscr("ETOT", [2, 128, 4, 34])
    VG = dscr("VG", [NTOK, 1024], BF16)
    RG = dscr("RG", [NLAT, 1024])
    QDT = dscr("QDT", [8, 128, NLAT], BF16)
    KDT = dscr("KDT", [8, 128, NTOK], BF16)
    VD = dscr("VD", [NTOK, 1024], BF16)
    OFS = dscr("OFS", [NLAT, 1024])
    AL = dscr("AL", [NLAT, D])
    X1 = dscr("X1", [NLAT, D])
    H = dscr("H", [NLAT, D])
    AFF = dscr("AFF", [NLAT, NE])
    FACC = dscr("FACC", [NLAT, D])
    IDXD = dscr("IDXD", [128, NE, 4], I32)
    GATED = dscr("GATED", [128, NE, 4])
    bMOD, bAT, bBT, bETOT, bVG, bRG, bQDT, bKDT, bVD, bOFS, bAL, bX1, bH, bAFF, bFACC, bOUT = [
        Buf(None, waw=False) for _ in range(16)]
    bFACCs = Buf(None, waw=True)

    with ExitStack() as es:
        k = K(nc, es)
        PS = [k.ps("psb%d" % i, [128, 512], F32) for i in range(8)]
        identf = k.sb("identf", [128, 128], F32)
        identb = k.sb("identb", [128, 128], BF16)
        k.dma("sp", identf.t[:], c_identf.ap, writes=[identf])
        k.dma("sp", identb.t[:], c_identb.ap, writes=[identb])
        modT = k.sb("modT", [128, 2, 96], F32)

        k.push()
        cT = k.sb("cT", [128, 16, 128], F32)
        k.op("pool", lambda e: e.memset(cT.t[:], 0.0), writes=[cT])
        with nc.allow_non_contiguous_dma(reason="tiny transposed vector load"):
            for r in range(2):
                k.dma("sp", cT.t[:, :, r], cvec.ap[r, :].rearrange("(c p) -> p c", p=128), writes=[cT])
        scT = k.sb("scT", [128, 16, 128], F32R)
        k.op("act", lambda e: e.activation(out=scT.t[:], in_=cT.t[:], func=AF.Silu), reads=[cT], writes=[scT])
        bada2 = k.sb("bada2", [2, 6 * D], F32)
        k.dma("sp", bada2.t[:], b_ada.ap.partition_broadcast(2), writes=[bada2])
        mod_sb = k.sb("mod_sb", [2, 6 * D], F32)
        slabs = [k.sb("aslab%d" % i, [128, 16, 512], F32R) for i in range(2)]
        for blk in range(24):
            slab = slabs[blk % 2]
            k.dma("pool", slab.t[:], w_ada.ap[:, blk * 512:(blk + 1) * 512].rearrange("(c p) f -> p c f", p=128),
                  writes=[slab])
            ps = PS[blk % 2]
            for c in range(16):
                k.op("pe", lambda e: e.matmul(ps.t[:], scT.t[:, c, :], slab.t[:, c, :], start=(c == 0), stop=(c == 15)),
                     reads=[scT, slab], writes=[ps])
            k.op("dve", lambda e: e.tensor_tensor(out=mod_sb.t[:, blk * 512:(blk + 1) * 512], in0=ps.t[0:2, :],
                                                  in1=bada2.t[:, blk * 512:(blk + 1) * 512], op=ALU.add),
                 reads=[ps, bada2], writes=[mod_sb])
        k.dma("sp", MOD, mod_sb.t[:], reads=[mod_sb], writes=[bMOD])
        with nc.allow_non_contiguous_dma(reason="small transposed reload of modulation vectors"):
            k.dma("sp", modT.t[:], MOD.rearrange("r (j p) -> p r j", p=128), reads=[bMOD], writes=[modT])
        for j0 in (16, 64):
            k.op("dve", lambda e: e.tensor_scalar(out=modT.t[:, :, j0:j0 + 16], in0=modT.t[:, :, j0:j0 + 16],
                                                  scalar1=1.0, scalar2=None, op0=ALU.add),
                 reads=[modT], writes=[modT])
        k.pop()
        if STOP_AFTER == "A":
            return finish(nc, k, out)


        k.push()
        Cblk = k.sb("Cblk", [128, 512], F32)
        Sblk = k.sb("Sblk", [128, 512], F32)
        qraw = [k.sb("qraw%d" % i, [128, 512], BF16) for i in range(2)]
        uT = k.sb("uT", [128, 16, 512], F32R)
        xt = [k.sb("xt%d" % i, [128, D], F32) for i in range(2)]
        slabs = [k.sb("bslab%d" % i, [128, 16, 512], F32R) for i in range(2)]
        wg2 = k.sb("wg2", [48, 512], F32)
        k.dma("sp", wg2.t[0:16, :], w_gate2.ap[0], writes=[wg2])
        k.dma("sp", wg2.t[32:48, :], w_gate2.ap[1], writes=[wg2])
        negbg = k.sb("negbg", [128, 2, 4], F32)
        k.dma("sp", negbg.t[:], b_gate.ap, writes=[negbg])
        k.op("dve", lambda e: e.tensor_scalar(out=negbg.t[:], in0=negbg.t[:], scalar1=-1.0, scalar2=None, op0=ALU.mult),
             reads=[negbg], writes=[negbg])
        perm = k.sb("perm", [128, 128], BF16)
        k.dma("sp", perm.t[:], c_perm.ap, writes=[perm])
        resetm = k.sb("resetm", [128, 512], F32)
        k.dma("sp", resetm.t[:], c_reset.ap, writes=[resetm])
        lrT = k.sb("lrT", [48, 512], F32)
        zf = xt[0]
        k.op("pool", lambda e: e.memset(zf.t[:], 0.0), writes=[zf])
        lrslab = k.sb("lrslab", [128, 16, 128], F32R)
        k.op("act", lambda e: e.activation(out=lrslab.t[:].rearrange("p c f -> p (c f)"), in_=zf.t[:], func=AF.Copy),
             reads=[zf], writes=[lrslab])
        with nc.allow_non_contiguous_dma(reason="small low-rank gate columns"):
            for d in range(2):
                k.dma("pool", lrslab.t[:, :, d * 32:d * 32 + 16],
                      w_in.ap[:, 3072 + d * 16:3072 + d * 16 + 16].rearrange("(c p) f -> p c f", p=128), writes=[lrslab])
        EA = k.sb("EA", [128, 2, 4, 512], BF16)
        EB = k.sb("EB", [128, 2, 4, 512], BF16)
        ABst = k.sb("ABst", [128, 2, 2, 4, 512], BF16)
        etst = k.sb("etst", [128, 2, 4, 4], F32)
        tmpE = [k.sb("tmpE%d" % i, [128, 512], F32) for i in range(2)]
        spt = [k.sb("spt%d" % i, [128, 512], F32) for i in range(2)]
        cumt = [k.sb("cumt%d" % i, [128, 512], F32) for i in range(2)]
        argt = [k.sb("argt%d" % i, [128, 512], F32) for i in range(2)]
        Vst = [k.sb("Vst%d" % i, [128, 4, 512], BF16) for i in range(2)]
        Rst = [k.sb("Rst0", [128, 4, 512], F32)] * 2
        QKst = [k.sb("QKst%d" % i, [128, 4, 512], BF16) for i in range(2)]
        t1 = tmpE
        t2 = spt
        SLABS = [("glr", 3072, 32), ("gq", 0, 512), ("gk", 512, 512), ("gv", 1024, 512), ("gv", 1536, 512),
                 ("gr", 2048, 512), ("gr", 2560, 512), ("dq", 3104, 512), ("dq", 3616, 512),
                 ("dk", 4128, 512), ("dk", 4640, 512), ("dv", 5152, 512), ("dv", 5664, 512)]
        cnt = {"x": 0, "slab": 0, "acc": 0, "g": 0, "st": 0, "r": 0}
        NBLK = 9 if STOP_AFTER != "B1" else 2
        for tb in range(NBLK):
            isctx = tb == 0
            NT = 256 if isctx else 512
            tok0 = 0 if isctx else 256 + (tb - 1) * 512
            lat0 = 0 if isctx else (tb - 1) * 512
            mr = 1 if isctx else 0
            src = ctx.ap if isctx else x.ap[lat0:lat0 + 512, :]
            nt = NT // 128
            for t in range(nt):
                xb = xt[cnt["x"] % 2]
                cnt["x"] += 1
                k.dma("sp", xb.t[:], src[t * 128:(t + 1) * 128, :], writes=[xb])
                for c in range(16):
                    pb = PS[c // 4]
                    k.op("pe", lambda e: e.transpose(pb.t[:, (c % 4) * 128:(c % 4 + 1) * 128], xb.t[:, c * 128:(c + 1) * 128],
                                                     identf.t[:]), reads=[xb, identf], writes=[pb])
                for c in range(16):
                    pb = PS[c // 4]
                    k.op("act", lambda e: e.activation(out=uT.t[:, c, t * 128:(t + 1) * 128],
                                                       in_=pb.t[:, (c % 4) * 128:(c % 4 + 1) * 128], func=AF.Identity,
                                                       scale=modT.t[:, mr, 16 + c:17 + c], bias=modT.t[:, mr, c:c + 1]),
                         reads=[pb, modT], writes=[uT])
            if not isctx:
                k.dma("sp", Cblk.t[:], c_ropec.ap[:, lat0:lat0 + 512], writes=[Cblk])
                k.dma("sp", Sblk.t[:], c_ropes.ap[:, lat0:lat0 + 512], writes=[Sblk])
            half = {}
            for (kind, c0, ncols) in SLABS:
                hf = half.get(kind, 0)
                half[kind] = hf + 1
                if isctx and kind in ("gr", "dq"):
                    continue
                if B_KINDS is not None and kind not in B_KINDS:
                    continue
                if kind != "glr":
                    slab = slabs[cnt["slab"] % 2]
                    cnt["slab"] += 1
                if kind == "glr":
                    slab = lrslab
                else:
                    k.dma("pool", slab.t[:], w_in.ap[:, c0:c0 + 512].rearrange("(c p) f -> p c f", p=128), writes=[slab])

                def acc_feat(j):
                    pa = PS[4 + cnt["acc"] % 2]
                    cnt["acc"] += 1
                    for c in range(16):
                        k.op("pe", lambda e: e.matmul(pa.t[:, :NT], slab.t[:, c, j * 128:(j + 1) * 128], uT.t[:, c, :NT],
                                                      start=(c == 0), stop=(c == 15)), reads=[slab, uT], writes=[pa])
                    return pa

                def acc_tok(t):
                    pa = PS[4 + cnt["acc"] % 2]
                    cnt["acc"] += 1
                    for c in range(16):
                        k.op("pe", lambda e: e.matmul(pa.t[:], uT.t[:, c, t * 128:(t + 1) * 128], slab.t[:, c, :],
                                                      start=(c == 0), stop=(c == 15)), reads=[slab, uT], writes=[pa])
                    return pa

                if kind == "glr":
                    pa = acc_feat(0)
                    k.op("act", lambda e: e.activation(out=lrT.t[:, :NT], in_=pa.t[0:48, :NT], func=AF.Copy),
                         reads=[pa], writes=[lrT])
                    nch = NT // 128
                    for d in range(2):
                        for j in range(4):
                            g = cnt["g"] % 2
                            cnt["g"] += 1
                            pz = PS[6 + g]
                            k.op("pe", lambda e: e.matmul(pz.t[:, :NT], wg2.t[d * 32:d * 32 + 16, j * 128:(j + 1) * 128],
                                                          lrT.t[d * 32:d * 32 + 16, :NT], start=True, stop=True),
                                 reads=[wg2, lrT], writes=[pz])
                            k.op("act", lambda e: e.activation(out=tmpE[g].t[:, :NT], in_=pz.t[:, :NT], func=AF.Exp,
                                                               scale=-1.0, bias=negbg.t[:, d, j:j + 1]),
                                 reads=[pz, negbg], writes=[tmpE[g]])
                            k.op("act", lambda e: e.activation(out=spt[g].t[:, :NT], in_=tmpE[g].t[:, :NT], func=AF.Ln,
                                                               scale=1.0, bias=1.0), reads=[tmpE[g]], writes=[spt[g]])
                            k.op("dve", lambda e: e.tensor_tensor_scan(out=cumt[g].t[:, :NT], data0=resetm.t[:, :NT],
                                                                       data1=spt[g].t[:, :NT], initial=0.0, op0=ALU.mult,
                                                                       op1=ALU.add), reads=[resetm, spt[g]], writes=[cumt[g]])
                            if d == 0:
                                arg = cumt[g]
                            else:
                                arg = argt[g]
                                k.op("dve", lambda e: e.tensor_tensor(out=arg.t[:, :NT], in0=cumt[g].t[:, :NT],
                                                                      in1=spt[g].t[:, :NT], op=ALU.subtract),
                                     reads=[cumt[g], spt[g]], writes=[arg])
                            sa = -1.0 / 16 if d == 0 else 1.0 / 16
                            k.op("act", lambda e: e.activation(out=EA.t[:, d, j, :NT], in_=arg.t[:, :NT], func=AF.Exp, scale=sa),
                                 reads=[arg], writes=[EA])
                            k.op("act", lambda e: e.activation(out=EB.t[:, d, j, :NT], in_=arg.t[:, :NT], func=AF.Exp, scale=-sa),
                                 reads=[arg], writes=[EB])
                            k.op("act", lambda e: e.activation(out=etst.t[:, d, j, 0:nch], in_=cumt[g].t[:, 127:NT:128],
                                                               func=AF.Exp, scale=-1.0 / 16), reads=[cumt[g]], writes=[etst])
                    ch0 = tok0 // 128
                    with nc.allow_non_contiguous_dma(reason="tiny per-chunk decay totals"):
                        for d in range(2):
                            k.dma("sp", ETOT[d, :, :, ch0:ch0 + nch], etst.t[:, d, :, 0:nch], reads=[etst], writes=[bETOT])
                elif kind in ("gq", "gk"):
                    isq = kind == "gq"
                    for j in range(4):
                        pa = acc_feat(j)
                        for d in range(2):
                            if isq:
                                k.op("dve", lambda e: e.scalar_tensor_tensor(out=ABst.t[:, 0, d, j, :NT], in0=pa.t[:, :NT],
                                                                             scalar=128.0 ** -0.5, in1=EA.t[:, d, j, :NT],
                                                                             op0=ALU.mult, op1=ALU.mult),
                                     reads=[pa, EA], writes=[ABst])
                            else:
                                k.op("dve", lambda e: e.tensor_tensor(out=ABst.t[:, 1, d, j, :NT], in0=pa.t[:, :NT],
                                                                      in1=EB.t[:, d, j, :NT], op=ALU.mult),
                                     reads=[pa, EB], writes=[ABst])
                    dst = AT if isq else BT
                    k.dma("sp", dst.rearrange("d j p n -> p d j n")[:, :, :, tok0:tok0 + NT],
                          ABst.t[:, 0 if isq else 1, :, :, :NT], reads=[ABst], writes=[bAT if isq else bBT])
                elif kind in ("gv", "dv", "gr"):
                    if kind == "gr":
                        st = Rst[cnt["r"] % 2]
                        cnt["r"] += 1
                    else:
                        st = Vst[cnt["st"] % 2]
                        cnt["st"] += 1
                    for t in range(nt):
                        pa = acc_tok(t)
                        fn = AF.Silu if kind == "gr" else AF.Copy
                        k.op("act", lambda e: e.activation(out=st.t[:, t, :], in_=pa.t[:], func=fn), reads=[pa], writes=[st])
                    if kind == "gr":
                        k.dma("sp", RG[lat0:lat0 + 512, hf * 512:(hf + 1) * 512].rearrange("(t p) f -> p t f", p=128),
                              st.t[:, 0:nt, :], reads=[st], writes=[bRG])
                    else:
                        dst, bd = (VG, bVG) if kind == "gv" else (VD, bVD)
                        k.dma("sp", dst[tok0:tok0 + NT, hf * 512:(hf + 1) * 512].rearrange("(t p) f -> p t f", p=128),
                              st.t[:, 0:nt, :], reads=[st], writes=[bd])
                elif kind in ("dq", "dk"):
                    st = QKst[cnt["st"] % 2]
                    cnt["st"] += 1
                    sc = 0.125 if kind == "dq" else 1.0
                    for jj in range(4):
                        pa = acc_feat(jj)
                        if isctx:
                            k.op("act", lambda e: e.activation(out=st.t[:, jj, :NT], in_=pa.t[:, :NT], func=AF.Copy),
                                 reads=[pa], writes=[st])
                            continue
                        g = cnt["g"] % 2
                        cnt["g"] += 1
                        k.op("act", lambda e: e.activation(out=qraw[g].t[:], in_=pa.t[:], func=AF.Copy), reads=[pa], writes=[qraw[g]])
                        pz = PS[6 + g]
                        if ROPE_STAGE >= 1:
                            k.op("pe", lambda e: e.matmul(pz.t[:], perm.t[:], qraw[g].t[:], start=True, stop=True),
                                 reads=[perm, qraw[g]], writes=[pz])
                        qf, pzf = argt[g], cumt[g]
                        if ROPE_STAGE >= 2:
                            k.op("act", lambda e: e.activation(out=qf.t[:], in_=pa.t[:], func=AF.Copy), reads=[pa], writes=[qf])
                            k.op("dve", lambda e: e.tensor_tensor(out=t1[g].t[:], in0=qf.t[:], in1=Cblk.t[:], op=ALU.mult),
                                 reads=[qf, Cblk], writes=[t1[g]])
                        if ROPE_STAGE >= 3:
                            k.op("act", lambda e: e.activation(out=pzf.t[:], in_=pz.t[:], func=AF.Copy), reads=[pz], writes=[pzf])
                            k.op("dve", lambda e: e.tensor_tensor(out=t2[g].t[:], in0=pzf.t[:], in1=Sblk.t[:], op=ALU.mult),
                                 reads=[pzf, Sblk], writes=[t2[g]])
                        if ROPE_STAGE >= 4:
                            k.op("dve", lambda e: e.tensor_tensor(out=t1[g].t[:], in0=t1[g].t[:], in1=t2[g].t[:], op=ALU.add),
                                 reads=[t1[g], t2[g]], writes=[t1[g]])
                            k.op("act", lambda e: e.activation(out=st.t[:, jj, :], in_=t1[g].t[:], func=AF.Copy, scale=sc),
                                 reads=[t1[g]], writes=[st])
                        else:
                            k.op("act", lambda e: e.activation(out=st.t[:, jj, :], in_=pa.t[:], func=AF.Copy), reads=[pa], writes=[st])
                    if kind == "dq":
                        k.dma("sp", QDT[hf * 4:hf * 4 + 4, :, lat0:lat0 + 512].rearrange("h p n -> p h n"), st.t[:],
                              reads=[st], writes=[bQDT])
                    else:
                        k.dma("sp", KDT[hf * 4:hf * 4 + 4, :, tok0:tok0 + NT].rearrange("h p n -> p h n"), st.t[:, :, :NT],
                              reads=[st], writes=[bKDT])
        k.pop()
        if STOP_AFTER in ("B", "B1"):
            return finish(nc, k, out)


        k.push()
        KT = k.sb("KT", [128, NTOK], BF16)
        Vh = k.sb("Vh", [128, 34, 132], BF16)
        k.op("pool", lambda e: e.memset(Vh.t[:, :, 128:129], 1.0), writes=[Vh])
        QT = [k.sb("QT%d" % i, [128, 512], BF16) for i in range(2)]
        PT = [k.sb("PT%d" % i, [128, 512], BF16) for i in range(3)]
        Om = [k.sb("Om%d" % i, [128, 4, 128], F32) for i in range(2)]
        dd = k.sb("dd", [128, 4, 128], F32)
        junk = k.sb("junk", [128, 256], F32)
        ost = [k.sb("ost%d" % i, [128, 4, 128], F32) for i in range(2)]
        rl = k.sb("rl", [128, 8], F32)
        ssq = k.sb("ssq", [128, 4], F32)
        rstd = k.sb("rstd", [128, 4], F32)
        dl = k.sb("dl", [128, 256], F32)
        k.dma("sp", dl.t[:], diff_lambda.ap.partition_broadcast(128), writes=[dl])
        prod = k.sb("prod", [128, 2, 64], F32)
        for i in range(2):
            k.op("dve", lambda e: e.tensor_tensor(out=prod.t[:, i, :], in0=dl.t[:, i * 128:i * 128 + 64],
                                                  in1=dl.t[:, i * 128 + 64:i * 128 + 128], op=ALU.mult), reads=[dl], writes=[prod])
        sums = k.sb("sums", [128, 2], F32)
        k.op("dve", lambda e: e.tensor_reduce(out=sums.t[:], in_=prod.t[:], axis=AX.X, op=ALU.add), reads=[prod], writes=[sums])
        exl = k.sb("exl", [128, 2], F32)
        k.op("act", lambda e: e.activation(out=exl.t[:], in_=sums.t[:], func=AF.Exp), reads=[sums], writes=[exl])
        neglam = k.sb("neglam", [128, 1], F32)
        k.op("dve", lambda e: e.tensor_tensor(out=neglam.t[:], in0=exl.t[:, 1:2], in1=exl.t[:, 0:1], op=ALU.subtract),
             reads=[exl], writes=[neglam])
        k.op("dve", lambda e: e.tensor_scalar(out=neglam.t[:], in0=neglam.t[:], scalar1=-LAM_INIT, scalar2=None, op0=ALU.add),
             reads=[neglam], writes=[neglam])
        g8 = k.sb("g8", [128, 128], F32)
        k.dma("sp", g8.t[:], diff_norm_g.ap.partition_broadcast(128), writes=[g8])
        k.op("dve", lambda e: e.tensor_scalar(out=g8.t[:], in0=g8.t[:], scalar1=1.0 - LAM_INIT, scalar2=None, op0=ALU.mult),
             reads=[g8], writes=[g8])
        cq = 0
        cp = 0
        NH = C_HEADS if STOP_AFTER != "C1" else 1
        for h in range(NH):
            k.dma("sp", KT.t[:], KDT[h], reads=[bKDT], writes=[KT])
            k.dma("sp", Vh.t[:, :, 0:128], VD[:, h * 128:(h + 1) * 128].rearrange("(t p) f -> p t f", p=128),
                  reads=[bVD], writes=[Vh])
            for qb in range(8):
                QTb = QT[cq % 2]
                osb = ost[cq % 2]
                cq += 1
                k.dma("sp", QTb.t[:], QDT[h, :, qb * 512:(qb + 1) * 512], reads=[bQDT], writes=[QTb])
                for m in range(2):
                    for kt in range(34):
                        pS = PS[kt % 2]
                        k.op("pe", lambda e: e.matmul(pS.t[:], KT.t[64 * m:64 * m + 64, kt * 128:(kt + 1) * 128],
                                                      QTb.t[64 * m:64 * m + 64, :], start=True, stop=True),
                             reads=[KT, QTb], writes=[pS])
                        PTb = PT[cp % 3]
                        cp += 1
                        k.op("act", lambda e: e.activation(out=PTb.t[:], in_=pS.t[:], func=AF.Exp), reads=[pS], writes=[PTb])
                        for qt in range(4):
                            po = PS[2 + qt]
                            k.op("pe", lambda e: e.matmul(po.t[:, 0:129], PTb.t[:, qt * 128:(qt + 1) * 128], Vh.t[:, kt, 0:129],
                                                          start=(kt == 0), stop=(kt == 33)), reads=[PTb, Vh], writes=[po])
                    for qt in range(4):
                        po = PS[2 + qt]
                        k.op("dve", lambda e: e.reciprocal(out=rl.t[:, m * 4 + qt:m * 4 + qt + 1], in_=po.t[:, 128:129]),
                             reads=[po], writes=[rl])
                        k.op("act", lambda e: e.activation(out=Om[m].t[:, qt, :], in_=po.t[:, 0:128], func=AF.Copy,
                                                           scale=rl.t[:, m * 4 + qt:m * 4 + qt + 1]),
                             reads=[po, rl], writes=[Om[m]])
                for qt in range(4):
                    k.op("dve", lambda e: e.scalar_tensor_tensor(out=dd.t[:, qt, :], in0=Om[1].t[:, qt, :], scalar=neglam.t[:, 0:1],
                                                                 in1=Om[0].t[:, qt, :], op0=ALU.mult, op1=ALU.add),
                         reads=[Om[0], Om[1], neglam], writes=[dd])
                    k.op("act", lambda e: e.activation(out=junk.t[:, 0:128], in_=dd.t[:, qt, :], func=AF.Square,
                                                       accum_out=ssq.t[:, qt:qt + 1]), reads=[dd], writes=[junk, ssq])
                k.op("dve", lambda e: e.tensor_scalar(out=rstd.t[:], in0=ssq.t[:], scalar1=1.0 / 128, scalar2=1e-6, op0=ALU.mult,
                                                      op1=ALU.add), reads=[ssq], writes=[rstd])
                k.op("act", lambda e: e.activation(out=rstd.t[:], in_=rstd.t[:], func=AF.Sqrt), reads=[rstd], writes=[rstd])
                k.op("dve", lambda e: e.reciprocal(out=rstd.t[:], in_=rstd.t[:]), reads=[rstd], writes=[rstd])
                for qt in range(4):
                    k.op("dve", lambda e: e.scalar_tensor_tensor(out=osb.t[:, qt, :], in0=dd.t[:, qt, :], scalar=rstd.t[:, qt:qt + 1],
                                                                 in1=g8.t[:], op0=ALU.mult, op1=ALU.mult),
                         reads=[dd, rstd, g8], writes=[osb])
                k.dma("sp", AL[qb * 512:(qb + 1) * 512, 1024 + h * 128:1024 + (h + 1) * 128].rearrange("(t p) f -> p t f", p=128),
                      osb.t[:], reads=[osb], writes=[bAL])
        k.pop()
        if STOP_AFTER in ("C", "C1"):
            return finish(nc, k, out)

        k.push()
        etot = k.sb("etot", [128, 2, 4, 34], F32)
        for d in range(2):
            k.dma("sp", etot.t[:, d], ETOT[d], reads=[bETOT], writes=[etot])
        masks = [k.sb("mask%d" % d, [128, 128], BF16) for d in range(2)]
        k.dma("sp", masks[0].t[:], c_maskf.ap, writes=[masks[0]])
        k.dma("sp", masks[1].t[:], c_maskb.ap, writes=[masks[1]])
        gn = k.sb("gn", [128, 256], F32)
        k.dma("sp", gn.t[:], gla_norm_g.ap.partition_broadcast(128), writes=[gn])
        S32 = [k.sb("S32_%d" % j, [128, 256], F32) for j in range(4)]
        Sb = [k.sb("Sb_%d" % j, [128, 256], BF16) for j in range(4)]
        Ab = [k.sb("Ab%d" % i, [128, 4, 128], BF16) for i in range(2)]
        Bb = [k.sb("Bb%d" % i, [128, 4, 128], BF16) for i in range(2)]
        Vg = [k.sb("Vg%d" % i, [128, 1024], BF16) for i in range(2)]
        OFb = [k.sb("OFb%d" % i, [128, 1024], F32) for i in range(2)]
        Rb = [k.sb("Rb%d" % i, [128, 1024], F32) for i in range(2)]
        OFst = [k.sb("OFst%d" % i, [128, 1024], F32) for i in range(2)]
        gl = [k.sb("gl%d" % i, [128, 1024], F32) for i in range(2)]
        Btok = [k.sb("Btok%d" % i, [128, 128], BF16) for i in range(2)]
        attT = [k.sb("attT%d" % i, [128, 128], BF16) for i in range(2)]
        ste = [k.sb("ste%d" % i, [128, 256], F32) for i in range(2)]
        osb2 = [k.sb("osb2_%d" % i, [128, 256], F32) for i in range(2)]
        junk2 = k.sb("junk2", [128, 256], F32)
        ssq2 = k.sb("ssq2", [128, 4], F32)
        rstd2 = k.sb("rstd2", [128, 4], F32)
        cs = 0
        cx = 0
        for d in range(2):
            for j in range(4):
                k.op("pool", lambda e: e.memset(S32[j].t[:], 0.0), writes=[S32[j]])
                k.op("pool", lambda e: e.memset(Sb[j].t[:], 0.0), writes=[Sb[j]])
            order = list(range(34)) if d == 0 else [1, 0] + list(range(33, 1, -1))
            if STOP_AFTER == "D1":
                order = order[:6]
            for ci in order:
                isctx = ci < 2
                tok0 = ci * 128
                lat0 = tok0 - 256
                b = cs % 2
                cs += 1
                k.dma("sp", Ab[b].t[:], AT[d, :, :, tok0:tok0 + 128].rearrange("j p n -> p j n"), reads=[bAT], writes=[Ab[b]])
                k.dma("sp", Bb[b].t[:], BT[d, :, :, tok0:tok0 + 128].rearrange("j p n -> p j n"), reads=[bBT], writes=[Bb[b]])
                k.dma("sp", Vg[b].t[:], VG[tok0:tok0 + 128, :], reads=[bVG], writes=[Vg[b]])
                if d == 1 and not isctx:
                    k.dma("sp", OFb[b].t[:], OFS[lat0:lat0 + 128, :], reads=[bOFS], writes=[OFb[b]])
                    k.dma("sp", Rb[b].t[:], RG[lat0:lat0 + 128, :], reads=[bRG], writes=[Rb[b]])
                for j in range(4):
                    x2 = cx % 2
                    cx += 1
                    et = etot.t[:, d, j, ci:ci + 1]
                    pT = PS[x2]
                    k.op("pe", lambda e: e.transpose(pT.t[:].bitcast(BF16)[:, 0:128], Bb[b].t[:, j, :], identb.t[:]),
                         reads=[Bb[b], identb], writes=[pT])
                    k.op("act", lambda e: e.activation(out=Btok[x2].t[:], in_=pT.t[:].bitcast(BF16)[:, 0:128], func=AF.Copy),
                         reads=[pT], writes=[Btok[x2]])
                    if d == 1:
                        k.op("dve", lambda e: e.tensor_scalar(out=S32[j].t[:], in0=S32[j].t[:], scalar1=et, scalar2=None,
                                                              op0=ALU.mult), reads=[S32[j], etot], writes=[S32[j]])
                        k.op("act", lambda e: e.activation(out=Sb[j].t[:], in_=S32[j].t[:], func=AF.Copy),
                             reads=[S32[j]], writes=[Sb[j]])
                    if not isctx:
                        pA = PS[2 + x2]
                        k.op("pe", lambda e: e.matmul(pA.t[:, 0:128], Bb[b].t[:, j, :], Ab[b].t[:, j, :], start=True, stop=True),
                             reads=[Ab[b], Bb[b]], writes=[pA])
                        k.op("dve", lambda e: e.tensor_tensor(out=attT[x2].t[:], in0=pA.t[:, 0:128], in1=masks[d].t[:], op=ALU.mult),
                             reads=[pA, masks[d]], writes=[attT[x2]])
                        pO = PS[4 + x2]
                        k.op("pe", lambda e: e.matmul(pO.t[:, 0:256], attT[x2].t[:], Vg[b].t[:, j * 256:(j + 1) * 256],
                                                      start=True, stop=False), reads=[attT[x2], Vg[b]], writes=[pO])
                        k.op("pe", lambda e: e.matmul(pO.t[:, 0:256], Ab[b].t[:, j, :], Sb[j].t[:], start=False, stop=True),
                             reads=[Ab[b], Sb[j]], writes=[pO])
                        if d == 0:
                            k.op("act", lambda e: e.activation(out=OFst[b].t[:, j * 256:(j + 1) * 256], in_=pO.t[:, 0:256],
                                                               func=AF.Copy), reads=[pO], writes=[OFst[b]])
                        else:
                            k.op("act", lambda e: e.activation(out=osb2[x2].t[:], in_=pO.t[:, 0:256], func=AF.Copy),
                                 reads=[pO], writes=[osb2[x2]])
                            k.op("dve", lambda e: e.tensor_tensor(out=gl[b].t[:, j * 256:(j + 1) * 256], in0=osb2[x2].t[:],
                                                                  in1=OFb[b].t[:, j * 256:(j + 1) * 256], op=ALU.add),
                                 reads=[osb2[x2], OFb[b]], writes=[gl[b]])
                            k.op("act", lambda e: e.activation(out=junk2.t[:], in_=gl[b].t[:, j * 256:(j + 1) * 256], func=AF.Square,
                                                               accum_out=ssq2.t[:, j:j + 1]), reads=[gl[b]], writes=[junk2, ssq2])
                    pS2 = PS[6 + x2]
                    k.op("pe", lambda e: e.matmul(pS2.t[:, 0:256], Btok[x2].t[:], Vg[b].t[:, j * 256:(j + 1) * 256],
                                                  start=True, stop=True), reads=[Btok[x2], Vg[b]], writes=[pS2])
                    if d == 0:
                        k.op("act", lambda e: e.activation(out=ste[x2].t[:], in_=pS2.t[:, 0:256], func=AF.Copy, scale=et),
                             reads=[pS2, etot], writes=[ste[x2]])
                        k.op("dve", lambda e: e.scalar_tensor_tensor(out=S32[j].t[:], in0=S32[j].t[:], scalar=et, in1=ste[x2].t[:],
                                                                     op0=ALU.mult, op1=ALU.add),
                             reads=[S32[j], ste[x2], etot], writes=[S32[j]])
                        k.op("act", lambda e: e.activation(out=Sb[j].t[:], in_=S32[j].t[:], func=AF.Copy),
                             reads=[S32[j]], writes=[Sb[j]])
                    else:
                        k.op("act", lambda e: e.activation(out=ste[x2].t[:], in_=pS2.t[:, 0:256], func=AF.Copy),
                             reads=[pS2], writes=[ste[x2]])
                        k.op("dve", lambda e: e.tensor_tensor(out=S32[j].t[:], in0=S32[j].t[:], in1=ste[x2].t[:], op=ALU.add),
                             reads=[S32[j], ste[x2]], writes=[S32[j]])
                if not isctx:
                    if d == 0:
                        k.dma("sp", OFS[lat0:lat0 + 128, :], OFst[b].t[:], reads=[OFst[b]], writes=[bOFS])
                    else:
                        k.op("dve", lambda e: e.tensor_scalar(out=rstd2.t[:], in0=ssq2.t[:], scalar1=1.0 / 256, scalar2=1e-6,
                                                              op0=ALU.mult, op1=ALU.add), reads=[ssq2], writes=[rstd2])
                        k.op("act", lambda e: e.activation(out=rstd2.t[:], in_=rstd2.t[:], func=AF.Sqrt), reads=[rstd2], writes=[rstd2])
                        k.op("dve", lambda e: e.reciprocal(out=rstd2.t[:], in_=rstd2.t[:]), reads=[rstd2], writes=[rstd2])
                        for j in range(4):
                            k.op("dve", lambda e: e.scalar_tensor_tensor(out=gl[b].t[:, j * 256:(j + 1) * 256],
                                                                         in0=gl[b].t[:, j * 256:(j + 1) * 256],
                                                                         scalar=rstd2.t[:, j:j + 1], in1=gn.t[:], op0=ALU.mult,
                                                                         op1=ALU.mult), reads=[gl[b], rstd2, gn], writes=[gl[b]])
                        k.op("pool", lambda e: e.tensor_tensor(out=gl[b].t[:], in0=gl[b].t[:], in1=Rb[b].t[:], op=ALU.mult),
                             reads=[gl[b], Rb[b]], writes=[gl[b]])
                        k.dma("sp", AL[lat0:lat0 + 128, 0:1024], gl[b].t[:], reads=[gl[b]], writes=[bAL])
        k.pop()
        if STOP_AFTER in ("D", "D1"):
            return finish(nc, k, out)


        def bcast_tile(name, src_ap):
            t = k.sb(name, [128, D], F32)
            k.dma("sp", t.t[:], src_ap.partition_broadcast(128), writes=[t])
            return t

        def layer_norm(y, gbc, bbc, outb, st, junkb):
            k.op("act", lambda e: e.activation(out=junkb.t[:], in_=y.t[:], func=AF.Copy, accum_out=st.t[:, 0:1]),
                 reads=[y], writes=[junkb, st])
            k.op("act", lambda e: e.activation(out=junkb.t[:], in_=y.t[:], func=AF.Square, accum_out=st.t[:, 1:2]),
                 reads=[y], writes=[junkb, st])
            k.op("dve", lambda e: e.tensor_scalar(out=st.t[:, 0:2], in0=st.t[:, 0:2], scalar1=1.0 / D, scalar2=None, op0=ALU.mult),
                 reads=[st], writes=[st])
            k.op("dve", lambda e: e.tensor_tensor(out=st.t[:, 2:3], in0=st.t[:, 0:1], in1=st.t[:, 0:1], op=ALU.mult),
                 reads=[st], writes=[st])
            k.op("dve", lambda e: e.tensor_tensor(out=st.t[:, 3:4], in0=st.t[:, 1:2], in1=st.t[:, 2:3], op=ALU.subtract),
                 reads=[st], writes=[st])
            k.op("dve", lambda e: e.tensor_scalar(out=st.t[:, 3:4], in0=st.t[:, 3:4], scalar1=1e-5, scalar2=None, op0=ALU.add),
                 reads=[st], writes=[st])
            k.op("act", lambda e: e.activation(out=st.t[:, 3:4], in_=st.t[:, 3:4], func=AF.Sqrt), reads=[st], writes=[st])
            k.op("dve", lambda e: e.reciprocal(out=st.t[:, 4:5], in_=st.t[:, 3:4]), reads=[st], writes=[st])
            k.op("dve", lambda e: e.scalar_tensor_tensor(out=st.t[:, 5:6], in0=st.t[:, 0:1], scalar=-1.0, in1=st.t[:, 4:5],
                                                         op0=ALU.mult, op1=ALU.mult), reads=[st], writes=[st])
            k.op("act", lambda e: e.activation(out=outb.t[:], in_=y.t[:], func=AF.Identity, scale=st.t[:, 4:5], bias=st.t[:, 5:6]),
                 reads=[y, st], writes=[outb])
            k.op("dve", lambda e: e.tensor_tensor(out=outb.t[:], in0=outb.t[:], in1=gbc.t[:], op=ALU.mult),
                 reads=[outb, gbc], writes=[outb])
            k.op("pool", lambda e: e.tensor_tensor(out=outb.t[:], in0=outb.t[:], in1=bbc.t[:], op=ALU.add),
                 reads=[outb, bbc], writes=[outb])

        affall = k.sb("affall", [128, 32, NE], F32)
        k.push()
        g1bc = bcast_tile("g1bc", MOD[0:1, 2 * D:3 * D])
        l1g = bcast_tile("l1g", ln1_g.ap)
        l1b = bcast_tile("l1b", ln1_b.ap)
        aT = k.sb("aT", [128, 16, 512], F32R)
        oslab = [k.sb("oslab%d" % i, [128, 16, 256], F32R) for i in range(2)]
        osb = k.sb("osbE", [128, 4, D], F32)
        alt = [k.sb("alt%d" % i, [128, D], F32) for i in range(2)]
        xe = k.sb("xe", [128, D], F32)
        x1t = [k.sb("x1t%d" % i, [128, D], F32) for i in range(2)]
        junkE = k.sb("junkE", [128, D], F32)
        stE = k.sb("stE", [128, 8], F32)
        hT = k.sb("hT", [128, 16, 128], F32R)
        wr = k.sb("wr", [128, 16, NE], F32R)
        k.dma("pool", wr.t[:], w_router.ap.rearrange("(c p) e -> p c e", p=128), writes=[wr])
        sm = k.sb("sm", [128, 4], F32)
        ee = k.sb("ee", [128, NE], F32)
        ca = 0
        cso = 0
        NBE = 8 if STOP_AFTER != "E1" else 1
        for tb in range(NBE):
            for t in range(4):
                al = alt[ca % 2]
                ca += 1
                r0 = tb * 512 + t * 128
                k.dma("sp", al.t[:], AL[r0:r0 + 128, :], reads=[bAL], writes=[al])
                for c in range(16):
                    pb = PS[c // 4]
                    k.op("pe", lambda e: e.transpose(pb.t[:, (c % 4) * 128:(c % 4 + 1) * 128], al.t[:, c * 128:(c + 1) * 128],
                                                     identf.t[:]), reads=[al, identf], writes=[pb])
                for q4 in range(4):
                    pb = PS[q4]
                    k.op("act", lambda e: e.activation(out=aT.t[:, q4 * 4:q4 * 4 + 4, t * 128:(t + 1) * 128],
                                                       in_=pb.t[:].rearrange("p (c n) -> p c n", c=4), func=AF.Copy),
                         reads=[pb], writes=[aT])
            for cb in range(8):
                sl = oslab[cso % 2]
                cso += 1
                k.dma("pool", sl.t[:], w_o.ap[:, cb * 256:(cb + 1) * 256].rearrange("(c p) f -> p c f", p=128), writes=[sl])
                for t in range(4):
                    pa = PS[4 + (t % 4)]
                    for c in range(16):
                        k.op("pe", lambda e: e.matmul(pa.t[:, 0:256], aT.t[:, c, t * 128:(t + 1) * 128], sl.t[:, c, :],
                                                      start=(c == 0), stop=(c == 15)), reads=[aT, sl], writes=[pa])
                    k.op("act", lambda e: e.activation(out=osb.t[:, t, cb * 256:(cb + 1) * 256], in_=pa.t[:, 0:256], func=AF.Copy),
                         reads=[pa], writes=[osb])
            for t in range(4):
                r0 = tb * 512 + t * 128
                tile_i = tb * 4 + t
                x1 = x1t[tile_i % 2]
                k.dma("sp", xe.t[:], x.ap[r0:r0 + 128, :], writes=[xe])
                k.op("dve", lambda e: e.tensor_tensor(out=osb.t[:, t, :], in0=osb.t[:, t, :], in1=g1bc.t[:], op=ALU.mult),
                     reads=[osb, g1bc], writes=[osb])
                k.op("dve", lambda e: e.scalar_tensor_tensor(out=xe.t[:], in0=xe.t[:], scalar=ALPHA, in1=osb.t[:, t, :],
                                                             op0=ALU.mult, op1=ALU.add), reads=[xe, osb], writes=[xe])
                layer_norm(xe, l1g, l1b, x1, stE, junkE)
                k.dma("sp", X1[r0:r0 + 128, :], x1.t[:], reads=[x1], writes=[bX1])
                for c in range(16):
                    pb = PS[c // 4]
                    k.op("pe", lambda e: e.transpose(pb.t[:, (c % 4) * 128:(c % 4 + 1) * 128], x1.t[:, c * 128:(c + 1) * 128],
                                                     identf.t[:]), reads=[x1, identf], writes=[pb])
                for c in range(16):
                    pb = PS[c // 4]
                    k.op("act", lambda e: e.activation(out=hT.t[:, c, :], in_=pb.t[:, (c % 4) * 128:(c % 4 + 1) * 128],
                                                       func=AF.Identity, scale=modT.t[:, 0, 64 + c:65 + c],
                                                       bias=modT.t[:, 0, 48 + c:49 + c]), reads=[pb, modT], writes=[hT])
                pl = PS[4]
                for c in range(16):
                    k.op("pe", lambda e: e.matmul(pl.t[:, 0:NE], hT.t[:, c, :], wr.t[:, c, :], start=(c == 0), stop=(c == 15)),
                         reads=[hT, wr], writes=[pl])
                k.op("dve", lambda e: e.tensor_reduce(out=sm.t[:, 0:1], in_=pl.t[:, 0:NE], axis=AX.X, op=ALU.max),
                     reads=[pl], writes=[sm])
                k.op("dve", lambda e: e.tensor_scalar(out=sm.t[:, 1:2], in0=sm.t[:, 0:1], scalar1=-1.0, scalar2=None, op0=ALU.mult),
                     reads=[sm], writes=[sm])
                k.op("act", lambda e: e.activation(out=ee.t[:], in_=pl.t[:, 0:NE], func=AF.Exp, bias=sm.t[:, 1:2],
                                                   accum_out=sm.t[:, 2:3]), reads=[pl, sm], writes=[ee, sm])
                k.op("dve", lambda e: e.reciprocal(out=sm.t[:, 3:4], in_=sm.t[:, 2:3]), reads=[sm], writes=[sm])
                k.op("dve", lambda e: e.tensor_scalar(out=affall.t[:, tile_i, :], in0=ee.t[:], scalar1=sm.t[:, 3:4], scalar2=None,
                                                      op0=ALU.mult), reads=[ee, sm], writes=[affall])
        k.pop()
        if DEBUG and "AFF" in DEBUG_NAMES:
            k.dma("sp", AFF.rearrange("(t p) e -> p t e", p=128), affall.t[:], reads=[affall], writes=[bAFF])
        if STOP_AFTER in ("E", "E1"):
            return finish(nc, k, out)

        idxI = k.sb("idxI", [128, NE, 4], I32)
        gateS = k.sb("gateS", [128, NE, 4], F32)
        k.push()
        affT = k.sb("affT", [16, NLAT], F32)
        for g8i in range(8):
            pb = PS[g8i % 2]
            for t4 in range(4):
                t = g8i * 4 + t4
                k.op("pe", lambda e: e.transpose(pb.t[0:16, t4 * 128:(t4 + 1) * 128], affall.t[:, t, :], identf.t[:]),
                     reads=[affall, identf], writes=[pb])
            k.op("act", lambda e: e.activation(out=affT.t[:, g8i * 512:(g8i + 1) * 512], in_=pb.t[0:16, :], func=AF.Copy),
                 reads=[pb], writes=[affT])
        bs = k.sb("bs", [16, 8], F32)
        junkT = k.sb("junkT", [16, NLAT], F32)
        k.op("dve", lambda e: e.memset(bs.t[:], 0.0), writes=[bs])
        k.op("dve", lambda e: e.memset(bs.t[:, 1:2], 1.0), reads=[bs], writes=[bs])
        for it in range(30):
            k.op("dve", lambda e: e.tensor_scalar(out=bs.t[:, 5:6], in0=bs.t[:, 1:2], scalar1=0.5, scalar2=None, op0=ALU.mult),
                 reads=[bs], writes=[bs])
            k.op("dve", lambda e: e.scalar_tensor_tensor(out=bs.t[:, 2:3], in0=bs.t[:, 0:1], scalar=0.5, in1=bs.t[:, 5:6],
                                                         op0=ALU.mult, op1=ALU.add), reads=[bs], writes=[bs])
            k.op("dve", lambda e: e.tensor_scalar(out=junkT.t[:], in0=affT.t[:], scalar1=bs.t[:, 2:3], scalar2=0.0, op0=ALU.is_ge,
                                                  op1=ALU.add, accum_out=bs.t[:, 3:4]), reads=[affT, bs], writes=[junkT, bs])
            k.op("dve", lambda e: e.tensor_scalar(out=bs.t[:, 4:5], in0=bs.t[:, 3:4], scalar1=float(CAP), scalar2=None,
                                                  op0=ALU.is_ge), reads=[bs], writes=[bs])
            k.op("dve", lambda e: e.tensor_tensor(out=bs.t[:, 5:6], in0=bs.t[:, 2:3], in1=bs.t[:, 0:1], op=ALU.subtract),
                 reads=[bs], writes=[bs])
            k.op("dve", lambda e: e.tensor_tensor(out=bs.t[:, 6:7], in0=bs.t[:, 1:2], in1=bs.t[:, 2:3], op=ALU.subtract),
                 reads=[bs], writes=[bs])
            k.op("dve", lambda e: e.scalar_tensor_tensor(out=bs.t[:, 0:1], in0=bs.t[:, 5:6], scalar=bs.t[:, 4:5], in1=bs.t[:, 0:1],
                                                         op0=ALU.mult, op1=ALU.add), reads=[bs], writes=[bs])
            k.op("dve", lambda e: e.scalar_tensor_tensor(out=bs.t[:, 1:2], in0=bs.t[:, 6:7], scalar=bs.t[:, 4:5], in1=bs.t[:, 2:3],
                                                         op0=ALU.mult, op1=ALU.add), reads=[bs], writes=[bs])
        Mt = k.sb("Mt", [16, NLAT], F32)
        k.op("dve", lambda e: e.tensor_scalar(out=Mt.t[:], in0=affT.t[:], scalar1=bs.t[:, 0:1], scalar2=None, op0=ALU.is_ge),
             reads=[affT, bs], writes=[Mt])
        k.op("dve", lambda e: e.memset(junkT.t[:], 1.0), writes=[junkT])
        cumM = k.sb("cumM", [16, NLAT], F32)
        k.op("dve", lambda e: e.tensor_tensor_scan(out=cumM.t[:], data0=junkT.t[:], data1=Mt.t[:], initial=0.0, op0=ALU.mult,
                                                   op1=ALU.add), reads=[junkT, Mt], writes=[cumM])
        k.op("dve", lambda e: e.scalar_tensor_tensor(out=cumM.t[:], in0=Mt.t[:], scalar=-8193.0, in1=cumM.t[:], op0=ALU.mult,
                                                     op1=ALU.add), reads=[Mt, cumM], writes=[cumM])
        k.op("dve", lambda e: e.tensor_scalar(out=cumM.t[:], in0=cumM.t[:], scalar1=8192.0, scalar2=None, op0=ALU.add),
             reads=[cumM], writes=[cumM])
        slotTM = k.sb("slotTM", [128, 32, NE], F32)
        pb = PS[2]
        for t in range(32):
            k.op("pe", lambda e: e.transpose(pb.t[:, t * 16:(t + 1) * 16], cumM.t[:, t * 128:(t + 1) * 128], identf.t[0:16, 0:16]),
                 reads=[cumM, identf], writes=[pb])
        k.op("act", lambda e: e.activation(out=slotTM.t[:].rearrange("p t e -> p (t e)"), in_=pb.t[:], func=AF.Copy),
             reads=[pb], writes=[slotTM])
        vals = k.sb("vals", [128, 32, NE, 4], F32)
        tokv = k.sb("tokv", [128, 32, 2], F32)
        k.dma("sp", tokv.t[:], c_tokv.ap, writes=[tokv])
        for e_ in range(NE):
            k.op("pool", lambda e: e.tensor_copy(out=vals.t[:, :, e_, 0:2], in_=tokv.t[:]), reads=[tokv], writes=[vals])
        k.op("pool", lambda e: e.tensor_copy(out=vals.t[:, :, :, 2], in_=affall.t[:]), reads=[affall], writes=[vals])
        k.op("pool", lambda e: e.tensor_copy(out=vals.t[:, :, :, 3], in_=affall.t[:]), reads=[affall], writes=[vals])
        iot = k.sb("iot", [128, 512], F32)
        k.dma("sp", iot.t[:], c_iota.ap, writes=[iot])
        Sel = [k.sb("Sel%d" % i, [128, 512], F32) for i in range(3)]
        res = k.sb("resF", [128, 4, 4], F32)
        idf = k.sb("idf", [128, 4], F32)
        csel = 0
        for e_ in range(NE):
            for t in range(32):
                sl = Sel[csel % 3]
                csel += 1
                k.op("dve", lambda e: e.tensor_scalar(out=sl.t[:], in0=iot.t[:], scalar1=slotTM.t[:, t, e_:e_ + 1], scalar2=None,
                                                      op0=ALU.is_equal), reads=[iot, slotTM], writes=[sl])
                for st in range(4):
                    k.op("pe", lambda e: e.matmul(PS[4 + st].t[:, 0:4], sl.t[:, st * 128:(st + 1) * 128], vals.t[:, t, e_, :],
                                                  start=(t == 0), stop=(t == 31)), reads=[sl, vals], writes=[PS[4 + st]])
            for st in range(4):
                k.op("act", lambda e: e.activation(out=res.t[:, st, :], in_=PS[4 + st].t[:, 0:4], func=AF.Copy),
                     reads=[PS[4 + st]], writes=[res])
            k.op("dve", lambda e: e.scalar_tensor_tensor(out=idf.t[:], in0=res.t[:, :, 0], scalar=64.0, in1=res.t[:, :, 1],
                                                         op0=ALU.mult, op1=ALU.add), reads=[res], writes=[idf])
            k.op("dve", lambda e: e.tensor_copy(out=idxI.t[:, e_, :], in_=idf.t[:]), reads=[idf], writes=[idxI])
            k.op("dve", lambda e: e.tensor_copy(out=gateS.t[:, e_, :], in_=res.t[:, :, 2]), reads=[res], writes=[gateS])
        k.pop()
        if DEBUG and "IDXD" in DEBUG_NAMES:
            k.dma("sp", IDXD, idxI.t[:], reads=[idxI], writes=[bAFF])
            k.dma("sp", GATED, gateS.t[:], reads=[gateS], writes=[bAFF])
        if STOP_AFTER == "F":
            return finish(nc, k, out)

        k.push()
        zt = k.sb("zt", [128, D], F32)
        k.op("pool", lambda e: e.memset(zt.t[:], 0.0), writes=[zt])
        for t in range(32):
            k.dma("sp", FACC[t * 128:(t + 1) * 128, :], zt.t[:], reads=[zt], writes=[bFACC])
        xsT = k.sb("xsT", [128, 16, 512], F32R)
        hidT = k.sb("hidT", [128, 16, 512], F32R)
        gsl = [k.sb("gsl%d" % i, [128, 16, 512], F32R) for i in range(2)]
        xg = k.sb("xg", [128, D], F32)
        yst = [k.sb("yst%d" % i, [128, D], F32) for i in range(4)]
        s1 = k.sb("s1", [128, 4, 512], F32)
        t3 = [k.sb("t3_%d" % i, [128, 512], F32) for i in range(2)]
        cg = 0
        cpp = 0
        scat_prev = [None]
        NEX = NE if STOP_AFTER != "G1" else 1
        for e_ in range(NEX):
            for st in range(4):
                k.idma(lambda e: e.indirect_dma_start(out=xg.t[:], out_offset=None, in_=X1,
                                                      in_offset=bass.IndirectOffsetOnAxis(ap=idxI.t[:, e_, st:st + 1], axis=0)),
                       reads=[bX1, idxI], writes=[xg])
                for c in range(16):
                    pb = PS[c // 4]
                    k.op("pe", lambda e: e.transpose(pb.t[:, (c % 4) * 128:(c % 4 + 1) * 128], xg.t[:, c * 128:(c + 1) * 128],
                                                     identf.t[:]), reads=[xg, identf], writes=[pb])
                for c in range(16):
                    pb = PS[c // 4]
                    k.op("act", lambda e: e.activation(out=xsT.t[:, c, st * 128:(st + 1) * 128],
                                                       in_=pb.t[:, (c % 4) * 128:(c % 4 + 1) * 128], func=AF.Identity,
                                                       scale=modT.t[:, 0, 64 + c:65 + c], bias=modT.t[:, 0, 48 + c:49 + c]),
                         reads=[pb, modT], writes=[xsT])
            for fb in range(4):
                for wi, wsrc in enumerate((w1, w3)):
                    sl = gsl[cg % 2]
                    cg += 1
                    k.dma("pool", sl.t[:], wsrc.ap[e_, :, fb * 512:(fb + 1) * 512].rearrange("(c p) f -> p c f", p=128), writes=[sl])
                    for fc in range(4):
                        pa = PS[4 + cpp % 4]
                        cpp += 1
                        for c in range(16):
                            k.op("pe", lambda e: e.matmul(pa.t[:], sl.t[:, c, fc * 128:(fc + 1) * 128], xsT.t[:, c, :],
                                                          start=(c == 0), stop=(c == 15)), reads=[sl, xsT], writes=[pa])
                        if wi == 0:
                            k.op("act", lambda e: e.activation(out=s1.t[:, fc, :], in_=pa.t[:], func=AF.Silu), reads=[pa], writes=[s1])
                        else:
                            tt = t3[fc % 2]
                            k.op("act", lambda e: e.activation(out=tt.t[:], in_=pa.t[:], func=AF.Copy), reads=[pa], writes=[tt])
                            k.op("dve", lambda e: e.tensor_tensor(out=hidT.t[:, fb * 4 + fc, :], in0=tt.t[:], in1=s1.t[:, fc, :],
                                                                  op=ALU.mult), reads=[tt, s1], writes=[hidT])
            for db in range(4):
                sl = gsl[cg % 2]
                cg += 1
                k.dma("pool", sl.t[:], w2.ap[e_, :, db * 512:(db + 1) * 512].rearrange("(c p) f -> p c f", p=128), writes=[sl])
                for st in range(4):
                    pa = PS[4 + cpp % 4]
                    cpp += 1
                    for c in range(16):
                        k.op("pe", lambda e: e.matmul(pa.t[:], hidT.t[:, c, st * 128:(st + 1) * 128], sl.t[:, c, :],
                                                      start=(c == 0), stop=(c == 15)), reads=[sl, hidT], writes=[pa])
                    k.op("act", lambda e: e.activation(out=yst[st].t[:, db * 512:(db + 1) * 512], in_=pa.t[:], func=AF.Copy,
                                                       scale=gateS.t[:, e_, st:st + 1]), reads=[pa, gateS], writes=[yst[st]])
            for st in range(4):
                k.idma(lambda e: e.indirect_dma_start(out=FACC, out_offset=bass.IndirectOffsetOnAxis(ap=idxI.t[:, e_, st:st + 1], axis=0),
                                                      in_=yst[st].t[:], in_offset=None, compute_op=ALU.add),
                       reads=[yst[st], idxI, bFACCs, bFACC], writes=[bFACCs])
        k.pop()
        if STOP_AFTER in ("G", "G1"):
            return finish(nc, k, out)

        k.push()
        g2bc = bcast_tile("g2bc", MOD[0:1, 5 * D:6 * D])
        l2g = bcast_tile("l2g", ln2_g.ap)
        l2b = bcast_tile("l2b", ln2_b.ap)
        xh = [k.sb("xh%d" % i, [128, D], F32) for i in range(2)]
        fh = [k.sb("fh%d" % i, [128, D], F32) for i in range(2)]
        oh = [k.sb("oh%d" % i, [128, D], F32) for i in range(2)]
        junkH = k.sb("junkH", [128, D], F32)
        stH = k.sb("stH", [128, 8], F32)
        for t in range(32):
            b = t % 2
            k.dma("sp", xh[b].t[:], X1[t * 128:(t + 1) * 128, :], reads=[bX1], writes=[xh[b]])
            k.dma("sp", fh[b].t[:], FACC[t * 128:(t + 1) * 128, :], reads=[bFACC, bFACCs], writes=[fh[b]])
            k.op("dve", lambda e: e.tensor_tensor(out=fh[b].t[:], in0=fh[b].t[:], in1=g2bc.t[:], op=ALU.mult),
                 reads=[fh[b], g2bc], writes=[fh[b]])
            k.op("dve", lambda e: e.scalar_tensor_tensor(out=xh[b].t[:], in0=xh[b].t[:], scalar=ALPHA, in1=fh[b].t[:],
                                                         op0=ALU.mult, op1=ALU.add), reads=[xh[b], fh[b]], writes=[xh[b]])
            layer_norm(xh[b], l2g, l2b, oh[b], stH, junkH)
            k.dma("sp", out[t * 128:(t + 1) * 128, :], oh[b].t[:], reads=[oh[b]], writes=[bOUT])
        k.pop()

        finish(nc, k, out)
    return nc


def finish(nc, k, out):
    k.barrier(["sp"])
    return nc


_PROGRAM = None


def host_constants():
    import ml_dtypes
    c = {}
    c["c_identf"] = np.eye(128, dtype=np.float32)
    c["c_identb"] = np.eye(128, dtype=np.float32).astype(ml_dtypes.bfloat16)
    perm = np.zeros((128, 128), np.float32)
    for m in range(128):
        s = m + 32 if (m % 64) < 32 else m - 32
        perm[s, m] = 1.0
    c["c_perm"] = perm.astype(ml_dtypes.bfloat16)
    n = np.arange(NLAT)
    row = (n // 64).astype(np.float32)
    col = (n % 64).astype(np.float32)
    inv = (np.float32(10000.0) ** (-np.arange(16, dtype=np.float32) / np.float32(16))).astype(np.float32)
    ang = np.concatenate([row[:, None] * inv[None, :], col[:, None] * inv[None, :]], axis=-1).astype(np.float32)
    cs = np.cos(ang).astype(np.float32).T
    sn = np.sin(ang).astype(np.float32).T
    c["c_ropec"] = np.ascontiguousarray(np.concatenate([cs, cs, cs, cs], axis=0))
    c["c_ropes"] = np.ascontiguousarray(np.concatenate([-sn, sn, -sn, sn], axis=0))
    rs = np.ones((128, 512), np.float32)
    rs[:, ::128] = 0.0
    c["c_reset"] = rs
    j = np.arange(128)[:, None]
    i = np.arange(128)[None, :]
    c["c_maskf"] = (j <= i).astype(np.float32).astype(ml_dtypes.bfloat16)
    c["c_maskb"] = (j >= i).astype(np.float32).astype(ml_dtypes.bfloat16)
    c["c_iota"] = np.tile(np.arange(512, dtype=np.float32)[None, :], (128, 1))
    tok = (np.arange(32)[None, :] * 128 + np.arange(128)[:, None])
    c["c_tokv"] = np.stack([(tok // 64).astype(np.float32), (tok % 64).astype(np.float32)], axis=-1)
    return c


def make_in_maps(inp):
    consts = host_constants()
    f = lambda a: np.ascontiguousarray(np.asarray(a, dtype=np.float32))
    shared = {
        "w_ada": f(inp["w_ada"][0]), "b_ada": f(inp["b_ada"][0]).reshape(1, -1), "w_in": f(inp["w_in"][0]),
        "w_gate2": f(inp["w_gate2"][0]), "b_gate": np.ascontiguousarray(f(inp["b_gate"][0]).reshape(2, 4, 128).transpose(2, 0, 1)),
        "gla_norm_g": f(inp["gla_norm_g"][0]).reshape(1, -1), "diff_lambda": f(inp["diff_lambda"][0]).reshape(1, -1),
        "diff_norm_g": f(inp["diff_norm_g"][0]).reshape(1, -1), "w_o": f(inp["w_o"][0]),
        "ln1_g": f(inp["ln1_g"][0]).reshape(1, -1), "ln1_b": f(inp["ln1_b"][0]).reshape(1, -1),
        "w_router": f(inp["w_router"][0]), "w1": f(inp["w1"][0]), "w3": f(inp["w3"][0]), "w2": f(inp["w2"][0]),
        "ln2_g": f(inp["ln2_g"][0]).reshape(1, -1), "ln2_b": f(inp["ln2_b"][0]).reshape(1, -1),
    }
    shared.update(consts)
    maps = []
    for core in range(8):
        b = core % 4
        m = dict(shared)
        m["x"] = f(inp["x"][b])
        m["ctx"] = f(inp["ctx"][b])
        m["cvec"] = np.ascontiguousarray(np.stack([np.asarray(inp["c"][b], np.float32), np.asarray(inp["c_ctx"], np.float32)]))
        maps.append(m)
    return maps


def kernel(**inputs):
    nc = build_program()
    in_maps = make_in_maps(inputs)
    in_maps = [{kk: v for kk, v in m.items() if kk in nc.used_inputs} for m in in_maps]
    res = run_bass_kernel_spmd(nc, in_maps, core_ids=list(range(8)))
    outs = [np.asarray(res.results[b]["out"]) for b in range(4)]
    return np.stack(outs, axis=0).astype(np.float32)
```

```python
import math
from contextlib import ExitStack

import numpy as np
import concourse.bass as bass
import concourse.mybir as mybir
from concourse.bass_utils import run_bass_kernel_spmd

F32 = mybir.dt.float32
F32R = mybir.dt.float32r
BF16 = mybir.dt.bfloat16
I32 = mybir.dt.int32
AF = mybir.ActivationFunctionType
ALU = mybir.AluOpType
AX = mybir.AxisListType

D = 2048
NLAT = 4096
NCTX = 256
NTOK = NLAT + NCTX
DIN = 6176
NE = 16
CAP = 512
ALPHA = 2.0 ** 0.25
LAM_INIT = 0.2
NDSEM = 80

STOP_AFTER = "Z"
DEBUG = False
DEBUG_NAMES = ()
B_KINDS = None
ROPE_STAGE = 9
C_HEADS = 8


class Buf:
    def __init__(self, t=None, waw=True):
        self.t = t
        self.w = {}
        self.r = {}
        self.waw = waw


class K:
    def __init__(self, nc, es):
        self.nc = nc
        self.es = es
        self.E = {"pe": nc.tensor, "act": nc.scalar, "dve": nc.vector, "pool": nc.gpsimd, "sp": nc.sync}
        self.sem = {e: es.enter_context(nc.semaphore("s_" + e)) for e in self.E}
        self.seq = {e: 0 for e in self.E}
        self.known = {e: {} for e in self.E}
        self.dsem = [es.enter_context(nc.semaphore("d%d" % i)) for i in range(NDSEM)]
        self.duse = [0] * NDSEM
        self.dcount = 0
        self.dcount_sw = 0
        self.scopes = []
        self.n = 0

    def push(self):
        self.scopes.append(ExitStack())

    def pop(self):
        self.barrier()
        self.scopes.pop().close()

    def sb(self, name, shape, dtype):
        st = self.scopes[-1] if self.scopes else self.es
        return Buf(st.enter_context(self.nc.sbuf_tensor(name, list(shape), dtype)))

    def ps(self, name, shape, dtype):
        st = self.scopes[-1] if self.scopes else self.es
        return Buf(st.enter_context(self.nc.psum_tensor(name, list(shape), dtype)))

    def _semobj(self, key):
        return self.sem[key[1]] if key[0] == "e" else self.dsem[key[1]]

    def _wait(self, eng, key, val):
        if self.known[eng].get(key, 0) >= val:
            return
        self.E[eng].wait_ge(self._semobj(key), val)
        self.known[eng][key] = val

    def _deps(self, eng, reads, writes):
        for b in reads:
            for kk, v in b.w.items():
                if eng == "pe" and kk == ("e", "pe"):
                    continue
                self._wait(eng, kk, v)
        for b in writes:
            if b.waw:
                for kk, v in b.w.items():
                    if eng == "pe" and kk == ("e", "pe"):
                        continue
                    self._wait(eng, kk, v)
            for kk, v in b.r.items():
                if eng == "pe" and kk == ("e", "pe"):
                    continue
                self._wait(eng, kk, v)

    def _commit(self, key, val, reads, writes):
        for b in reads:
            b.r[key] = max(b.r.get(key, 0), val)
        for b in writes:
            if b.waw:
                b.w = {key: val}
                b.r = {}
            else:
                b.w[key] = max(b.w.get(key, 0), val)

    def op(self, eng, fn, reads=(), writes=()):
        self._deps(eng, reads, writes)
        inst = fn(self.E[eng])
        self.seq[eng] += 1
        inst.then_inc(self.sem[eng], 1)
        self._commit(("e", eng), self.seq[eng], reads, writes)
        self.n += 1

    def _dslot(self, q):
        half = NDSEM // 2
        if q == "pool":
            i = half + self.dcount_sw % half
            self.dcount_sw += 1
        else:
            i = self.dcount % half
            self.dcount += 1
        if self.duse[i] > 0:
            self._wait(q, ("d", i), self.duse[i] * 16)
        return i

    def dma(self, q, out, in_, reads=(), writes=(), **kw):
        self._deps(q, reads, writes)
        i = self._dslot(q)
        inst = self.E[q].dma_start(out=out, in_=in_, **kw)
        self.duse[i] += 1
        inst.then_inc(self.dsem[i], 16)
        self._commit(("d", i), self.duse[i] * 16, reads, writes)
        self.n += 1

    def idma(self, fn, reads=(), writes=()):
        q = "pool"
        self._deps(q, reads, writes)
        i = self._dslot(q)
        inst = fn(self.E[q])
        self.duse[i] += 1
        inst.then_inc(self.dsem[i], 16)
        self._commit(("d", i), self.duse[i] * 16, reads, writes)
        self.n += 1

    def barrier(self, engines=None):
        for e in (engines or list(self.E)):
            for f in self.E:
                if self.seq[f] > 0 and not (e == f == "pe"):
                    self._wait(e, ("e", f), self.seq[f])
            for i in range(NDSEM):
                if self.duse[i] > 0:
                    self._wait(e, ("d", i), self.duse[i] * 16)


def build_program():
    nc = bass.Bass("TRN2", target_bir_lowering=False)

    used_inputs = []

    class Lazy:
        def __init__(self, name, shape, dt=F32):
            self.name, self.shape, self.dt, self._ap = name, list(shape), dt, None

        @property
        def ap(self):
            if self._ap is None:
                self._ap = nc.dram_tensor(self.name, self.shape, self.dt, kind="ExternalInput").ap()
                used_inputs.append(self.name)
            return self._ap

    def din(name, shape, dt=F32):
        return Lazy(name, shape, dt)

    def dscr(name, shape, dt=F32):
        kind = "ExternalOutput" if (DEBUG and name in DEBUG_NAMES) else "Internal"
        return nc.dram_tensor(name, list(shape), dt, kind=kind).ap()

    x = din("x", [NLAT, D])
    ctx = din("ctx", [NCTX, D])
    cvec = din("cvec", [2, D])
    w_ada = din("w_ada", [D, 6 * D])
    b_ada = din("b_ada", [1, 6 * D])
    w_in = din("w_in", [D, DIN])
    w_gate2 = din("w_gate2", [2, 16, 512])
    b_gate = din("b_gate", [128, 2, 4])
    gla_norm_g = din("gla_norm_g", [1, 256])
    diff_lambda = din("diff_lambda", [1, 256])
    diff_norm_g = din("diff_norm_g", [1, 128])
    w_o = din("w_o", [D, D])
    ln1_g = din("ln1_g", [1, D])
    ln1_b = din("ln1_b", [1, D])
    w_router = din("w_router", [D, NE])
    w1 = din("w1", [NE, D, D])
    w3 = din("w3", [NE, D, D])
    w2 = din("w2", [NE, D, D])
    ln2_g = din("ln2_g", [1, D])
    ln2_b = din("ln2_b", [1, D])
    c_identf = din("c_identf", [128, 128])
    c_identb = din("c_identb", [128, 128], BF16)
    c_perm = din("c_perm", [128, 128], BF16)
    c_ropec = din("c_ropec", [128, NLAT])
    c_ropes = din("c_ropes", [128, NLAT])
    c_reset = din("c_reset", [128, 512])
    c_maskf = din("c_maskf", [128, 128], BF16)
    c_maskb = din("c_maskb", [128, 128], BF16)
    c_iota = din("c_iota", [128, 512])
    c_tokv = din("c_tokv", [128, 32, 2])
    out = nc.dram_tensor("out", [NLAT, D], F32, kind="ExternalOutput").ap()
    nc.used_inputs = used_inputs

    MOD = dscr("MOD", [2, 6 * D])
    AT = dscr("AT", [2, 4, 128, NTOK], BF16)
    BT = dscr("BT", [2, 4, 128, NTOK], BF16)
    ETOT = dscr("ETOT", [2, 128, 4, 34])
    VG = dscr("VG", [NTOK, 1024], BF16)
    RG = dscr("RG", [NLAT, 1024])
    QDT = dscr("QDT", [8, 128, NLAT], BF16)
    KDT = dscr("KDT", [8, 128, NTOK], BF16)
    VD = dscr("VD", [NTOK, 1024], BF16)
    OFS = dscr("OFS", [NLAT, 1024])
    AL = dscr("AL", [NLAT, D])
    X1 = dscr("X1", [NLAT, D])
    H = dscr("H", [NLAT, D])
    AFF = dscr("AFF", [NLAT, NE])
    FACC = dscr("FACC", [NLAT, D])
    IDXD = dscr("IDXD", [128, NE, 4], I32)
    GATED = dscr("GATED", [128, NE, 4])
    bMOD, bAT, bBT, bETOT, bVG, bRG, bQDT, bKDT, bVD, bOFS, bAL, bX1, bH, bAFF, bFACC, bOUT = [
        Buf(None, waw=False) for _ in range(16)]
    bFACCs = Buf(None, waw=True)

    with ExitStack() as es:
        k = K(nc, es)
        PS = [k.ps("psb%d" % i, [128, 512], F32) for i in range(8)]
        identf = k.sb("identf", [128, 128], F32)
        identb = k.sb("identb", [128, 128], BF16)
        k.dma("sp", identf.t[:], c_identf.ap, writes=[identf])
        k.dma("sp", identb.t[:], c_identb.ap, writes=[identb])
        modT = k.sb("modT", [128, 2, 96], F32)

        k.push()
        cT = k.sb("cT", [128, 16, 128], F32)
        k.op("pool", lambda e: e.memset(cT.t[:], 0.0), writes=[cT])
        with nc.allow_non_contiguous_dma(reason="tiny transposed vector load"):
            for r in range(2):
                k.dma("sp", cT.t[:, :, r], cvec.ap[r, :].rearrange("(c p) -> p c", p=128), writes=[cT])
        scT = k.sb("scT", [128, 16, 128], F32R)
        k.op("act", lambda e: e.activation(out=scT.t[:], in_=cT.t[:], func=AF.Silu), reads=[cT], writes=[scT])
        bada2 = k.sb("bada2", [2, 6 * D], F32)
        k.dma("sp", bada2.t[:], b_ada.ap.partition_broadcast(2), writes=[bada2])
        mod_sb = k.sb("mod_sb", [2, 6 * D], F32)
        slabs = [k.sb("aslab%d" % i, [128, 16, 512], F32R) for i in range(2)]
        for blk in range(24):
            slab = slabs[blk % 2]
            k.dma("pool", slab.t[:], w_ada.ap[:, blk * 512:(blk + 1) * 512].rearrange("(c p) f -> p c f", p=128),
                  writes=[slab])
            ps = PS[blk % 2]
            for c in range(16):
                k.op("pe", lambda e: e.matmul(ps.t[:], scT.t[:, c, :], slab.t[:, c, :], start=(c == 0), stop=(c == 15)),
                     reads=[scT, slab], writes=[ps])
            k.op("dve", lambda e: e.tensor_tensor(out=mod_sb.t[:, blk * 512:(blk + 1) * 512], in0=ps.t[0:2, :],
                                                  in1=bada2.t[:, blk * 512:(blk + 1) * 512], op=ALU.add),
                 reads=[ps, bada2], writes=[mod_sb])
        k.dma("sp", MOD, mod_sb.t[:], reads=[mod_sb], writes=[bMOD])
        with nc.allow_non_contiguous_dma(reason="small transposed reload of modulation vectors"):
            k.dma("sp", modT.t[:], MOD.rearrange("r (j p) -> p r j", p=128), reads=[bMOD], writes=[modT])
        for j0 in (16, 64):
            k.op("dve", lambda e: e.tensor_scalar(out=modT.t[:, :, j0:j0 + 16], in0=modT.t[:, :, j0:j0 + 16],
                                                  scalar1=1.0, scalar2=None, op0=ALU.add),
                 reads=[modT], writes=[modT])
        k.pop()
        if STOP_AFTER == "A":
            return finish(nc, k, out)


        k.push()
        Cblk = k.sb("Cblk", [128, 512], F32)
        Sblk = k.sb("Sblk", [128, 512], F32)
        qraw = [k.sb("qraw%d" % i, [128, 512], BF16) for i in range(2)]
        uT = k.sb("uT", [128, 16, 512], F32R)
        xt = [k.sb("xt%d" % i, [128, D], F32) for i in range(2)]
        slabs = [k.sb("bslab%d" % i, [128, 16, 512], F32R) for i in range(2)]
        wg2 = k.sb("wg2", [48, 512], F32)
        k.dma("sp", wg2.t[0:16, :], w_gate2.ap[0], writes=[wg2])
        k.dma("sp", wg2.t[32:48, :], w_gate2.ap[1], writes=[wg2])
        negbg = k.sb("negbg", [128, 2, 4], F32)
        k.dma("sp", negbg.t[:], b_gate.ap, writes=[negbg])
        k.op("dve", lambda e: e.tensor_scalar(out=negbg.t[:], in0=negbg.t[:], scalar1=-1.0, scalar2=None, op0=ALU.mult),
             reads=[negbg], writes=[negbg])
        perm = k.sb("perm", [128, 128], BF16)
        k.dma("sp", perm.t[:], c_perm.ap, writes=[perm])
        resetm = k.sb("resetm", [128, 512], F32)
        k.dma("sp", resetm.t[:], c_reset.ap, writes=[resetm])
        lrT = k.sb("lrT", [48, 512], F32)
        zf = xt[0]
        k.op("pool", lambda e: e.memset(zf.t[:], 0.0), writes=[zf])
        lrslab = k.sb("lrslab", [128, 16, 128], F32R)
        k.op("act", lambda e: e.activation(out=lrslab.t[:].rearrange("p c f -> p (c f)"), in_=zf.t[:], func=AF.Copy),
             reads=[zf], writes=[lrslab])
        with nc.allow_non_contiguous_dma(reason="small low-rank gate columns"):
            for d in range(2):
                k.dma("pool", lrslab.t[:, :, d * 32:d * 32 + 16],
                      w_in.ap[:, 3072 + d * 16:3072 + d * 16 + 16].rearrange("(c p) f -> p c f", p=128), writes=[lrslab])
        EA = k.sb("EA", [128, 2, 4, 512], BF16)
        EB = k.sb("EB", [128, 2, 4, 512], BF16)
        ABst = k.sb("ABst", [128, 2, 2, 4, 512], BF16)
        etst = k.sb("etst", [128, 2, 4, 4], F32)
        tmpE = [k.sb("tmpE%d" % i, [128, 512], F32) for i in range(2)]
        spt = [k.sb("spt%d" % i, [128, 512], F32) for i in range(2)]
        cumt = [k.sb("cumt%d" % i, [128, 512], F32) for i in range(2)]
        argt = [k.sb("argt%d" % i, [128, 512], F32) for i in range(2)]
        Vst = [k.sb("Vst%d" % i, [128, 4, 512], BF16) for i in range(2)]
        Rst = [k.sb("Rst0", [128, 4, 512], F32)] * 2
        QKst = [k.sb("QKst%d" % i, [128, 4, 512], BF16) for i in range(2)]
        t1 = tmpE
        t2 = spt
        SLABS = [("glr", 3072, 32), ("gq", 0, 512), ("gk", 512, 512), ("gv", 1024, 512), ("gv", 1536, 512),
                 ("gr", 2048, 512), ("gr", 2560, 512), ("dq", 3104, 512), ("dq", 3616, 512),
                 ("dk", 4128, 512), ("dk", 4640, 512), ("dv", 5152, 512), ("dv", 5664, 512)]
        cnt = {"x": 0, "slab": 0, "acc": 0, "g": 0, "st": 0, "r": 0}
        NBLK = 9 if STOP_AFTER != "B1" else 2
        for tb in range(NBLK):
            isctx = tb == 0
            NT = 256 if isctx else 512
            tok0 = 0 if isctx else 256 + (tb - 1) * 512
            lat0 = 0 if isctx else (tb - 1) * 512
            mr = 1 if isctx else 0
            src = ctx.ap if isctx else x.ap[lat0:lat0 + 512, :]
            nt = NT // 128
            for t in range(nt):
                xb = xt[cnt["x"] % 2]
                cnt["x"] += 1
                k.dma("sp", xb.t[:], src[t * 128:(t + 1) * 128, :], writes=[xb])
                for c in range(16):
                    pb = PS[c // 4]
                    k.op("pe", lambda e: e.transpose(pb.t[:, (c % 4) * 128:(c % 4 + 1) * 128], xb.t[:, c * 128:(c + 1) * 128],
                                                     identf.t[:]), reads=[xb, identf], writes=[pb])
                for c in range(16):
                    pb = PS[c // 4]
                    k.op("act", lambda e: e.activation(out=uT.t[:, c, t * 128:(t + 1) * 128],
                                                       in_=pb.t[:, (c % 4) * 128:(c % 4 + 1) * 128], func=AF.Identity,
                                                       scale=modT.t[:, mr, 16 + c:17 + c], bias=modT.t[:, mr, c:c + 1]),
                         reads=[pb, modT], writes=[uT])
            if not isctx:
                k.dma("sp", Cblk.t[:], c_ropec.ap[:, lat0:lat0 + 512], writes=[Cblk])
                k.dma("sp", Sblk.t[:], c_ropes.ap[:, lat0:lat0 + 512], writes=[Sblk])
            half = {}
            for (kind, c0, ncols) in SLABS:
                hf = half.get(kind, 0)
                half[kind] = hf + 1
                if isctx and kind in ("gr", "dq"):
                    continue
                if B_KINDS is not None and kind not in B_KINDS:
                    continue
                if kind != "glr":
                    slab = slabs[cnt["slab"] % 2]
                    cnt["slab"] += 1
                if kind == "glr":
                    slab = lrslab
                else:
                    k.dma("pool", slab.t[:], w_in.ap[:, c0:c0 + 512].rearrange("(c p) f -> p c f", p=128), writes=[slab])

                def acc_feat(j):
                    pa = PS[4 + cnt["acc"] % 2]
                    cnt["acc"] += 1
                    for c in range(16):
                        k.op("pe", lambda e: e.matmul(pa.t[:, :NT], slab.t[:, c, j * 128:(j + 1) * 128], uT.t[:, c, :NT],
                                                      start=(c == 0), stop=(c == 15)), reads=[slab, uT], writes=[pa])
                    return pa

                def acc_tok(t):
                    pa = PS[4 + cnt["acc"] % 2]
                    cnt["acc"] += 1
                    for c in range(16):
                        k.op("pe", lambda e: e.matmul(pa.t[:], uT.t[:, c, t * 128:(t + 1) * 128], slab.t[:, c, :],
                                                      start=(c == 0), stop=(c == 15)), reads=[slab, uT], writes=[pa])
                    return pa

                if kind == "glr":
                    pa = acc_feat(0)
                    k.op("act", lambda e: e.activation(out=lrT.t[:, :NT], in_=pa.t[0:48, :NT], func=AF.Copy),
                         reads=[pa], writes=[lrT])
                    nch = NT // 128
                    for d in range(2):
                        for j in range(4):
                            g = cnt["g"] % 2
                            cnt["g"] += 1
                            pz = PS[6 + g]
                            k.op("pe", lambda e: e.matmul(pz.t[:, :NT], wg2.t[d * 32:d * 32 + 16, j * 128:(j + 1) * 128],
                                                          lrT.t[d * 32:d * 32 + 16, :NT], start=True, stop=True),
                                 reads=[wg2, lrT], writes=[pz])
                            k.op("act", lambda e: e.activation(out=tmpE[g].t[:, :NT], in_=pz.t[:, :NT], func=AF.Exp,
                                                               scale=-1.0, bias=negbg.t[:, d, j:j + 1]),
                                 reads=[pz, negbg], writes=[tmpE[g]])
                            k.op("act", lambda e: e.activation(out=spt[g].t[:, :NT], in_=tmpE[g].t[:, :NT], func=AF.Ln,
                                                               scale=1.0, bias=1.0), reads=[tmpE[g]], writes=[spt[g]])
                            k.op("dve", lambda e: e.tensor_tensor_scan(out=cumt[g].t[:, :NT], data0=resetm.t[:, :NT],
                                                                       data1=spt[g].t[:, :NT], initial=0.0, op0=ALU.mult,
                                                                       op1=ALU.add), reads=[resetm, spt[g]], writes=[cumt[g]])
                            if d == 0:
                                arg = cumt[g]
                            else:
                                arg = argt[g]
                                k.op("dve", lambda e: e.tensor_tensor(out=arg.t[:, :NT], in0=cumt[g].t[:, :NT],
                                                                      in1=spt[g].t[:, :NT], op=ALU.subtract),
                                     reads=[cumt[g], spt[g]], writes=[arg])
                            sa = -1.0 / 16 if d == 0 else 1.0 / 16
                            k.op("act", lambda e: e.activation(out=EA.t[:, d, j, :NT], in_=arg.t[:, :NT], func=AF.Exp, scale=sa),
                                 reads=[arg], writes=[EA])
                            k.op("act", lambda e: e.activation(out=EB.t[:, d, j, :NT], in_=arg.t[:, :NT], func=AF.Exp, scale=-sa),
                                 reads=[arg], writes=[EB])
                            k.op("act", lambda e: e.activation(out=etst.t[:, d, j, 0:nch], in_=cumt[g].t[:, 127:NT:128],
                                                               func=AF.Exp, scale=-1.0 / 16), reads=[cumt[g]], writes=[etst])
                    ch0 = tok0 // 128
                    with nc.allow_non_contiguous_dma(reason="tiny per-chunk decay totals"):
                        for d in range(2):
                            k.dma("sp", ETOT[d, :, :, ch0:ch0 + nch], etst.t[:, d, :, 0:nch], reads=[etst], writes=[bETOT])
                elif kind in ("gq", "gk"):
                    isq = kind == "gq"
                    for j in range(4):
                        pa = acc_feat(j)
                        for d in range(2):
                            if isq:
                                k.op("dve", lambda e: e.scalar_tensor_tensor(out=ABst.t[:, 0, d, j, :NT], in0=pa.t[:, :NT],
                                                                             scalar=128.0 ** -0.5, in1=EA.t[:, d, j, :NT],
                                                                             op0=ALU.mult, op1=ALU.mult),
                                     reads=[pa, EA], writes=[ABst])
                            else:
                                k.op("dve", lambda e: e.tensor_tensor(out=ABst.t[:, 1, d, j, :NT], in0=pa.t[:, :NT],
                                                                      in1=EB.t[:, d, j, :NT], op=ALU.mult),
                                     reads=[pa, EB], writes=[ABst])
                    dst = AT if isq else BT
                    k.dma("sp", dst.rearrange("d j p n -> p d j n")[:, :, :, tok0:tok0 + NT],
                          ABst.t[:, 0 if isq else 1, :, :, :NT], reads=[ABst], writes=[bAT if isq else bBT])
                elif kind in ("gv", "dv", "gr"):
                    if kind == "gr":
                        st = Rst[cnt["r"] % 2]
                        cnt["r"] += 1
                    else:
                        st = Vst[cnt["st"] % 2]
                        cnt["st"] += 1
                    for t in range(nt):
                        pa = acc_tok(t)
                        fn = AF.Silu if kind == "gr" else AF.Copy
                        k.op("act", lambda e: e.activation(out=st.t[:, t, :], in_=pa.t[:], func=fn), reads=[pa], writes=[st])
                    if kind == "gr":
                        k.dma("sp", RG[lat0:lat0 + 512, hf * 512:(hf + 1) * 512].rearrange("(t p) f -> p t f", p=128),
                              st.t[:, 0:nt, :], reads=[st], writes=[bRG])
                    else:
                        dst, bd = (VG, bVG) if kind == "gv" else (VD, bVD)
                        k.dma("sp", dst[tok0:tok0 + NT, hf * 512:(hf + 1) * 512].rearrange("(t p) f -> p t f", p=128),
                              st.t[:, 0:nt, :], reads=[st], writes=[bd])
                elif kind in ("dq", "dk"):
                    st = QKst[cnt["st"] % 2]
                    cnt["st"] += 1
                    sc = 0.125 if kind == "dq" else 1.0
                    for jj in range(4):
                        pa = acc_feat(jj)
                        if isctx:
                            k.op("act", lambda e: e.activation(out=st.t[:, jj, :NT], in_=pa.t[:, :NT], func=AF.Copy),
                                 reads=[pa], writes=[st])
                            continue
                        g = cnt["g"] % 2
                        cnt["g"] += 1
                        k.op("act", lambda e: e.activation(out=qraw[g].t[:], in_=pa.t[:], func=AF.Copy), reads=[pa], writes=[qraw[g]])
                        pz = PS[6 + g]
                        if ROPE_STAGE >= 1:
                            k.op("pe", lambda e: e.matmul(pz.t[:], perm.t[:], qraw[g].t[:], start=True, stop=True),
                                 reads=[perm, qraw[g]], writes=[pz])
                        qf, pzf = argt[g], cumt[g]
                        if ROPE_STAGE >= 2:
                            k.op("act", lambda e: e.activation(out=qf.t[:], in_=pa.t[:], func=AF.Copy), reads=[pa], writes=[qf])
                            k.op("dve", lambda e: e.tensor_tensor(out=t1[g].t[:], in0=qf.t[:], in1=Cblk.t[:], op=ALU.mult),
                                 reads=[qf, Cblk], writes=[t1[g]])
                        if ROPE_STAGE >= 3:
                            k.op("act", lambda e: e.activation(out=pzf.t[:], in_=pz.t[:], func=AF.Copy), reads=[pz], writes=[pzf])
                            k.op("dve", lambda e: e.tensor_tensor(out=t2[g].t[:], in0=pzf.t[:], in1=Sblk.t[:], op=ALU.mult),
                                 reads=[pzf, Sblk], writes=[t2[g]])
                        if ROPE_STAGE >= 4:
                            k.op("dve", lambda e: e.tensor_tensor(out=t1[g].t[:], in0=t1[g].t[:], in1=t2[g].t[:], op=ALU.add),
                                 reads=[t1[g], t2[g]], writes=[t1[g]])
                            k.op("act", lambda e: e.activation(out=st.t[:, jj, :], in_=t1[g].t[:], func=AF.Copy, scale=sc),
                                 reads=[t1[g]], writes=[st])
                        else:
                            k.op("act", lambda e: e.activation(out=st.t[:, jj, :], in_=pa.t[:], func=AF.Copy), reads=[pa], writes=[st])
                    if kind == "dq":
                        k.dma("sp", QDT[hf * 4:hf * 4 + 4, :, lat0:lat0 + 512].rearrange("h p n -> p h n"), st.t[:],
                              reads=[st], writes=[bQDT])
                    else:
                        k.dma("sp", KDT[hf * 4:hf * 4 + 4, :, tok0:tok0 + NT].rearrange("h p n -> p h n"), st.t[:, :, :NT],
                              reads=[st], writes=[bKDT])
        k.pop()
        if STOP_AFTER in ("B", "B1"):
            return finish(nc, k, out)


        k.push()
        KT = k.sb("KT", [128, NTOK], BF16)
        Vh = k.sb("Vh", [128, 34, 132], BF16)
        k.op("pool", lambda e: e.memset(Vh.t[:, :, 128:129], 1.0), writes=[Vh])
        QT = [k.sb("QT%d" % i, [128, 512], BF16) for i in range(2)]
        PT = [k.sb("PT%d" % i, [128, 512], BF16) for i in range(3)]
        Om = [k.sb("Om%d" % i, [128, 4, 128], F32) for i in range(2)]
        dd = k.sb("dd", [128, 4, 128], F32)
        junk = k.sb("junk", [128, 256], F32)
        ost = [k.sb("ost%d" % i, [128, 4, 128], F32) for i in range(2)]
        rl = k.sb("rl", [128, 8], F32)
        ssq = k.sb("ssq", [128, 4], F32)
        rstd = k.sb("rstd", [128, 4], F32)
        dl = k.sb("dl", [128, 256], F32)
        k.dma("sp", dl.t[:], diff_lambda.ap.partition_broadcast(128), writes=[dl])
        prod = k.sb("prod", [128, 2, 64], F32)
        for i in range(2):
            k.op("dve", lambda e: e.tensor_tensor(out=prod.t[:, i, :], in0=dl.t[:, i * 128:i * 128 + 64],
                                                  in1=dl.t[:, i * 128 + 64:i * 128 + 128], op=ALU.mult), reads=[dl], writes=[prod])
        sums = k.sb("sums", [128, 2], F32)
        k.op("dve", lambda e: e.tensor_reduce(out=sums.t[:], in_=prod.t[:], axis=AX.X, op=ALU.add), reads=[prod], writes=[sums])
        exl = k.sb("exl", [128, 2], F32)
        k.op("act", lambda e: e.activation(out=exl.t[:], in_=sums.t[:], func=AF.Exp), reads=[sums], writes=[exl])
        neglam = k.sb("neglam", [128, 1], F32)
        k.op("dve", lambda e: e.tensor_tensor(out=neglam.t[:], in0=exl.t[:, 1:2], in1=exl.t[:, 0:1], op=ALU.subtract),
             reads=[exl], writes=[neglam])
        k.op("dve", lambda e: e.tensor_scalar(out=neglam.t[:], in0=neglam.t[:], scalar1=-LAM_INIT, scalar2=None, op0=ALU.add),
             reads=[neglam], writes=[neglam])
        g8 = k.sb("g8", [128, 128], F32)
        k.dma("sp", g8.t[:], diff_norm_g.ap.partition_broadcast(128), writes=[g8])
        k.op("dve", lambda e: e.tensor_scalar(out=g8.t[:], in0=g8.t[:], scalar1=1.0 - LAM_INIT, scalar2=None, op0=ALU.mult),
             reads=[g8], writes=[g8])
        cq = 0
        cp = 0
        NH = C_HEADS if STOP_AFTER != "C1" else 1
        for h in range(NH):
            k.dma("sp", KT.t[:], KDT[h], reads=[bKDT], writes=[KT])
            k.dma("sp", Vh.t[:, :, 0:128], VD[:, h * 128:(h + 1) * 128].rearrange("(t p) f -> p t f", p=128),
                  reads=[bVD], writes=[Vh])
            for qb in range(8):
                QTb = QT[cq % 2]
                osb = ost[cq % 2]
                cq += 1
                k.dma("sp", QTb.t[:], QDT[h, :, qb * 512:(qb + 1) * 512], reads=[bQDT], writes=[QTb])
                steps = [(m, kt) for m in range(2) for kt in range(34)]

                def emit_qk(i):
                    m, kt = steps[i]
                    pS = PS[i % 3]
                    k.op("pe", lambda e: e.matmul(pS.t[:], KT.t[64 * m:64 * m + 64, kt * 128:(kt + 1) * 128],
                                                  QTb.t[64 * m:64 * m + 64, :], start=True, stop=True),
                         reads=[KT, QTb], writes=[pS])

                emit_qk(0)
                emit_qk(1)
                for i, (m, kt) in enumerate(steps):
                    if i + 2 < len(steps):
                        emit_qk(i + 2)
                    pS = PS[i % 3]
                    PTb = PT[cp % 3]
                    cp += 1
                    k.op("act", lambda e: e.activation(out=PTb.t[:], in_=pS.t[:], func=AF.Exp), reads=[pS], writes=[PTb])
                    for qt in range(4):
                        po = PS[3 + qt]
                        k.op("pe", lambda e: e.matmul(po.t[:, 0:129], PTb.t[:, qt * 128:(qt + 1) * 128], Vh.t[:, kt, 0:129],
                                                      start=(kt == 0), stop=(kt == 33)), reads=[PTb, Vh], writes=[po])
                    if kt == 33:
                        for qt in range(4):
                            po = PS[3 + qt]
                            k.op("dve", lambda e: e.reciprocal(out=rl.t[:, m * 4 + qt:m * 4 + qt + 1], in_=po.t[:, 128:129]),
                                 reads=[po], writes=[rl])
                            k.op("act", lambda e: e.activation(out=Om[m].t[:, qt, :], in_=po.t[:, 0:128], func=AF.Copy,
                                                               scale=rl.t[:, m * 4 + qt:m * 4 + qt + 1]),
                                 reads=[po, rl], writes=[Om[m]])
                for qt in range(4):
                    k.op("dve", lambda e: e.scalar_tensor_tensor(out=dd.t[:, qt, :], in0=Om[1].t[:, qt, :], scalar=neglam.t[:, 0:1],
                                                                 in1=Om[0].t[:, qt, :], op0=ALU.mult, op1=ALU.add),
                         reads=[Om[0], Om[1], neglam], writes=[dd])
                    k.op("act", lambda e: e.activation(out=junk.t[:, 0:128], in_=dd.t[:, qt, :], func=AF.Square,
                                                       accum_out=ssq.t[:, qt:qt + 1]), reads=[dd], writes=[junk, ssq])
                k.op("dve", lambda e: e.tensor_scalar(out=rstd.t[:], in0=ssq.t[:], scalar1=1.0 / 128, scalar2=1e-6, op0=ALU.mult,
                                                      op1=ALU.add), reads=[ssq], writes=[rstd])
                k.op("act", lambda e: e.activation(out=rstd.t[:], in_=rstd.t[:], func=AF.Sqrt), reads=[rstd], writes=[rstd])
                k.op("dve", lambda e: e.reciprocal(out=rstd.t[:], in_=rstd.t[:]), reads=[rstd], writes=[rstd])
                for qt in range(4):
                    k.op("dve", lambda e: e.scalar_tensor_tensor(out=osb.t[:, qt, :], in0=dd.t[:, qt, :], scalar=rstd.t[:, qt:qt + 1],
                                                                 in1=g8.t[:], op0=ALU.mult, op1=ALU.mult),
                         reads=[dd, rstd, g8], writes=[osb])
                k.dma("sp", AL[qb * 512:(qb + 1) * 512, 1024 + h * 128:1024 + (h + 1) * 128].rearrange("(t p) f -> p t f", p=128),
                      osb.t[:], reads=[osb], writes=[bAL])
        k.pop()
        if STOP_AFTER in ("C", "C1"):
            return finish(nc, k, out)

        k.push()
        etot = k.sb("etot", [128, 2, 4, 34], F32)
        for d in range(2):
            k.dma("sp", etot.t[:, d], ETOT[d], reads=[bETOT], writes=[etot])
        masks = [k.sb("mask%d" % d, [128, 128], BF16) for d in range(2)]
        k.dma("sp", masks[0].t[:], c_maskf.ap, writes=[masks[0]])
        k.dma("sp", masks[1].t[:], c_maskb.ap, writes=[masks[1]])
        gn = k.sb("gn", [128, 256], F32)
        k.dma("sp", gn.t[:], gla_norm_g.ap.partition_broadcast(128), writes=[gn])
        S32 = [k.sb("S32_%d" % j, [128, 256], F32) for j in range(4)]
        Sb = [k.sb("Sb_%d" % j, [128, 256], BF16) for j in range(4)]
        Ab = [k.sb("Ab%d" % i, [128, 4, 128], BF16) for i in range(2)]
        Bb = [k.sb("Bb%d" % i, [128, 4, 128], BF16) for i in range(2)]
        Vg = [k.sb("Vg%d" % i, [128, 1024], BF16) for i in range(2)]
        OFb = [k.sb("OFb%d" % i, [128, 1024], F32) for i in range(2)]
        Rb = [k.sb("Rb%d" % i, [128, 1024], F32) for i in range(2)]
        OFst = [k.sb("OFst%d" % i, [128, 1024], F32) for i in range(2)]
        gl = [k.sb("gl%d" % i, [128, 1024], F32) for i in range(2)]
        Btok = [k.sb("Btok%d" % i, [128, 128], BF16) for i in range(2)]
        attT = [k.sb("attT%d" % i, [128, 128], BF16) for i in range(2)]
        ste = [k.sb("ste%d" % i, [128, 256], F32) for i in range(2)]
        osb2 = [k.sb("osb2_%d" % i, [128, 256], F32) for i in range(2)]
        junk2 = k.sb("junk2", [128, 256], F32)
        ssq2 = k.sb("ssq2", [128, 4], F32)
        rstd2 = k.sb("rstd2", [128, 4], F32)
        cs = 0
        cx = 0
        for d in range(2):
            for j in range(4):
                k.op("pool", lambda e: e.memset(S32[j].t[:], 0.0), writes=[S32[j]])
                k.op("pool", lambda e: e.memset(Sb[j].t[:], 0.0), writes=[Sb[j]])
            order = list(range(34)) if d == 0 else [1, 0] + list(range(33, 1, -1))
            if STOP_AFTER == "D1":
                order = order[:6]
            for ci in order:
                isctx = ci < 2
                tok0 = ci * 128
                lat0 = tok0 - 256
                b = cs % 2
                cs += 1
                k.dma("sp", Ab[b].t[:], AT[d, :, :, tok0:tok0 + 128].rearrange("j p n -> p j n"), reads=[bAT], writes=[Ab[b]])
                k.dma("sp", Bb[b].t[:], BT[d, :, :, tok0:tok0 + 128].rearrange("j p n -> p j n"), reads=[bBT], writes=[Bb[b]])
                k.dma("sp", Vg[b].t[:], VG[tok0:tok0 + 128, :], reads=[bVG], writes=[Vg[b]])
                if d == 1 and not isctx:
                    k.dma("sp", OFb[b].t[:], OFS[lat0:lat0 + 128, :], reads=[bOFS], writes=[OFb[b]])
                    k.dma("sp", Rb[b].t[:], RG[lat0:lat0 + 128, :], reads=[bRG], writes=[Rb[b]])
                for j in range(4):
                    x2 = cx % 2
                    cx += 1
                    et = etot.t[:, d, j, ci:ci + 1]
                    pT = PS[x2]
                    k.op("pe", lambda e: e.transpose(pT.t[:].bitcast(BF16)[:, 0:128], Bb[b].t[:, j, :], identb.t[:]),
                         reads=[Bb[b], identb], writes=[pT])
                    k.op("act", lambda e: e.activation(out=Btok[x2].t[:], in_=pT.t[:].bitcast(BF16)[:, 0:128], func=AF.Copy),
                         reads=[pT], writes=[Btok[x2]])
                    if d == 1:
                        k.op("dve", lambda e: e.tensor_scalar(out=S32[j].t[:], in0=S32[j].t[:], scalar1=et, scalar2=None,
                                                              op0=ALU.mult), reads=[S32[j], etot], writes=[S32[j]])
                        k.op("act", lambda e: e.activation(out=Sb[j].t[:], in_=S32[j].t[:], func=AF.Copy),
                             reads=[S32[j]], writes=[Sb[j]])
                    if not isctx:
                        pA = PS[2 + x2]
                        k.op("pe", lambda e: e.matmul(pA.t[:, 0:128], Bb[b].t[:, j, :], Ab[b].t[:, j, :], start=True, stop=True),
                             reads=[Ab[b], Bb[b]], writes=[pA])
                        k.op("dve", lambda e: e.tensor_tensor(out=attT[x2].t[:], in0=pA.t[:, 0:128], in1=masks[d].t[:], op=ALU.mult),
                             reads=[pA, masks[d]], writes=[attT[x2]])
                        pO = PS[4 + x2]
                        k.op("pe", lambda e: e.matmul(pO.t[:, 0:256], attT[x2].t[:], Vg[b].t[:, j * 256:(j + 1) * 256],
                                                      start=True, stop=False), reads=[attT[x2], Vg[b]], writes=[pO])
                        k.op("pe", lambda e: e.matmul(pO.t[:, 0:256], Ab[b].t[:, j, :], Sb[j].t[:], start=False, stop=True),
                             reads=[Ab[b], Sb[j]], writes=[pO])
                        if d == 0:
                            k.op("act", lambda e: e.activation(out=OFst[b].t[:, j * 256:(j + 1) * 256], in_=pO.t[:, 0:256],
                                                               func=AF.Copy), reads=[pO], writes=[OFst[b]])
                        else:
                            k.op("act", lambda e: e.activation(out=osb2[x2].t[:], in_=pO.t[:, 0:256], func=AF.Copy),
                                 reads=[pO], writes=[osb2[x2]])
                            k.op("dve", lambda e: e.tensor_tensor(out=gl[b].t[:, j * 256:(j + 1) * 256], in0=osb2[x2].t[:],
                                                                  in1=OFb[b].t[:, j * 256:(j + 1) * 256], op=ALU.add),
                                 reads=[osb2[x2], OFb[b]], writes=[gl[b]])
                            k.op("act", lambda e: e.activation(out=junk2.t[:], in_=gl[b].t[:, j * 256:(j + 1) * 256], func=AF.Square,
                                                               accum_out=ssq2.t[:, j:j + 1]), reads=[gl[b]], writes=[junk2, ssq2])
                    pS2 = PS[6 + x2]
                    k.op("pe", lambda e: e.matmul(pS2.t[:, 0:256], Btok[x2].t[:], Vg[b].t[:, j * 256:(j + 1) * 256],
                                                  start=True, stop=True), reads=[Btok[x2], Vg[b]], writes=[pS2])
                    if d == 0:
                        k.op("act", lambda e: e.activation(out=ste[x2].t[:], in_=pS2.t[:, 0:256], func=AF.Copy, scale=et),
                             reads=[pS2, etot], writes=[ste[x2]])
                        k.op("dve", lambda e: e.scalar_tensor_tensor(out=S32[j].t[:], in0=S32[j].t[:], scalar=et, in1=ste[x2].t[:],
                                                                     op0=ALU.mult, op1=ALU.add),
                             reads=[S32[j], ste[x2], etot], writes=[S32[j]])
                        k.op("act", lambda e: e.activation(out=Sb[j].t[:], in_=S32[j].t[:], func=AF.Copy),
                             reads=[S32[j]], writes=[Sb[j]])
                    else:
                        k.op("act", lambda e: e.activation(out=ste[x2].t[:], in_=pS2.t[:, 0:256], func=AF.Copy),
                             reads=[pS2], writes=[ste[x2]])
                        k.op("dve", lambda e: e.tensor_tensor(out=S32[j].t[:], in0=S32[j].t[:], in1=ste[x2].t[:], op=ALU.add),
                             reads=[S32[j], ste[x2]], writes=[S32[j]])
                if not isctx:
                    if d == 0:
                        k.dma("sp", OFS[lat0:lat0 + 128, :], OFst[b].t[:], reads=[OFst[b]], writes=[bOFS])
                    else:
                        k.op("dve", lambda e: e.tensor_scalar(out=rstd2.t[:], in0=ssq2.t[:], scalar1=1.0 / 256, scalar2=1e-6,
                                                              op0=ALU.mult, op1=ALU.add), reads=[ssq2], writes=[rstd2])
                        k.op("act", lambda e: e.activation(out=rstd2.t[:], in_=rstd2.t[:], func=AF.Sqrt), reads=[rstd2], writes=[rstd2])
                        k.op("dve", lambda e: e.reciprocal(out=rstd2.t[:], in_=rstd2.t[:]), reads=[rstd2], writes=[rstd2])
                        for j in range(4):
                            k.op("dve", lambda e: e.scalar_tensor_tensor(out=gl[b].t[:, j * 256:(j + 1) * 256],
                                                                         in0=gl[b].t[:, j * 256:(j + 1) * 256],
                                                                         scalar=rstd2.t[:, j:j + 1], in1=gn.t[:], op0=ALU.mult,
                                                                         op1=ALU.mult), reads=[gl[b], rstd2, gn], writes=[gl[b]])
                        k.op("pool", lambda e: e.tensor_tensor(out=gl[b].t[:], in0=gl[b].t[:], in1=Rb[b].t[:], op=ALU.mult),
                             reads=[gl[b], Rb[b]], writes=[gl[b]])
                        k.dma("sp", AL[lat0:lat0 + 128, 0:1024], gl[b].t[:], reads=[gl[b]], writes=[bAL])
        k.pop()
        if STOP_AFTER in ("D", "D1"):
            return finish(nc, k, out)


        def bcast_tile(name, src_ap):
            t = k.sb(name, [128, D], F32)
            k.dma("sp", t.t[:], src_ap.partition_broadcast(128), writes=[t])
            return t

        def layer_norm(y, gbc, bbc, outb, st, junkb):
            k.op("act", lambda e: e.activation(out=junkb.t[:], in_=y.t[:], func=AF.Copy, accum_out=st.t[:, 0:1]),
                 reads=[y], writes=[junkb, st])
            k.op("act", lambda e: e.activation(out=junkb.t[:], in_=y.t[:], func=AF.Square, accum_out=st.t[:, 1:2]),
                 reads=[y], writes=[junkb, st])
            k.op("dve", lambda e: e.tensor_scalar(out=st.t[:, 0:2], in0=st.t[:, 0:2], scalar1=1.0 / D, scalar2=None, op0=ALU.mult),
                 reads=[st], writes=[st])
            k.op("dve", lambda e: e.tensor_tensor(out=st.t[:, 2:3], in0=st.t[:, 0:1], in1=st.t[:, 0:1], op=ALU.mult),
                 reads=[st], writes=[st])
            k.op("dve", lambda e: e.tensor_tensor(out=st.t[:, 3:4], in0=st.t[:, 1:2], in1=st.t[:, 2:3], op=ALU.subtract),
                 reads=[st], writes=[st])
            k.op("dve", lambda e: e.tensor_scalar(out=st.t[:, 3:4], in0=st.t[:, 3:4], scalar1=1e-5, scalar2=None, op0=ALU.add),
                 reads=[st], writes=[st])
            k.op("act", lambda e: e.activation(out=st.t[:, 3:4], in_=st.t[:, 3:4], func=AF.Sqrt), reads=[st], writes=[st])
            k.op("dve", lambda e: e.reciprocal(out=st.t[:, 4:5], in_=st.t[:, 3:4]), reads=[st], writes=[st])
            k.op("dve", lambda e: e.scalar_tensor_tensor(out=st.t[:, 5:6], in0=st.t[:, 0:1], scalar=-1.0, in1=st.t[:, 4:5],
                                                         op0=ALU.mult, op1=ALU.mult), reads=[st], writes=[st])
            k.op("act", lambda e: e.activation(out=outb.t[:], in_=y.t[:], func=AF.Identity, scale=st.t[:, 4:5], bias=st.t[:, 5:6]),
                 reads=[y, st], writes=[outb])
            k.op("dve", lambda e: e.tensor_tensor(out=outb.t[:], in0=outb.t[:], in1=gbc.t[:], op=ALU.mult),
                 reads=[outb, gbc], writes=[outb])
            k.op("pool", lambda e: e.tensor_tensor(out=outb.t[:], in0=outb.t[:], in1=bbc.t[:], op=ALU.add),
                 reads=[outb, bbc], writes=[outb])

        affall = k.sb("affall", [128, 32, NE], F32)
        k.push()
        g1bc = bcast_tile("g1bc", MOD[0:1, 2 * D:3 * D])
        l1g = bcast_tile("l1g", ln1_g.ap)
        l1b = bcast_tile("l1b", ln1_b.ap)
        aT = k.sb("aT", [128, 16, 512], F32R)
        oslab = [k.sb("oslab%d" % i, [128, 16, 256], F32R) for i in range(2)]
        osb = k.sb("osbE", [128, 4, D], F32)
        alt = [k.sb("alt%d" % i, [128, D], F32) for i in range(2)]
        xe = k.sb("xe", [128, D], F32)
        x1t = [k.sb("x1t%d" % i, [128, D], F32) for i in range(2)]
        junkE = k.sb("junkE", [128, D], F32)
        stE = k.sb("stE", [128, 8], F32)
        hT = k.sb("hT", [128, 16, 128], F32R)
        wr = k.sb("wr", [128, 16, NE], F32R)
        k.dma("pool", wr.t[:], w_router.ap.rearrange("(c p) e -> p c e", p=128), writes=[wr])
        sm = k.sb("sm", [128, 4], F32)
        ee = k.sb("ee", [128, NE], F32)
        ca = 0
        cso = 0
        NBE = 8 if STOP_AFTER != "E1" else 1
        for tb in range(NBE):
            for t in range(4):
                al = alt[ca % 2]
                ca += 1
                r0 = tb * 512 + t * 128
                k.dma("sp", al.t[:], AL[r0:r0 + 128, :], reads=[bAL], writes=[al])
                for c in range(16):
                    pb = PS[c // 4]
                    k.op("pe", lambda e: e.transpose(pb.t[:, (c % 4) * 128:(c % 4 + 1) * 128], al.t[:, c * 128:(c + 1) * 128],
                                                     identf.t[:]), reads=[al, identf], writes=[pb])
                for q4 in range(4):
                    pb = PS[q4]
                    k.op("act", lambda e: e.activation(out=aT.t[:, q4 * 4:q4 * 4 + 4, t * 128:(t + 1) * 128],
                                                       in_=pb.t[:].rearrange("p (c n) -> p c n", c=4), func=AF.Copy),
                         reads=[pb], writes=[aT])
            for cb in range(8):
                sl = oslab[cso % 2]
                cso += 1
                k.dma("pool", sl.t[:], w_o.ap[:, cb * 256:(cb + 1) * 256].rearrange("(c p) f -> p c f", p=128), writes=[sl])
                for t in range(4):
                    pa = PS[4 + (t % 4)]
                    for c in range(16):
                        k.op("pe", lambda e: e.matmul(pa.t[:, 0:256], aT.t[:, c, t * 128:(t + 1) * 128], sl.t[:, c, :],
                                                      start=(c == 0), stop=(c == 15)), reads=[aT, sl], writes=[pa])
                    k.op("act", lambda e: e.activation(out=osb.t[:, t, cb * 256:(cb + 1) * 256], in_=pa.t[:, 0:256], func=AF.Copy),
                         reads=[pa], writes=[osb])
            for t in range(4):
                r0 = tb * 512 + t * 128
                tile_i = tb * 4 + t
                x1 = x1t[tile_i % 2]
                k.dma("sp", xe.t[:], x.ap[r0:r0 + 128, :], writes=[xe])
                k.op("dve", lambda e: e.tensor_tensor(out=osb.t[:, t, :], in0=osb.t[:, t, :], in1=g1bc.t[:], op=ALU.mult),
                     reads=[osb, g1bc], writes=[osb])
                k.op("dve", lambda e: e.scalar_tensor_tensor(out=xe.t[:], in0=xe.t[:], scalar=ALPHA, in1=osb.t[:, t, :],
                                                             op0=ALU.mult, op1=ALU.add), reads=[xe, osb], writes=[xe])
                layer_norm(xe, l1g, l1b, x1, stE, junkE)
                k.dma("sp", X1[r0:r0 + 128, :], x1.t[:], reads=[x1], writes=[bX1])
                for c in range(16):
                    pb = PS[c // 4]
                    k.op("pe", lambda e: e.transpose(pb.t[:, (c % 4) * 128:(c % 4 + 1) * 128], x1.t[:, c * 128:(c + 1) * 128],
                                                     identf.t[:]), reads=[x1, identf], writes=[pb])
                for c in range(16):
                    pb = PS[c // 4]
                    k.op("act", lambda e: e.activation(out=hT.t[:, c, :], in_=pb.t[:, (c % 4) * 128:(c % 4 + 1) * 128],
                                                       func=AF.Identity, scale=modT.t[:, 0, 64 + c:65 + c],
                                                       bias=modT.t[:, 0, 48 + c:49 + c]), reads=[pb, modT], writes=[hT])
                pl = PS[4]
                for c in range(16):
                    k.op("pe", lambda e: e.matmul(pl.t[:, 0:NE], hT.t[:, c, :], wr.t[:, c, :], start=(c == 0), stop=(c == 15)),
                         reads=[hT, wr], writes=[pl])
                k.op("dve", lambda e: e.tensor_reduce(out=sm.t[:, 0:1], in_=pl.t[:, 0:NE], axis=AX.X, op=ALU.max),
                     reads=[pl], writes=[sm])
                k.op("dve", lambda e: e.tensor_scalar(out=sm.t[:, 1:2], in0=sm.t[:, 0:1], scalar1=-1.0, scalar2=None, op0=ALU.mult),
                     reads=[sm], writes=[sm])
                k.op("act", lambda e: e.activation(out=ee.t[:], in_=pl.t[:, 0:NE], func=AF.Exp, bias=sm.t[:, 1:2],
                                                   accum_out=sm.t[:, 2:3]), reads=[pl, sm], writes=[ee, sm])
                k.op("dve", lambda e: e.reciprocal(out=sm.t[:, 3:4], in_=sm.t[:, 2:3]), reads=[sm], writes=[sm])
                k.op("dve", lambda e: e.tensor_scalar(out=affall.t[:, tile_i, :], in0=ee.t[:], scalar1=sm.t[:, 3:4], scalar2=None,
                                                      op0=ALU.mult), reads=[ee, sm], writes=[affall])
        k.pop()
        if DEBUG and "AFF" in DEBUG_NAMES:
            k.dma("sp", AFF.rearrange("(t p) e -> p t e", p=128), affall.t[:], reads=[affall], writes=[bAFF])
        if STOP_AFTER in ("E", "E1"):
            return finish(nc, k, out)

        idxI = k.sb("idxI", [128, NE, 4], I32)
        gateS = k.sb("gateS", [128, NE, 4], F32)
        k.push()
        affT = k.sb("affT", [16, NLAT], F32)
        for g8i in range(8):
            pb = PS[g8i % 2]
            for t4 in range(4):
                t = g8i * 4 + t4
                k.op("pe", lambda e: e.transpose(pb.t[0:16, t4 * 128:(t4 + 1) * 128], affall.t[:, t, :], identf.t[:]),
                     reads=[affall, identf], writes=[pb])
            k.op("act", lambda e: e.activation(out=affT.t[:, g8i * 512:(g8i + 1) * 512], in_=pb.t[0:16, :], func=AF.Copy),
                 reads=[pb], writes=[affT])
        bs = k.sb("bs", [16, 8], F32)
        junkT = k.sb("junkT", [16, NLAT], F32)
        k.op("dve", lambda e: e.memset(bs.t[:], 0.0), writes=[bs])
        k.op("dve", lambda e: e.memset(bs.t[:, 1:2], 1.0), reads=[bs], writes=[bs])
        for it in range(30):
            k.op("dve", lambda e: e.tensor_scalar(out=bs.t[:, 5:6], in0=bs.t[:, 1:2], scalar1=0.5, scalar2=None, op0=ALU.mult),
                 reads=[bs], writes=[bs])
            k.op("dve", lambda e: e.scalar_tensor_tensor(out=bs.t[:, 2:3], in0=bs.t[:, 0:1], scalar=0.5, in1=bs.t[:, 5:6],
                                                         op0=ALU.mult, op1=ALU.add), reads=[bs], writes=[bs])
            k.op("dve", lambda e: e.tensor_scalar(out=junkT.t[:], in0=affT.t[:], scalar1=bs.t[:, 2:3], scalar2=0.0, op0=ALU.is_ge,
                                                  op1=ALU.add, accum_out=bs.t[:, 3:4]), reads=[affT, bs], writes=[junkT, bs])
            k.op("dve", lambda e: e.tensor_scalar(out=bs.t[:, 4:5], in0=bs.t[:, 3:4], scalar1=float(CAP), scalar2=None,
                                                  op0=ALU.is_ge), reads=[bs], writes=[bs])
            k.op("dve", lambda e: e.tensor_tensor(out=bs.t[:, 5:6], in0=bs.t[:, 2:3], in1=bs.t[:, 0:1], op=ALU.subtract),
                 reads=[bs], writes=[bs])
            k.op("dve", lambda e: e.tensor_tensor(out=bs.t[:, 6:7], in0=bs.t[:, 1:2], in1=bs.t[:, 2:3], op=ALU.subtract),
                 reads=[bs], writes=[bs])
            k.op("dve", lambda e: e.scalar_tensor_tensor(out=bs.t[:, 0:1], in0=bs.t[:, 5:6], scalar=bs.t[:, 4:5], in1=bs.t[:, 0:1],
                                                         op0=ALU.mult, op1=ALU.add), reads=[bs], writes=[bs])
            k.op("dve", lambda e: e.scalar_tensor_tensor(out=bs.t[:, 1:2], in0=bs.t[:, 6:7], scalar=bs.t[:, 4:5], in1=bs.t[:, 2:3],
                                                         op0=ALU.mult, op1=ALU.add), reads=[bs], writes=[bs])
        Mt = k.sb("Mt", [16, NLAT], F32)
        k.op("dve", lambda e: e.tensor_scalar(out=Mt.t[:], in0=affT.t[:], scalar1=bs.t[:, 0:1], scalar2=None, op0=ALU.is_ge),
             reads=[affT, bs], writes=[Mt])
        k.op("dve", lambda e: e.memset(junkT.t[:], 1.0), writes=[junkT])
        cumM = k.sb("cumM", [16, NLAT], F32)
        k.op("dve", lambda e: e.tensor_tensor_scan(out=cumM.t[:], data0=junkT.t[:], data1=Mt.t[:], initial=0.0, op0=ALU.mult,
                                                   op1=ALU.add), reads=[junkT, Mt], writes=[cumM])
        k.op("dve", lambda e: e.scalar_tensor_tensor(out=cumM.t[:], in0=Mt.t[:], scalar=-8193.0, in1=cumM.t[:], op0=ALU.mult,
                                                     op1=ALU.add), reads=[Mt, cumM], writes=[cumM])
        k.op("dve", lambda e: e.tensor_scalar(out=cumM.t[:], in0=cumM.t[:], scalar1=8192.0, scalar2=None, op0=ALU.add),
             reads=[cumM], writes=[cumM])
        slotTM = k.sb("slotTM", [128, 32, NE], F32)
        pb = PS[2]
        for t in range(32):
            k.op("pe", lambda e: e.transpose(pb.t[:, t * 16:(t + 1) * 16], cumM.t[:, t * 128:(t + 1) * 128], identf.t[0:16, 0:16]),
                 reads=[cumM, identf], writes=[pb])
        k.op("act", lambda e: e.activation(out=slotTM.t[:].rearrange("p t e -> p (t e)"), in_=pb.t[:], func=AF.Copy),
             reads=[pb], writes=[slotTM])
        vals = k.sb("vals", [128, 32, NE, 4], F32)
        tokv = k.sb("tokv", [128, 32, 2], F32)
        k.dma("sp", tokv.t[:], c_tokv.ap, writes=[tokv])
        for e_ in range(NE):
            k.op("pool", lambda e: e.tensor_copy(out=vals.t[:, :, e_, 0:2], in_=tokv.t[:]), reads=[tokv], writes=[vals])
        k.op("pool", lambda e: e.tensor_copy(out=vals.t[:, :, :, 2], in_=affall.t[:]), reads=[affall], writes=[vals])
        k.op("pool", lambda e: e.tensor_copy(out=vals.t[:, :, :, 3], in_=affall.t[:]), reads=[affall], writes=[vals])
        iot = k.sb("iot", [128, 512], F32)
        k.dma("sp", iot.t[:], c_iota.ap, writes=[iot])
        Sel = [k.sb("Sel%d" % i, [128, 512], F32) for i in range(3)]
        res = k.sb("resF", [128, 4, 4], F32)
        idf = k.sb("idf", [128, 4], F32)
        csel = 0
        for e_ in range(NE):
            for t in range(32):
                sl = Sel[csel % 3]
                csel += 1
                k.op("dve", lambda e: e.tensor_scalar(out=sl.t[:], in0=iot.t[:], scalar1=slotTM.t[:, t, e_:e_ + 1], scalar2=None,
                                                      op0=ALU.is_equal), reads=[iot, slotTM], writes=[sl])
                for st in range(4):
                    k.op("pe", lambda e: e.matmul(PS[4 + st].t[:, 0:4], sl.t[:, st * 128:(st + 1) * 128], vals.t[:, t, e_, :],
                                                  start=(t == 0), stop=(t == 31)), reads=[sl, vals], writes=[PS[4 + st]])
            for st in range(4):
                k.op("act", lambda e: e.activation(out=res.t[:, st, :], in_=PS[4 + st].t[:, 0:4], func=AF.Copy),
                     reads=[PS[4 + st]], writes=[res])
            k.op("dve", lambda e: e.scalar_tensor_tensor(out=idf.t[:], in0=res.t[:, :, 0], scalar=64.0, in1=res.t[:, :, 1],
                                                         op0=ALU.mult, op1=ALU.add), reads=[res], writes=[idf])
            k.op("dve", lambda e: e.tensor_copy(out=idxI.t[:, e_, :], in_=idf.t[:]), reads=[idf], writes=[idxI])
            k.op("dve", lambda e: e.tensor_copy(out=gateS.t[:, e_, :], in_=res.t[:, :, 2]), reads=[res], writes=[gateS])
        k.pop()
        if DEBUG and "IDXD" in DEBUG_NAMES:
            k.dma("sp", IDXD, idxI.t[:], reads=[idxI], writes=[bAFF])
            k.dma("sp", GATED, gateS.t[:], reads=[gateS], writes=[bAFF])
        if STOP_AFTER == "F":
            return finish(nc, k, out)

        k.push()
        zt = k.sb("zt", [128, D], F32)
        k.op("pool", lambda e: e.memset(zt.t[:], 0.0), writes=[zt])
        for t in range(32):
            k.dma("sp", FACC[t * 128:(t + 1) * 128, :], zt.t[:], reads=[zt], writes=[bFACC])
        xsT = k.sb("xsT", [128, 16, 512], F32R)
        hidT = k.sb("hidT", [128, 16, 512], F32R)
        gsl = [k.sb("gsl%d" % i, [128, 16, 512], F32R) for i in range(2)]
        xg = k.sb("xg", [128, D], F32)
        yst = [k.sb("yst%d" % i, [128, D], F32) for i in range(4)]
        s1 = k.sb("s1", [128, 4, 512], F32)
        t3 = [k.sb("t3_%d" % i, [128, 512], F32) for i in range(2)]
        cg = 0
        cpp = 0
        scat_prev = [None]
        NEX = NE if STOP_AFTER != "G1" else 1
        for e_ in range(NEX):
            for st in range(4):
                k.idma(lambda e: e.indirect_dma_start(out=xg.t[:], out_offset=None, in_=X1,
                                                      in_offset=bass.IndirectOffsetOnAxis(ap=idxI.t[:, e_, st:st + 1], axis=0)),
                       reads=[bX1, idxI], writes=[xg])
                for c in range(16):
                    pb = PS[c // 4]
                    k.op("pe", lambda e: e.transpose(pb.t[:, (c % 4) * 128:(c % 4 + 1) * 128], xg.t[:, c * 128:(c + 1) * 128],
                                                     identf.t[:]), reads=[xg, identf], writes=[pb])
                for c in range(16):
                    pb = PS[c // 4]
                    k.op("act", lambda e: e.activation(out=xsT.t[:, c, st * 128:(st + 1) * 128],
                                                       in_=pb.t[:, (c % 4) * 128:(c % 4 + 1) * 128], func=AF.Identity,
                                                       scale=modT.t[:, 0, 64 + c:65 + c], bias=modT.t[:, 0, 48 + c:49 + c]),
                         reads=[pb, modT], writes=[xsT])
            for fb in range(4):
                for wi, wsrc in enumerate((w1, w3)):
                    sl = gsl[cg % 2]
                    cg += 1
                    k.dma("pool", sl.t[:], wsrc.ap[e_, :, fb * 512:(fb + 1) * 512].rearrange("(c p) f -> p c f", p=128), writes=[sl])
                    for fc in range(4):
                        pa = PS[4 + cpp % 4]
                        cpp += 1
                        for c in range(16):
                            k.op("pe", lambda e: e.matmul(pa.t[:], sl.t[:, c, fc * 128:(fc + 1) * 128], xsT.t[:, c, :],
                                                          start=(c == 0), stop=(c == 15)), reads=[sl, xsT], writes=[pa])
                        if wi == 0:
                            k.op("act", lambda e: e.activation(out=s1.t[:, fc, :], in_=pa.t[:], func=AF.Silu), reads=[pa], writes=[s1])
                        else:
                            tt = t3[fc % 2]
                            k.op("act", lambda e: e.activation(out=tt.t[:], in_=pa.t[:], func=AF.Copy), reads=[pa], writes=[tt])
                            k.op("dve", lambda e: e.tensor_tensor(out=hidT.t[:, fb * 4 + fc, :], in0=tt.t[:], in1=s1.t[:, fc, :],
                                                                  op=ALU.mult), reads=[tt, s1], writes=[hidT])
            for db in range(4):
                sl = gsl[cg % 2]
                cg += 1
                k.dma("pool", sl.t[:], w2.ap[e_, :, db * 512:(db + 1) * 512].rearrange("(c p) f -> p c f", p=128), writes=[sl])
                for st in range(4):
                    pa = PS[4 + cpp % 4]
                    cpp += 1
                    for c in range(16):
                        k.op("pe", lambda e: e.matmul(pa.t[:], hidT.t[:, c, st * 128:(st + 1) * 128], sl.t[:, c, :],
                                                      start=(c == 0), stop=(c == 15)), reads=[sl, hidT], writes=[pa])
                    k.op("act", lambda e: e.activation(out=yst[st].t[:, db * 512:(db + 1) * 512], in_=pa.t[:], func=AF.Copy,
                                                       scale=gateS.t[:, e_, st:st + 1]), reads=[pa, gateS], writes=[yst[st]])
            for st in range(4):
                k.idma(lambda e: e.indirect_dma_start(out=FACC, out_offset=bass.IndirectOffsetOnAxis(ap=idxI.t[:, e_, st:st + 1], axis=0),
                                                      in_=yst[st].t[:], in_offset=None, compute_op=ALU.add),
                       reads=[yst[st], idxI, bFACCs, bFACC], writes=[bFACCs])
        k.pop()
        if STOP_AFTER in ("G", "G1"):
            return finish(nc, k, out)

        k.push()
        g2bc = bcast_tile("g2bc", MOD[0:1, 5 * D:6 * D])
        l2g = bcast_tile("l2g", ln2_g.ap)
        l2b = bcast_tile("l2b", ln2_b.ap)
        xh = [k.sb("xh%d" % i, [128, D], F32) for i in range(2)]
        fh = [k.sb("fh%d" % i, [128, D], F32) for i in range(2)]
        oh = [k.sb("oh%d" % i, [128, D], F32) for i in range(2)]
        junkH = k.sb("junkH", [128, D], F32)
        stH = k.sb("stH", [128, 8], F32)
        for t in range(32):
            b = t % 2
            k.dma("sp", xh[b].t[:], X1[t * 128:(t + 1) * 128, :], reads=[bX1], writes=[xh[b]])
            k.dma("sp", fh[b].t[:], FACC[t * 128:(t + 1) * 128, :], reads=[bFACC, bFACCs], writes=[fh[b]])
            k.op("dve", lambda e: e.tensor_tensor(out=fh[b].t[:], in0=fh[b].t[:], in1=g2bc.t[:], op=ALU.mult),
                 reads=[fh[b], g2bc], writes=[fh[b]])
            k.op("dve", lambda e: e.scalar_tensor_tensor(out=xh[b].t[:], in0=xh[b].t[:], scalar=ALPHA, in1=fh[b].t[:],
                                                         op0=ALU.mult, op1=ALU.add), reads=[xh[b], fh[b]], writes=[xh[b]])
            layer_norm(xh[b], l2g, l2b, oh[b], stH, junkH)
            k.dma("sp", out[t * 128:(t + 1) * 128, :], oh[b].t[:], reads=[oh[b]], writes=[bOUT])
        k.pop()

        finish(nc, k, out)
    return nc


def finish(nc, k, out):
    k.barrier(["sp"])
    return nc


_PROGRAM = None


def host_constants():
    import ml_dtypes
    c = {}
    c["c_identf"] = np.eye(128, dtype=np.float32)
    c["c_identb"] = np.eye(128, dtype=np.float32).astype(ml_dtypes.bfloat16)
    perm = np.zeros((128, 128), np.float32)
    for m in range(128):
        s = m + 32 if (m % 64) < 32 else m - 32
        perm[s, m] = 1.0
    c["c_perm"] = perm.astype(ml_dtypes.bfloat16)
    n = np.arange(NLAT)
    row = (n // 64).astype(np.float32)
    col = (n % 64).astype(np.float32)
    inv = (np.float32(10000.0) ** (-np.arange(16, dtype=np.float32) / np.float32(16))).astype(np.float32)
    ang = np.concatenate([row[:, None] * inv[None, :], col[:, None] * inv[None, :]], axis=-1).astype(np.float32)
    cs = np.cos(ang).astype(np.float32).T
    sn = np.sin(ang).astype(np.float32).T
    c["c_ropec"] = np.ascontiguousarray(np.concatenate([cs, cs, cs, cs], axis=0))
    c["c_ropes"] = np.ascontiguousarray(np.concatenate([-sn, sn, -sn, sn], axis=0))
    rs = np.ones((128, 512), np.float32)
    rs[:, ::128] = 0.0
    c["c_reset"] = rs
    j = np.arange(128)[:, None]
    i = np.arange(128)[None, :]
    c["c_maskf"] = (j <= i).astype(np.float32).astype(ml_dtypes.bfloat16)
    c["c_maskb"] = (j >= i).astype(np.float32).astype(ml_dtypes.bfloat16)
    c["c_iota"] = np.tile(np.arange(512, dtype=np.float32)[None, :], (128, 1))
    tok = (np.arange(32)[None, :] * 128 + np.arange(128)[:, None])
    c["c_tokv"] = np.stack([(tok // 64).astype(np.float32), (tok % 64).astype(np.float32)], axis=-1)
    return c


def make_in_maps(inp):
    consts = host_constants()
    f = lambda a: np.ascontiguousarray(np.asarray(a, dtype=np.float32))
    shared = {
        "w_ada": f(inp["w_ada"][0]), "b_ada": f(inp["b_ada"][0]).reshape(1, -1), "w_in": f(inp["w_in"][0]),
        "w_gate2": f(inp["w_gate2"][0]), "b_gate": np.ascontiguousarray(f(inp["b_gate"][0]).reshape(2, 4, 128).transpose(2, 0, 1)),
        "gla_norm_g": f(inp["gla_norm_g"][0]).reshape(1, -1), "diff_lambda": f(inp["diff_lambda"][0]).reshape(1, -1),
        "diff_norm_g": f(inp["diff_norm_g"][0]).reshape(1, -1), "w_o": f(inp["w_o"][0]),
        "ln1_g": f(inp["ln1_g"][0]).reshape(1, -1), "ln1_b": f(inp["ln1_b"][0]).reshape(1, -1),
        "w_router": f(inp["w_router"][0]), "w1": f(inp["w1"][0]), "w3": f(inp["w3"][0]), "w2": f(inp["w2"][0]),
        "ln2_g": f(inp["ln2_g"][0]).reshape(1, -1), "ln2_b": f(inp["ln2_b"][0]).reshape(1, -1),
    }
    shared.update(consts)
    maps = []
    for core in range(8):
        b = core % 4
        m = dict(shared)
        m["x"] = f(inp["x"][b])
        m["ctx"] = f(inp["ctx"][b])
        m["cvec"] = np.ascontiguousarray(np.stack([np.asarray(inp["c"][b], np.float32), np.asarray(inp["c_ctx"], np.float32)]))
        maps.append(m)
    return maps


def kernel(**inputs):
    nc = build_program()
    in_maps = make_in_maps(inputs)
    in_maps = [{kk: v for kk, v in m.items() if kk in nc.used_inputs} for m in in_maps]
    res = run_bass_kernel_spmd(nc, in_maps, core_ids=list(range(8)))
    outs = [np.asarray(res.results[b]["out"]) for b in range(4)]
    return np.stack(outs, axis=0).astype(np.float32)
```

```python
import math
from contextlib import ExitStack

import numpy as np
import concourse.bass as bass
import concourse.mybir as mybir
from concourse.bass_utils import run_bass_kernel_spmd

F32 = mybir.dt.float32
F32R = mybir.dt.float32r
BF16 = mybir.dt.bfloat16
I32 = mybir.dt.int32
AF = mybir.ActivationFunctionType
ALU = mybir.AluOpType
AX = mybir.AxisListType

D = 2048
NLAT = 4096
NCTX = 256
NTOK = NLAT + NCTX
DIN = 6176
NE = 16
CAP = 512
ALPHA = 2.0 ** 0.25
LAM_INIT = 0.2
NDSEM = 80

STOP_AFTER = "Z"
DEBUG = False
DEBUG_NAMES = ()
B_KINDS = None
ROPE_STAGE = 9
C_HEADS = 8
N_FILL = 0


class Buf:
    def __init__(self, t=None, waw=True):
        self.t = t
        self.w = {}
        self.r = {}
        self.waw = waw


class K:
    def __init__(self, nc, es):
        self.nc = nc
        self.es = es
        self.E = {"pe": nc.tensor, "act": nc.scalar, "dve": nc.vector, "pool": nc.gpsimd, "sp": nc.sync}
        self.sem = {e: es.enter_context(nc.semaphore("s_" + e)) for e in self.E}
        self.seq = {e: 0 for e in self.E}
        self.known = {e: {} for e in self.E}
        self.dsem = [es.enter_context(nc.semaphore("d%d" % i)) for i in range(NDSEM)]
        self.duse = [0] * NDSEM
        self.dcount = 0
        self.dcount_sw = 0
        self.scopes = []
        self.n = 0

    def push(self):
        self.scopes.append(ExitStack())

    def pop(self):
        self.barrier()
        self.scopes.pop().close()

    def sb(self, name, shape, dtype):
        st = self.scopes[-1] if self.scopes else self.es
        return Buf(st.enter_context(self.nc.sbuf_tensor(name, list(shape), dtype)))

    def ps(self, name, shape, dtype):
        st = self.scopes[-1] if self.scopes else self.es
        return Buf(st.enter_context(self.nc.psum_tensor(name, list(shape), dtype)))

    def _semobj(self, key):
        return self.sem[key[1]] if key[0] == "e" else self.dsem[key[1]]

    def _wait(self, eng, key, val):
        if self.known[eng].get(key, 0) >= val:
            return
        self.E[eng].wait_ge(self._semobj(key), val)
        self.known[eng][key] = val

    def _deps(self, eng, reads, writes):
        for b in reads:
            for kk, v in b.w.items():
                if eng == "pe" and kk == ("e", "pe"):
                    continue
                self._wait(eng, kk, v)
        for b in writes:
            if b.waw:
                for kk, v in b.w.items():
                    if eng == "pe" and kk == ("e", "pe"):
                        continue
                    self._wait(eng, kk, v)
            for kk, v in b.r.items():
                if eng == "pe" and kk == ("e", "pe"):
                    continue
                self._wait(eng, kk, v)

    def _commit(self, key, val, reads, writes):
        for b in reads:
            b.r[key] = max(b.r.get(key, 0), val)
        for b in writes:
            if b.waw:
                b.w = {key: val}
                b.r = {}
            else:
                b.w[key] = max(b.w.get(key, 0), val)

    def op(self, eng, fn, reads=(), writes=()):
        self._deps(eng, reads, writes)
        inst = fn(self.E[eng])
        self.seq[eng] += 1
        inst.then_inc(self.sem[eng], 1)
        self._commit(("e", eng), self.seq[eng], reads, writes)
        self.n += 1

    def _dslot(self, q):
        half = NDSEM // 2
        if q == "pool":
            i = half + self.dcount_sw % half
            self.dcount_sw += 1
        else:
            i = self.dcount % half
            self.dcount += 1
        if self.duse[i] > 0:
            self._wait(q, ("d", i), self.duse[i] * 16)
        return i

    def dma(self, q, out, in_, reads=(), writes=(), **kw):
        self._deps(q, reads, writes)
        i = self._dslot(q)
        inst = self.E[q].dma_start(out=out, in_=in_, **kw)
        self.duse[i] += 1
        inst.then_inc(self.dsem[i], 16)
        self._commit(("d", i), self.duse[i] * 16, reads, writes)
        self.n += 1

    def idma(self, fn, reads=(), writes=()):
        q = "pool"
        self._deps(q, reads, writes)
        i = self._dslot(q)
        inst = fn(self.E[q])
        self.duse[i] += 1
        inst.then_inc(self.dsem[i], 16)
        self._commit(("d", i), self.duse[i] * 16, reads, writes)
        self.n += 1

    def barrier(self, engines=None):
        for e in (engines or list(self.E)):
            for f in self.E:
                if self.seq[f] > 0 and not (e == f == "pe"):
                    self._wait(e, ("e", f), self.seq[f])
            for i in range(NDSEM):
                if self.duse[i] > 0:
                    self._wait(e, ("d", i), self.duse[i] * 16)


def build_program():
    nc = bass.Bass("TRN2", target_bir_lowering=False)

    used_inputs = []

    class Lazy:
        def __init__(self, name, shape, dt=F32):
            self.name, self.shape, self.dt, self._ap = name, list(shape), dt, None

        @property
        def ap(self):
            if self._ap is None:
                self._ap = nc.dram_tensor(self.name, self.shape, self.dt, kind="ExternalInput").ap()
                used_inputs.append(self.name)
            return self._ap

    def din(name, shape, dt=F32):
        return Lazy(name, shape, dt)

    def dscr(name, shape, dt=F32):
        kind = "ExternalOutput" if (DEBUG and name in DEBUG_NAMES) else "Internal"
        return nc.dram_tensor(name, list(shape), dt, kind=kind).ap()

    x = din("x", [NLAT, D])
    ctx = din("ctx", [NCTX, D])
    cvec = din("cvec", [2, D])
    w_ada = din("w_ada", [D, 6 * D])
    b_ada = din("b_ada", [1, 6 * D])
    w_in = din("w_in", [D, DIN])
    w_gate2 = din("w_gate2", [2, 16, 512])
    b_gate = din("b_gate", [128, 2, 4])
    gla_norm_g = din("gla_norm_g", [1, 256])
    diff_lambda = din("diff_lambda", [1, 256])
    diff_norm_g = din("diff_norm_g", [1, 128])
    w_o = din("w_o", [D, D])
    ln1_g = din("ln1_g", [1, D])
    ln1_b = din("ln1_b", [1, D])
    w_router = din("w_router", [D, NE])
    w1 = din("w1", [NE, D, D])
    w3 = din("w3", [NE, D, D])
    w2 = din("w2", [NE, D, D])
    ln2_g = din("ln2_g", [1, D])
    ln2_b = din("ln2_b", [1, D])
    c_identf = din("c_identf", [128, 128])
    c_identb = din("c_identb", [128, 128], BF16)
    c_perm = din("c_perm", [128, 128], BF16)
    c_ropec = din("c_ropec", [128, NLAT])
    c_ropes = din("c_ropes", [128, NLAT])
    c_reset = din("c_reset", [128, 512])
    c_maskf = din("c_maskf", [128, 128], BF16)
    c_maskb = din("c_maskb", [128, 128], BF16)
    c_iota = din("c_iota", [128, 512])
    c_tokv = din("c_tokv", [128, 32, 2])
    out = nc.dram_tensor("out", [NLAT, D], F32, kind="ExternalOutput").ap()
    nc.used_inputs = used_inputs

    MOD = dscr("MOD", [2, 6 * D])
    AT = dscr("AT", [2, 4, 128, NTOK], BF16)
    BT = dscr("BT", [2, 4, 128, NTOK], BF16)
    ETOT = dscr("ETOT", [2, 128, 4, 34])
    VG = dscr("VG", [NTOK, 1024], BF16)
    RG = dscr("RG", [NLAT, 1024])
    QDT = dscr("QDT", [8, 128, NLAT], BF16)
    KDT = dscr("KDT", [8, 128, NTOK], BF16)
    VD = dscr("VD", [NTOK, 1024], BF16)
    OFS = dscr("OFS", [NLAT, 1024])
    AL = dscr("AL", [NLAT, D])
    X1 = dscr("X1", [NLAT, D])
    H = dscr("H", [NLAT, D])
    AFF = dscr("AFF", [NLAT, NE])
    FACC = dscr("FACC", [NLAT, D])
    ALT = dscr("ALT", [1024, NLAT])
    IDXD = dscr("IDXD", [128, NE, 4], I32)
    GATED = dscr("GATED", [128, NE, 4])
    bMOD, bAT, bBT, bETOT, bVG, bRG, bQDT, bKDT, bVD, bOFS, bAL, bX1, bH, bAFF, bFACC, bOUT = [
        Buf(None, waw=False) for _ in range(16)]
    bALT = Buf(None, waw=False)
    bFACCs = Buf(None, waw=True)

    with ExitStack() as es:
        k = K(nc, es)
        PS = [k.ps("psb%d" % i, [128, 512], F32) for i in range(8)]
        identf = k.sb("identf", [128, 128], F32)
        identb = k.sb("identb", [128, 128], BF16)
        k.dma("sp", identf.t[:], c_identf.ap, writes=[identf])
        k.dma("sp", identb.t[:], c_identb.ap, writes=[identb])
        modT = k.sb("modT", [128, 2, 96], F32)

        k.push()
        cT = k.sb("cT", [128, 16, 128], F32)
        k.op("pool", lambda e: e.memset(cT.t[:], 0.0), writes=[cT])
        with nc.allow_non_contiguous_dma(reason="tiny transposed vector load"):
            for r in range(2):
                k.dma("sp", cT.t[:, :, r], cvec.ap[r, :].rearrange("(c p) -> p c", p=128), writes=[cT])
        scT = k.sb("scT", [128, 16, 128], F32R)
        k.op("act", lambda e: e.activation(out=scT.t[:], in_=cT.t[:], func=AF.Silu), reads=[cT], writes=[scT])
        bada2 = k.sb("bada2", [2, 6 * D], F32)
        k.dma("sp", bada2.t[:], b_ada.ap.partition_broadcast(2), writes=[bada2])
        mod_sb = k.sb("mod_sb", [2, 6 * D], F32)
        slabs = [k.sb("aslab%d" % i, [128, 16, 512], F32R) for i in range(2)]
        for blk in range(24):
            slab = slabs[blk % 2]
            k.dma("pool", slab.t[:], w_ada.ap[:, blk * 512:(blk + 1) * 512].rearrange("(c p) f -> p c f", p=128),
                  writes=[slab])
            ps = PS[blk % 2]
            for c in range(16):
                k.op("pe", lambda e: e.matmul(ps.t[:], scT.t[:, c, :], slab.t[:, c, :], start=(c == 0), stop=(c == 15)),
                     reads=[scT, slab], writes=[ps])
            k.op("dve", lambda e: e.tensor_tensor(out=mod_sb.t[:, blk * 512:(blk + 1) * 512], in0=ps.t[0:2, :],
                                                  in1=bada2.t[:, blk * 512:(blk + 1) * 512], op=ALU.add),
                 reads=[ps, bada2], writes=[mod_sb])
        k.dma("sp", MOD, mod_sb.t[:], reads=[mod_sb], writes=[bMOD])
        with nc.allow_non_contiguous_dma(reason="small transposed reload of modulation vectors"):
            k.dma("sp", modT.t[:], MOD.rearrange("r (j p) -> p r j", p=128), reads=[bMOD], writes=[modT])
        for j0 in (16, 64):
            k.op("dve", lambda e: e.tensor_scalar(out=modT.t[:, :, j0:j0 + 16], in0=modT.t[:, :, j0:j0 + 16],
                                                  scalar1=1.0, scalar2=None, op0=ALU.add),
                 reads=[modT], writes=[modT])
        k.pop()
        if STOP_AFTER == "A":
            return finish(nc, k, out)


        k.push()
        Cblk = k.sb("Cblk", [128, 512], F32)
        Sblk = k.sb("Sblk", [128, 512], F32)
        qraw = [k.sb("qraw%d" % i, [128, 512], BF16) for i in range(2)]
        uT = k.sb("uT", [128, 16, 512], F32R)
        xt = [k.sb("xt%d" % i, [128, D], F32) for i in range(2)]
        slabs = [k.sb("bslab%d" % i, [128, 16, 512], F32R) for i in range(2)]
        wg2 = k.sb("wg2", [48, 512], F32)
        k.dma("sp", wg2.t[0:16, :], w_gate2.ap[0], writes=[wg2])
        k.dma("sp", wg2.t[32:48, :], w_gate2.ap[1], writes=[wg2])
        negbg = k.sb("negbg", [128, 2, 4], F32)
        k.dma("sp", negbg.t[:], b_gate.ap, writes=[negbg])
        k.op("dve", lambda e: e.tensor_scalar(out=negbg.t[:], in0=negbg.t[:], scalar1=-1.0, scalar2=None, op0=ALU.mult),
             reads=[negbg], writes=[negbg])
        perm = k.sb("perm", [128, 128], BF16)
        k.dma("sp", perm.t[:], c_perm.ap, writes=[perm])
        resetm = k.sb("resetm", [128, 512], F32)
        k.dma("sp", resetm.t[:], c_reset.ap, writes=[resetm])
        lrT = k.sb("lrT", [48, 512], F32)
        zf = xt[0]
        k.op("pool", lambda e: e.memset(zf.t[:], 0.0), writes=[zf])
        lrslab = k.sb("lrslab", [128, 16, 128], F32R)
        k.op("act", lambda e: e.activation(out=lrslab.t[:].rearrange("p c f -> p (c f)"), in_=zf.t[:], func=AF.Copy),
             reads=[zf], writes=[lrslab])
        with nc.allow_non_contiguous_dma(reason="small low-rank gate columns"):
            for d in range(2):
                k.dma("pool", lrslab.t[:, :, d * 32:d * 32 + 16],
                      w_in.ap[:, 3072 + d * 16:3072 + d * 16 + 16].rearrange("(c p) f -> p c f", p=128), writes=[lrslab])
        EA = k.sb("EA", [128, 2, 4, 512], BF16)
        EB = k.sb("EB", [128, 2, 4, 512], BF16)
        ABst = k.sb("ABst", [128, 2, 2, 4, 512], BF16)
        etst = k.sb("etst", [128, 2, 4, 4], F32)
        tmpE = [k.sb("tmpE%d" % i, [128, 512], F32) for i in range(2)]
        spt = [k.sb("spt%d" % i, [128, 512], F32) for i in range(2)]
        cumt = [k.sb("cumt%d" % i, [128, 512], F32) for i in range(2)]
        argt = [k.sb("argt%d" % i, [128, 512], F32) for i in range(2)]
        Vst = [k.sb("Vst%d" % i, [128, 4, 512], BF16) for i in range(2)]
        Rst = [k.sb("Rst0", [128, 4, 512], F32)] * 2
        QKst = [k.sb("QKst%d" % i, [128, 4, 512], BF16) for i in range(2)]
        t1 = tmpE
        t2 = spt
        SLABS = [("glr", 3072, 32), ("gq", 0, 512), ("gk", 512, 512), ("gv", 1024, 512), ("gv", 1536, 512),
                 ("gr", 2048, 512), ("gr", 2560, 512), ("dq", 3104, 512), ("dq", 3616, 512),
                 ("dk", 4128, 512), ("dk", 4640, 512), ("dv", 5152, 512), ("dv", 5664, 512)]
        cnt = {"x": 0, "slab": 0, "acc": 0, "g": 0, "st": 0, "r": 0}
        NBLK = 9 if STOP_AFTER != "B1" else 2
        for tb in range(NBLK):
            isctx = tb == 0
            NT = 256 if isctx else 512
            tok0 = 0 if isctx else 256 + (tb - 1) * 512
            lat0 = 0 if isctx else (tb - 1) * 512
            mr = 1 if isctx else 0
            src = ctx.ap if isctx else x.ap[lat0:lat0 + 512, :]
            nt = NT // 128
            for t in range(nt):
                xb = xt[cnt["x"] % 2]
                cnt["x"] += 1
                k.dma("sp", xb.t[:], src[t * 128:(t + 1) * 128, :], writes=[xb])
                for c in range(16):
                    pb = PS[c // 4]
                    k.op("pe", lambda e: e.transpose(pb.t[:, (c % 4) * 128:(c % 4 + 1) * 128], xb.t[:, c * 128:(c + 1) * 128],
                                                     identf.t[:]), reads=[xb, identf], writes=[pb])
                for c in range(16):
                    pb = PS[c // 4]
                    k.op("act", lambda e: e.activation(out=uT.t[:, c, t * 128:(t + 1) * 128],
                                                       in_=pb.t[:, (c % 4) * 128:(c % 4 + 1) * 128], func=AF.Identity,
                                                       scale=modT.t[:, mr, 16 + c:17 + c], bias=modT.t[:, mr, c:c + 1]),
                         reads=[pb, modT], writes=[uT])
            if not isctx:
                k.dma("sp", Cblk.t[:], c_ropec.ap[:, lat0:lat0 + 512], writes=[Cblk])
                k.dma("sp", Sblk.t[:], c_ropes.ap[:, lat0:lat0 + 512], writes=[Sblk])
            half = {}
            for (kind, c0, ncols) in SLABS:
                hf = half.get(kind, 0)
                half[kind] = hf + 1
                if isctx and kind in ("gr", "dq"):
                    continue
                if B_KINDS is not None and kind not in B_KINDS:
                    continue
                if kind != "glr":
                    slab = slabs[cnt["slab"] % 2]
                    cnt["slab"] += 1
                if kind == "glr":
                    slab = lrslab
                else:
                    k.dma("pool", slab.t[:], w_in.ap[:, c0:c0 + 512].rearrange("(c p) f -> p c f", p=128), writes=[slab])

                def acc_feat(j):
                    pa = PS[4 + cnt["acc"] % 2]
                    cnt["acc"] += 1
                    for c in range(16):
                        k.op("pe", lambda e: e.matmul(pa.t[:, :NT], slab.t[:, c, j * 128:(j + 1) * 128], uT.t[:, c, :NT],
                                                      start=(c == 0), stop=(c == 15)), reads=[slab, uT], writes=[pa])
                    return pa

                def acc_tok(t):
                    pa = PS[4 + cnt["acc"] % 2]
                    cnt["acc"] += 1
                    for c in range(16):
                        k.op("pe", lambda e: e.matmul(pa.t[:], uT.t[:, c, t * 128:(t + 1) * 128], slab.t[:, c, :],
                                                      start=(c == 0), stop=(c == 15)), reads=[slab, uT], writes=[pa])
                    return pa

                if kind == "glr":
                    pa = acc_feat(0)
                    k.op("act", lambda e: e.activation(out=lrT.t[:, :NT], in_=pa.t[0:48, :NT], func=AF.Copy),
                         reads=[pa], writes=[lrT])
                    nch = NT // 128
                    for d in range(2):
                        for j in range(4):
                            g = cnt["g"] % 2
                            cnt["g"] += 1
                            pz = PS[6 + g]
                            k.op("pe", lambda e: e.matmul(pz.t[:, :NT], wg2.t[d * 32:d * 32 + 16, j * 128:(j + 1) * 128],
                                                          lrT.t[d * 32:d * 32 + 16, :NT], start=True, stop=True),
                                 reads=[wg2, lrT], writes=[pz])
                            k.op("act", lambda e: e.activation(out=tmpE[g].t[:, :NT], in_=pz.t[:, :NT], func=AF.Exp,
                                                               scale=-1.0, bias=negbg.t[:, d, j:j + 1]),
                                 reads=[pz, negbg], writes=[tmpE[g]])
                            k.op("act", lambda e: e.activation(out=spt[g].t[:, :NT], in_=tmpE[g].t[:, :NT], func=AF.Ln,
                                                               scale=1.0, bias=1.0), reads=[tmpE[g]], writes=[spt[g]])
                            k.op("dve", lambda e: e.tensor_tensor_scan(out=cumt[g].t[:, :NT], data0=resetm.t[:, :NT],
                                                                       data1=spt[g].t[:, :NT], initial=0.0, op0=ALU.mult,
                                                                       op1=ALU.add), reads=[resetm, spt[g]], writes=[cumt[g]])
                            if d == 0:
                                arg = cumt[g]
                            else:
                                arg = argt[g]
                                k.op("dve", lambda e: e.tensor_tensor(out=arg.t[:, :NT], in0=cumt[g].t[:, :NT],
                                                                      in1=spt[g].t[:, :NT], op=ALU.subtract),
                                     reads=[cumt[g], spt[g]], writes=[arg])
                            sa = -1.0 / 16 if d == 0 else 1.0 / 16
                            k.op("act", lambda e: e.activation(out=EA.t[:, d, j, :NT], in_=arg.t[:, :NT], func=AF.Exp, scale=sa),
                                 reads=[arg], writes=[EA])
                            k.op("act", lambda e: e.activation(out=EB.t[:, d, j, :NT], in_=arg.t[:, :NT], func=AF.Exp, scale=-sa),
                                 reads=[arg], writes=[EB])
                            k.op("act", lambda e: e.activation(out=etst.t[:, d, j, 0:nch], in_=cumt[g].t[:, 127:NT:128],
                                                               func=AF.Exp, scale=-1.0 / 16), reads=[cumt[g]], writes=[etst])
                    ch0 = tok0 // 128
                    with nc.allow_non_contiguous_dma(reason="tiny per-chunk decay totals"):
                        for d in range(2):
                            k.dma("sp", ETOT[d, :, :, ch0:ch0 + nch], etst.t[:, d, :, 0:nch], reads=[etst], writes=[bETOT])
                elif kind in ("gq", "gk"):
                    isq = kind == "gq"
                    for j in range(4):
                        pa = acc_feat(j)
                        for d in range(2):
                            if isq:
                                k.op("dve", lambda e: e.scalar_tensor_tensor(out=ABst.t[:, 0, d, j, :NT], in0=pa.t[:, :NT],
                                                                             scalar=128.0 ** -0.5, in1=EA.t[:, d, j, :NT],
                                                                             op0=ALU.mult, op1=ALU.mult),
                                     reads=[pa, EA], writes=[ABst])
                            else:
                                k.op("dve", lambda e: e.tensor_tensor(out=ABst.t[:, 1, d, j, :NT], in0=pa.t[:, :NT],
                                                                      in1=EB.t[:, d, j, :NT], op=ALU.mult),
                                     reads=[pa, EB], writes=[ABst])
                    dst = AT if isq else BT
                    k.dma("sp", dst.rearrange("d j p n -> p d j n")[:, :, :, tok0:tok0 + NT],
                          ABst.t[:, 0 if isq else 1, :, :, :NT], reads=[ABst], writes=[bAT if isq else bBT])
                elif kind in ("gv", "dv", "gr"):
                    if kind == "gr":
                        st = Rst[cnt["r"] % 2]
                        cnt["r"] += 1
                    else:
                        st = Vst[cnt["st"] % 2]
                        cnt["st"] += 1
                    for t in range(nt):
                        pa = acc_tok(t)
                        fn = AF.Silu if kind == "gr" else AF.Copy
                        k.op("act", lambda e: e.activation(out=st.t[:, t, :], in_=pa.t[:], func=fn), reads=[pa], writes=[st])
                    if kind == "gr":
                        k.dma("sp", RG[lat0:lat0 + 512, hf * 512:(hf + 1) * 512].rearrange("(t p) f -> p t f", p=128),
                              st.t[:, 0:nt, :], reads=[st], writes=[bRG])
                    else:
                        dst, bd = (VG, bVG) if kind == "gv" else (VD, bVD)
                        k.dma("sp", dst[tok0:tok0 + NT, hf * 512:(hf + 1) * 512].rearrange("(t p) f -> p t f", p=128),
                              st.t[:, 0:nt, :], reads=[st], writes=[bd])
                elif kind in ("dq", "dk"):
                    st = QKst[cnt["st"] % 2]
                    cnt["st"] += 1
                    sc = 0.125 if kind == "dq" else 1.0
                    for jj in range(4):
                        pa = acc_feat(jj)
                        if isctx:
                            k.op("act", lambda e: e.activation(out=st.t[:, jj, :NT], in_=pa.t[:, :NT], func=AF.Copy),
                                 reads=[pa], writes=[st])
                            continue
                        g = cnt["g"] % 2
                        cnt["g"] += 1
                        k.op("act", lambda e: e.activation(out=qraw[g].t[:], in_=pa.t[:], func=AF.Copy), reads=[pa], writes=[qraw[g]])
                        pz = PS[6 + g]
                        if ROPE_STAGE >= 1:
                            k.op("pe", lambda e: e.matmul(pz.t[:], perm.t[:], qraw[g].t[:], start=True, stop=True),
                                 reads=[perm, qraw[g]], writes=[pz])
                        qf, pzf = argt[g], cumt[g]
                        if ROPE_STAGE >= 2:
                            k.op("act", lambda e: e.activation(out=qf.t[:], in_=pa.t[:], func=AF.Copy), reads=[pa], writes=[qf])
                            k.op("dve", lambda e: e.tensor_tensor(out=t1[g].t[:], in0=qf.t[:], in1=Cblk.t[:], op=ALU.mult),
                                 reads=[qf, Cblk], writes=[t1[g]])
                        if ROPE_STAGE >= 3:
                            k.op("act", lambda e: e.activation(out=pzf.t[:], in_=pz.t[:], func=AF.Copy), reads=[pz], writes=[pzf])
                            k.op("dve", lambda e: e.tensor_tensor(out=t2[g].t[:], in0=pzf.t[:], in1=Sblk.t[:], op=ALU.mult),
                                 reads=[pzf, Sblk], writes=[t2[g]])
                        if ROPE_STAGE >= 4:
                            k.op("dve", lambda e: e.tensor_tensor(out=t1[g].t[:], in0=t1[g].t[:], in1=t2[g].t[:], op=ALU.add),
                                 reads=[t1[g], t2[g]], writes=[t1[g]])
                            k.op("act", lambda e: e.activation(out=st.t[:, jj, :], in_=t1[g].t[:], func=AF.Copy, scale=sc),
                                 reads=[t1[g]], writes=[st])
                        else:
                            k.op("act", lambda e: e.activation(out=st.t[:, jj, :], in_=pa.t[:], func=AF.Copy), reads=[pa], writes=[st])
                    if kind == "dq":
                        k.dma("sp", QDT[hf * 4:hf * 4 + 4, :, lat0:lat0 + 512].rearrange("h p n -> p h n"), st.t[:],
                              reads=[st], writes=[bQDT])
                    else:
                        k.dma("sp", KDT[hf * 4:hf * 4 + 4, :, tok0:tok0 + NT].rearrange("h p n -> p h n"), st.t[:, :, :NT],
                              reads=[st], writes=[bKDT])
        k.pop()
        if STOP_AFTER in ("B", "B1"):
            return finish(nc, k, out)


        k.push()
        KT = k.sb("KT", [128, NTOK], BF16)
        Vh = k.sb("Vh", [128, 34, 128], BF16)
        onesb = k.sb("onesb", [128, 128], BF16)
        k.op("pool", lambda e: e.memset(onesb.t[:], 1.0), writes=[onesb])
        onesf = k.sb("onesf", [128, 128], F32)
        k.op("pool", lambda e: e.memset(onesf.t[:], 1.0), writes=[onesf])
        Pacc = [k.sb("Pacc%d" % i, [128, 512], F32) for i in range(2)]
        QT = [[k.sb("QT%d_%d" % (i, m), [128, 512], BF16) for m in range(2)] for i in range(2)]
        for i in range(2):
            for m in range(2):
                k.op("pool", lambda e: e.memset(QT[i][m].t[:], 0.0), writes=[QT[i][m]])
        PT = [k.sb("PT%d" % i, [128, 512], BF16) for i in range(3)]
        On = [k.sb("On%d" % i, [128, 512], F32) for i in range(2)]
        Rt = k.sb("Rt", [128, 512], F32)
        dd = k.sb("dd", [128, 512], F32)
        sqb = k.sb("sqb", [128, 512], BF16)
        rs = k.sb("rs", [128, 512], F32)
        ost = [k.sb("ost%d" % i, [128, 512], F32) for i in range(2)]
        dl = k.sb("dl", [128, 256], F32)
        k.dma("sp", dl.t[:], diff_lambda.ap.partition_broadcast(128), writes=[dl])
        prod = k.sb("prod", [128, 2, 64], F32)
        for i in range(2):
            k.op("dve", lambda e: e.tensor_tensor(out=prod.t[:, i, :], in0=dl.t[:, i * 128:i * 128 + 64],
                                                  in1=dl.t[:, i * 128 + 64:i * 128 + 128], op=ALU.mult), reads=[dl], writes=[prod])
        sums = k.sb("sums", [128, 2], F32)
        k.op("dve", lambda e: e.tensor_reduce(out=sums.t[:], in_=prod.t[:], axis=AX.X, op=ALU.add), reads=[prod], writes=[sums])
        exl = k.sb("exl", [128, 2], F32)
        k.op("act", lambda e: e.activation(out=exl.t[:], in_=sums.t[:], func=AF.Exp), reads=[sums], writes=[exl])
        neglam = k.sb("neglam", [128, 1], F32)
        k.op("dve", lambda e: e.tensor_tensor(out=neglam.t[:], in0=exl.t[:, 1:2], in1=exl.t[:, 0:1], op=ALU.subtract),
             reads=[exl], writes=[neglam])
        k.op("dve", lambda e: e.tensor_scalar(out=neglam.t[:], in0=neglam.t[:], scalar1=-LAM_INIT, scalar2=None, op0=ALU.add),
             reads=[neglam], writes=[neglam])
        g8c = k.sb("g8c", [128, 1], F32)
        with nc.allow_non_contiguous_dma(reason="tiny per-partition gain vector"):
            k.dma("sp", g8c.t[:], diff_norm_g.ap.rearrange("o (p u) -> (o p) u", u=1), writes=[g8c])
        k.op("dve", lambda e: e.tensor_scalar(out=g8c.t[:], in0=g8c.t[:], scalar1=1.0 - LAM_INIT, scalar2=None, op0=ALU.mult),
             reads=[g8c], writes=[g8c])
        cq = 0
        cp = 0
        cm = 0
        NH = C_HEADS if STOP_AFTER != "C1" else 1
        for h in range(NH):
            k.dma("sp", KT.t[:], KDT[h], reads=[bKDT], writes=[KT])
            k.dma("sp", Vh.t[:], VD[:, h * 128:(h + 1) * 128].rearrange("(t p) f -> p t f", p=128), reads=[bVD], writes=[Vh])
            for qb in range(8):
                QTb = QT[cq % 2]
                osb = ost[cq % 2]
                cq += 1
                for m in range(2):
                    k.dma("sp", QTb[m].t[64 * m:64 * m + 64, :], QDT[h, 64 * m:64 * m + 64, qb * 512:(qb + 1) * 512],
                          reads=[bQDT], writes=[QTb[m]])
                steps = [(m, kt) for m in range(2) for kt in range(34)]

                def emit_qk(i):
                    m, kt = steps[i]
                    pS = PS[i % 3]
                    k.op("pe", lambda e: e.matmul(pS.t[:], KT.t[:, kt * 128:(kt + 1) * 128], QTb[m].t[:], start=True, stop=True),
                         reads=[KT, QTb[m]], writes=[pS])

                emit_qk(0)
                emit_qk(1)
                for i, (m, kt) in enumerate(steps):
                    if i + 2 < len(steps):
                        emit_qk(i + 2)
                    if kt == 0:
                        pO = PS[3 + 2 * (cm % 2)]
                        pL = PS[4 + 2 * (cm % 2)]
                        cm += 1
                    pS = PS[i % 3]
                    PTb = PT[cp % 3]
                    cp += 1
                    for _f in range(N_FILL):
                        k.op("pe", lambda e: e.matmul(PS[7].t[:], onesb.t[:], KT.t[:, 0:512], start=True, stop=True),
                             reads=[onesb, KT], writes=[PS[7]])
                    k.op("act", lambda e: e.activation(out=PTb.t[:], in_=pS.t[:], func=AF.Exp), reads=[pS], writes=[PTb])
                    k.op("pe", lambda e: e.matmul(pO.t[:], Vh.t[:, kt, :], PTb.t[:], start=(kt == 0), stop=(kt == 33)),
                         reads=[PTb, Vh], writes=[pO])
                    if kt == 0:
                        k.op("dve", lambda e: e.tensor_copy(out=Pacc[m].t[:], in_=PTb.t[:]), reads=[PTb], writes=[Pacc[m]])
                    else:
                        k.op("dve", lambda e: e.tensor_tensor(out=Pacc[m].t[:], in0=Pacc[m].t[:], in1=PTb.t[:], op=ALU.add),
                             reads=[Pacc[m], PTb], writes=[Pacc[m]])
                    if kt == 33:
                        k.op("pe", lambda e: e.matmul(pL.t[:], onesf.t[:], Pacc[m].t[:], start=True, stop=True),
                             reads=[Pacc[m], onesf], writes=[pL])
                        k.op("act", lambda e: e.activation(out=On[m].t[:], in_=pO.t[:], func=AF.Copy), reads=[pO], writes=[On[m]])
                        k.op("dve", lambda e: e.reciprocal(out=Rt.t[:], in_=pL.t[:]), reads=[pL], writes=[Rt])
                        k.op("dve", lambda e: e.tensor_tensor(out=On[m].t[:], in0=On[m].t[:], in1=Rt.t[:], op=ALU.mult),
                             reads=[On[m], Rt], writes=[On[m]])
                k.op("dve", lambda e: e.scalar_tensor_tensor(out=dd.t[:], in0=On[1].t[:], scalar=neglam.t[:, 0:1], in1=On[0].t[:],
                                                             op0=ALU.mult, op1=ALU.add), reads=[On[0], On[1], neglam], writes=[dd])
                k.op("dve", lambda e: e.tensor_tensor(out=sqb.t[:], in0=dd.t[:], in1=dd.t[:], op=ALU.mult), reads=[dd], writes=[sqb])
                pR = PS[7]
                k.op("pe", lambda e: e.matmul(pR.t[:], onesb.t[:], sqb.t[:], start=True, stop=True), reads=[onesb, sqb], writes=[pR])
                k.op("dve", lambda e: e.tensor_scalar(out=rs.t[:], in0=pR.t[:], scalar1=1.0 / 128, scalar2=1e-6, op0=ALU.mult,
                                                      op1=ALU.add), reads=[pR], writes=[rs])
                k.op("act", lambda e: e.activation(out=rs.t[:], in_=rs.t[:], func=AF.Ln), reads=[rs], writes=[rs])
                k.op("act", lambda e: e.activation(out=rs.t[:], in_=rs.t[:], func=AF.Exp, scale=-0.5), reads=[rs], writes=[rs])
                k.op("dve", lambda e: e.scalar_tensor_tensor(out=osb.t[:], in0=dd.t[:], scalar=g8c.t[:, 0:1], in1=rs.t[:],
                                                             op0=ALU.mult, op1=ALU.mult), reads=[dd, g8c, rs], writes=[osb])
                k.dma("sp", ALT[h * 128:(h + 1) * 128, qb * 512:(qb + 1) * 512], osb.t[:], reads=[osb], writes=[bALT])
        k.pop()
        if STOP_AFTER in ("C", "C1"):
            return finish(nc, k, out)

        k.push()
        etot = k.sb("etot", [128, 2, 4, 34], F32)
        for d in range(2):
            k.dma("sp", etot.t[:, d], ETOT[d], reads=[bETOT], writes=[etot])
        masks = [k.sb("mask%d" % d, [128, 128], BF16) for d in range(2)]
        k.dma("sp", masks[0].t[:], c_maskf.ap, writes=[masks[0]])
        k.dma("sp", masks[1].t[:], c_maskb.ap, writes=[masks[1]])
        gn = k.sb("gn", [128, 256], F32)
        k.dma("sp", gn.t[:], gla_norm_g.ap.partition_broadcast(128), writes=[gn])
        S32 = [k.sb("S32_%d" % j, [128, 256], F32) for j in range(4)]
        Sb = [k.sb("Sb_%d" % j, [128, 256], BF16) for j in range(4)]
        Ab = [k.sb("Ab%d" % i, [128, 4, 128], BF16) for i in range(2)]
        Bb = [k.sb("Bb%d" % i, [128, 4, 128], BF16) for i in range(2)]
        Vg = [k.sb("Vg%d" % i, [128, 1024], BF16) for i in range(2)]
        OFb = [k.sb("OFb%d" % i, [128, 1024], F32) for i in range(2)]
        Rb = [k.sb("Rb%d" % i, [128, 1024], F32) for i in range(2)]
        OFst = [k.sb("OFst%d" % i, [128, 1024], F32) for i in range(2)]
        gl = [k.sb("gl%d" % i, [128, 1024], F32) for i in range(2)]
        Btok = [k.sb("Btok%d" % i, [128, 128], BF16) for i in range(2)]
        attT = [k.sb("attT%d" % i, [128, 128], BF16) for i in range(2)]
        ste = [k.sb("ste%d" % i, [128, 256], F32) for i in range(2)]
        osb2 = [k.sb("osb2_%d" % i, [128, 256], F32) for i in range(2)]
        junk2 = k.sb("junk2", [128, 256], F32)
        ssq2 = k.sb("ssq2", [128, 4], F32)
        rstd2 = k.sb("rstd2", [128, 4], F32)
        cs = 0
        cx = 0
        for d in range(2):
            for j in range(4):
                k.op("pool", lambda e: e.memset(S32[j].t[:], 0.0), writes=[S32[j]])
                k.op("pool", lambda e: e.memset(Sb[j].t[:], 0.0), writes=[Sb[j]])
            order = list(range(34)) if d == 0 else [1, 0] + list(range(33, 1, -1))
            if STOP_AFTER == "D1":
                order = order[:6]
            for ci in order:
                isctx = ci < 2
                tok0 = ci * 128
                lat0 = tok0 - 256
                b = cs % 2
                cs += 1
                k.dma("sp", Ab[b].t[:], AT[d, :, :, tok0:tok0 + 128].rearrange("j p n -> p j n"), reads=[bAT], writes=[Ab[b]])
                k.dma("sp", Bb[b].t[:], BT[d, :, :, tok0:tok0 + 128].rearrange("j p n -> p j n"), reads=[bBT], writes=[Bb[b]])
                k.dma("sp", Vg[b].t[:], VG[tok0:tok0 + 128, :], reads=[bVG], writes=[Vg[b]])
                if d == 1 and not isctx:
                    k.dma("sp", OFb[b].t[:], OFS[lat0:lat0 + 128, :], reads=[bOFS], writes=[OFb[b]])
                    k.dma("sp", Rb[b].t[:], RG[lat0:lat0 + 128, :], reads=[bRG], writes=[Rb[b]])
                for j in range(4):
                    x2 = cx % 2
                    cx += 1
                    et = etot.t[:, d, j, ci:ci + 1]
                    pT = PS[x2]
                    k.op("pe", lambda e: e.transpose(pT.t[:].bitcast(BF16)[:, 0:128], Bb[b].t[:, j, :], identb.t[:]),
                         reads=[Bb[b], identb], writes=[pT])
                    k.op("act", lambda e: e.activation(out=Btok[x2].t[:], in_=pT.t[:].bitcast(BF16)[:, 0:128], func=AF.Copy),
                         reads=[pT], writes=[Btok[x2]])
                    if d == 1:
                        k.op("dve", lambda e: e.tensor_scalar(out=S32[j].t[:], in0=S32[j].t[:], scalar1=et, scalar2=None,
                                                              op0=ALU.mult), reads=[S32[j], etot], writes=[S32[j]])
                        k.op("act", lambda e: e.activation(out=Sb[j].t[:], in_=S32[j].t[:], func=AF.Copy),
                             reads=[S32[j]], writes=[Sb[j]])
                    if not isctx:
                        pA = PS[2 + x2]
                        k.op("pe", lambda e: e.matmul(pA.t[:, 0:128], Bb[b].t[:, j, :], Ab[b].t[:, j, :], start=True, stop=True),
                             reads=[Ab[b], Bb[b]], writes=[pA])
                        k.op("dve", lambda e: e.tensor_tensor(out=attT[x2].t[:], in0=pA.t[:, 0:128], in1=masks[d].t[:], op=ALU.mult),
                             reads=[pA, masks[d]], writes=[attT[x2]])
                        pO = PS[4 + x2]
                        k.op("pe", lambda e: e.matmul(pO.t[:, 0:256], attT[x2].t[:], Vg[b].t[:, j * 256:(j + 1) * 256],
                                                      start=True, stop=False), reads=[attT[x2], Vg[b]], writes=[pO])
                        k.op("pe", lambda e: e.matmul(pO.t[:, 0:256], Ab[b].t[:, j, :], Sb[j].t[:], start=False, stop=True),
                             reads=[Ab[b], Sb[j]], writes=[pO])
                        if d == 0:
                            k.op("act", lambda e: e.activation(out=OFst[b].t[:, j * 256:(j + 1) * 256], in_=pO.t[:, 0:256],
                                                               func=AF.Copy), reads=[pO], writes=[OFst[b]])
                        else:
                            k.op("act", lambda e: e.activation(out=osb2[x2].t[:], in_=pO.t[:, 0:256], func=AF.Copy),
                                 reads=[pO], writes=[osb2[x2]])
                            k.op("dve", lambda e: e.tensor_tensor(out=gl[b].t[:, j * 256:(j + 1) * 256], in0=osb2[x2].t[:],
                                                                  in1=OFb[b].t[:, j * 256:(j + 1) * 256], op=ALU.add),
                                 reads=[osb2[x2], OFb[b]], writes=[gl[b]])
                            k.op("act", lambda e: e.activation(out=junk2.t[:], in_=gl[b].t[:, j * 256:(j + 1) * 256], func=AF.Square,
                                                               accum_out=ssq2.t[:, j:j + 1]), reads=[gl[b]], writes=[junk2, ssq2])
                    pS2 = PS[6 + x2]
                    k.op("pe", lambda e: e.matmul(pS2.t[:, 0:256], Btok[x2].t[:], Vg[b].t[:, j * 256:(j + 1) * 256],
                                                  start=True, stop=True), reads=[Btok[x2], Vg[b]], writes=[pS2])
                    if d == 0:
                        k.op("act", lambda e: e.activation(out=ste[x2].t[:], in_=pS2.t[:, 0:256], func=AF.Copy, scale=et),
                             reads=[pS2, etot], writes=[ste[x2]])
                        k.op("dve", lambda e: e.scalar_tensor_tensor(out=S32[j].t[:], in0=S32[j].t[:], scalar=et, in1=ste[x2].t[:],
                                                                     op0=ALU.mult, op1=ALU.add),
                             reads=[S32[j], ste[x2], etot], writes=[S32[j]])
                        k.op("act", lambda e: e.activation(out=Sb[j].t[:], in_=S32[j].t[:], func=AF.Copy),
                             reads=[S32[j]], writes=[Sb[j]])
                    else:
                        k.op("act", lambda e: e.activation(out=ste[x2].t[:], in_=pS2.t[:, 0:256], func=AF.Copy),
                             reads=[pS2], writes=[ste[x2]])
                        k.op("dve", lambda e: e.tensor_tensor(out=S32[j].t[:], in0=S32[j].t[:], in1=ste[x2].t[:], op=ALU.add),
                             reads=[S32[j], ste[x2]], writes=[S32[j]])
                if not isctx:
                    if d == 0:
                        k.dma("sp", OFS[lat0:lat0 + 128, :], OFst[b].t[:], reads=[OFst[b]], writes=[bOFS])
                    else:
                        k.op("dve", lambda e: e.tensor_scalar(out=rstd2.t[:], in0=ssq2.t[:], scalar1=1.0 / 256, scalar2=1e-6,
                                                              op0=ALU.mult, op1=ALU.add), reads=[ssq2], writes=[rstd2])
                        k.op("act", lambda e: e.activation(out=rstd2.t[:], in_=rstd2.t[:], func=AF.Sqrt), reads=[rstd2], writes=[rstd2])
                        k.op("dve", lambda e: e.reciprocal(out=rstd2.t[:], in_=rstd2.t[:]), reads=[rstd2], writes=[rstd2])
                        for j in range(4):
                            k.op("dve", lambda e: e.scalar_tensor_tensor(out=gl[b].t[:, j * 256:(j + 1) * 256],
                                                                         in0=gl[b].t[:, j * 256:(j + 1) * 256],
                                                                         scalar=rstd2.t[:, j:j + 1], in1=gn.t[:], op0=ALU.mult,
                                                                         op1=ALU.mult), reads=[gl[b], rstd2, gn], writes=[gl[b]])
                        k.op("pool", lambda e: e.tensor_tensor(out=gl[b].t[:], in0=gl[b].t[:], in1=Rb[b].t[:], op=ALU.mult),
                             reads=[gl[b], Rb[b]], writes=[gl[b]])
                        k.dma("sp", AL[lat0:lat0 + 128, 0:1024], gl[b].t[:], reads=[gl[b]], writes=[bAL])
        k.pop()
        if STOP_AFTER in ("D", "D1"):
            return finish(nc, k, out)


        def bcast_tile(name, src_ap):
            t = k.sb(name, [128, D], F32)
            k.dma("sp", t.t[:], src_ap.partition_broadcast(128), writes=[t])
            return t

        def layer_norm(y, gbc, bbc, outb, st, junkb):
            k.op("act", lambda e: e.activation(out=junkb.t[:], in_=y.t[:], func=AF.Copy, accum_out=st.t[:, 0:1]),
                 reads=[y], writes=[junkb, st])
            k.op("act", lambda e: e.activation(out=junkb.t[:], in_=y.t[:], func=AF.Square, accum_out=st.t[:, 1:2]),
                 reads=[y], writes=[junkb, st])
            k.op("dve", lambda e: e.tensor_scalar(out=st.t[:, 0:2], in0=st.t[:, 0:2], scalar1=1.0 / D, scalar2=None, op0=ALU.mult),
                 reads=[st], writes=[st])
            k.op("dve", lambda e: e.tensor_tensor(out=st.t[:, 2:3], in0=st.t[:, 0:1], in1=st.t[:, 0:1], op=ALU.mult),
                 reads=[st], writes=[st])
            k.op("dve", lambda e: e.tensor_tensor(out=st.t[:, 3:4], in0=st.t[:, 1:2], in1=st.t[:, 2:3], op=ALU.subtract),
                 reads=[st], writes=[st])
            k.op("dve", lambda e: e.tensor_scalar(out=st.t[:, 3:4], in0=st.t[:, 3:4], scalar1=1e-5, scalar2=None, op0=ALU.add),
                 reads=[st], writes=[st])
            k.op("act", lambda e: e.activation(out=st.t[:, 3:4], in_=st.t[:, 3:4], func=AF.Sqrt), reads=[st], writes=[st])
            k.op("dve", lambda e: e.reciprocal(out=st.t[:, 4:5], in_=st.t[:, 3:4]), reads=[st], writes=[st])
            k.op("dve", lambda e: e.scalar_tensor_tensor(out=st.t[:, 5:6], in0=st.t[:, 0:1], scalar=-1.0, in1=st.t[:, 4:5],
                                                         op0=ALU.mult, op1=ALU.mult), reads=[st], writes=[st])
            k.op("act", lambda e: e.activation(out=outb.t[:], in_=y.t[:], func=AF.Identity, scale=st.t[:, 4:5], bias=st.t[:, 5:6]),
                 reads=[y, st], writes=[outb])
            k.op("dve", lambda e: e.tensor_tensor(out=outb.t[:], in0=outb.t[:], in1=gbc.t[:], op=ALU.mult),
                 reads=[outb, gbc], writes=[outb])
            k.op("pool", lambda e: e.tensor_tensor(out=outb.t[:], in0=outb.t[:], in1=bbc.t[:], op=ALU.add),
                 reads=[outb, bbc], writes=[outb])

        affall = k.sb("affall", [128, 32, NE], F32)
        k.push()
        g1bc = bcast_tile("g1bc", MOD[0:1, 2 * D:3 * D])
        l1g = bcast_tile("l1g", ln1_g.ap)
        l1b = bcast_tile("l1b", ln1_b.ap)
        aT = k.sb("aT", [128, 16, 512], F32R)
        oslab = [k.sb("oslab%d" % i, [128, 16, 256], F32R) for i in range(2)]
        osb = k.sb("osbE", [128, 4, D], F32)
        alt = [k.sb("alt%d" % i, [128, D], F32) for i in range(2)]
        xe = k.sb("xe", [128, D], F32)
        x1t = [k.sb("x1t%d" % i, [128, D], F32) for i in range(2)]
        junkE = k.sb("junkE", [128, D], F32)
        stE = k.sb("stE", [128, 8], F32)
        hT = k.sb("hT", [128, 16, 128], F32R)
        wr = k.sb("wr", [128, 16, NE], F32R)
        k.dma("pool", wr.t[:], w_router.ap.rearrange("(c p) e -> p c e", p=128), writes=[wr])
        sm = k.sb("sm", [128, 4], F32)
        ee = k.sb("ee", [128, NE], F32)
        ca = 0
        cso = 0
        NBE = 8 if STOP_AFTER != "E1" else 1
        for tb in range(NBE):
            k.dma("pool", aT.t[:, 8:16, :], ALT[:, tb * 512:(tb + 1) * 512].rearrange("(c p) n -> p c n", p=128),
                  reads=[bALT], writes=[aT])
            for t in range(4):
                al = alt[ca % 2]
                ca += 1
                r0 = tb * 512 + t * 128
                k.dma("sp", al.t[:, 0:1024], AL[r0:r0 + 128, 0:1024], reads=[bAL], writes=[al])
                for c in range(8):
                    pb = PS[c // 4]
                    k.op("pe", lambda e: e.transpose(pb.t[:, (c % 4) * 128:(c % 4 + 1) * 128], al.t[:, c * 128:(c + 1) * 128],
                                                     identf.t[:]), reads=[al, identf], writes=[pb])
                for q4 in range(2):
                    pb = PS[q4]
                    k.op("act", lambda e: e.activation(out=aT.t[:, q4 * 4:q4 * 4 + 4, t * 128:(t + 1) * 128],
                                                       in_=pb.t[:].rearrange("p (c n) -> p c n", c=4), func=AF.Copy),
                         reads=[pb], writes=[aT])
            for cb in range(8):
                sl = oslab[cso % 2]
                cso += 1
                k.dma("pool", sl.t[:], w_o.ap[:, cb * 256:(cb + 1) * 256].rearrange("(c p) f -> p c f", p=128), writes=[sl])
                for t in range(4):
                    pa = PS[4 + (t % 4)]
                    for c in range(16):
                        k.op("pe", lambda e: e.matmul(pa.t[:, 0:256], aT.t[:, c, t * 128:(t + 1) * 128], sl.t[:, c, :],
                                                      start=(c == 0), stop=(c == 15)), reads=[aT, sl], writes=[pa])
                    k.op("act", lambda e: e.activation(out=osb.t[:, t, cb * 256:(cb + 1) * 256], in_=pa.t[:, 0:256], func=AF.Copy),
                         reads=[pa], writes=[osb])
            for t in range(4):
                r0 = tb * 512 + t * 128
                tile_i = tb * 4 + t
                x1 = x1t[tile_i % 2]
                k.dma("sp", xe.t[:], x.ap[r0:r0 + 128, :], writes=[xe])
                k.op("dve", lambda e: e.tensor_tensor(out=osb.t[:, t, :], in0=osb.t[:, t, :], in1=g1bc.t[:], op=ALU.mult),
                     reads=[osb, g1bc], writes=[osb])
                k.op("dve", lambda e: e.scalar_tensor_tensor(out=xe.t[:], in0=xe.t[:], scalar=ALPHA, in1=osb.t[:, t, :],
                                                             op0=ALU.mult, op1=ALU.add), reads=[xe, osb], writes=[xe])
                layer_norm(xe, l1g, l1b, x1, stE, junkE)
                k.dma("sp", X1[r0:r0 + 128, :], x1.t[:], reads=[x1], writes=[bX1])
                for c in range(16):
                    pb = PS[c // 4]
                    k.op("pe", lambda e: e.transpose(pb.t[:, (c % 4) * 128:(c % 4 + 1) * 128], x1.t[:, c * 128:(c + 1) * 128],
                                                     identf.t[:]), reads=[x1, identf], writes=[pb])
                for c in range(16):
                    pb = PS[c // 4]
                    k.op("act", lambda e: e.activation(out=hT.t[:, c, :], in_=pb.t[:, (c % 4) * 128:(c % 4 + 1) * 128],
                                                       func=AF.Identity, scale=modT.t[:, 0, 64 + c:65 + c],
                                                       bias=modT.t[:, 0, 48 + c:49 + c]), reads=[pb, modT], writes=[hT])
                pl = PS[4]
                for c in range(16):
                    k.op("pe", lambda e: e.matmul(pl.t[:, 0:NE], hT.t[:, c, :], wr.t[:, c, :], start=(c == 0), stop=(c == 15)),
                         reads=[hT, wr], writes=[pl])
                k.op("dve", lambda e: e.tensor_reduce(out=sm.t[:, 0:1], in_=pl.t[:, 0:NE], axis=AX.X, op=ALU.max),
                     reads=[pl], writes=[sm])
                k.op("dve", lambda e: e.tensor_scalar(out=sm.t[:, 1:2], in0=sm.t[:, 0:1], scalar1=-1.0, scalar2=None, op0=ALU.mult),
                     reads=[sm], writes=[sm])
                k.op("act", lambda e: e.activation(out=ee.t[:], in_=pl.t[:, 0:NE], func=AF.Exp, bias=sm.t[:, 1:2],
                                                   accum_out=sm.t[:, 2:3]), reads=[pl, sm], writes=[ee, sm])
                k.op("dve", lambda e: e.reciprocal(out=sm.t[:, 3:4], in_=sm.t[:, 2:3]), reads=[sm], writes=[sm])
                k.op("dve", lambda e: e.tensor_scalar(out=affall.t[:, tile_i, :], in0=ee.t[:], scalar1=sm.t[:, 3:4], scalar2=None,
                                                      op0=ALU.mult), reads=[ee, sm], writes=[affall])
        k.pop()
        if DEBUG and "AFF" in DEBUG_NAMES:
            k.dma("sp", AFF.rearrange("(t p) e -> p t e", p=128), affall.t[:], reads=[affall], writes=[bAFF])
        if STOP_AFTER in ("E", "E1"):
            return finish(nc, k, out)

        idxI = k.sb("idxI", [128, NE, 4], I32)
        gateS = k.sb("gateS", [128, NE, 4], F32)
        k.push()
        affT = k.sb("affT", [16, NLAT], F32)
        for g8i in range(8):
            pb = PS[g8i % 2]
            for t4 in range(4):
                t = g8i * 4 + t4
                k.op("pe", lambda e: e.transpose(pb.t[0:16, t4 * 128:(t4 + 1) * 128], affall.t[:, t, :], identf.t[:]),
                     reads=[affall, identf], writes=[pb])
            k.op("act", lambda e: e.activation(out=affT.t[:, g8i * 512:(g8i + 1) * 512], in_=pb.t[0:16, :], func=AF.Copy),
                 reads=[pb], writes=[affT])
        bs = k.sb("bs", [16, 8], F32)
        junkT = k.sb("junkT", [16, NLAT], F32)
        k.op("dve", lambda e: e.memset(bs.t[:], 0.0), writes=[bs])
        k.op("dve", lambda e: e.memset(bs.t[:, 1:2], 1.0), reads=[bs], writes=[bs])
        for it in range(30):
            k.op("dve", lambda e: e.tensor_scalar(out=bs.t[:, 5:6], in0=bs.t[:, 1:2], scalar1=0.5, scalar2=None, op0=ALU.mult),
                 reads=[bs], writes=[bs])
            k.op("dve", lambda e: e.scalar_tensor_tensor(out=bs.t[:, 2:3], in0=bs.t[:, 0:1], scalar=0.5, in1=bs.t[:, 5:6],
                                                         op0=ALU.mult, op1=ALU.add), reads=[bs], writes=[bs])
            k.op("dve", lambda e: e.tensor_scalar(out=junkT.t[:], in0=affT.t[:], scalar1=bs.t[:, 2:3], scalar2=0.0, op0=ALU.is_ge,
                                                  op1=ALU.add, accum_out=bs.t[:, 3:4]), reads=[affT, bs], writes=[junkT, bs])
            k.op("dve", lambda e: e.tensor_scalar(out=bs.t[:, 4:5], in0=bs.t[:, 3:4], scalar1=float(CAP), scalar2=None,
                                                  op0=ALU.is_ge), reads=[bs], writes=[bs])
            k.op("dve", lambda e: e.tensor_tensor(out=bs.t[:, 5:6], in0=bs.t[:, 2:3], in1=bs.t[:, 0:1], op=ALU.subtract),
                 reads=[bs], writes=[bs])
            k.op("dve", lambda e: e.tensor_tensor(out=bs.t[:, 6:7], in0=bs.t[:, 1:2], in1=bs.t[:, 2:3], op=ALU.subtract),
                 reads=[bs], writes=[bs])
            k.op("dve", lambda e: e.scalar_tensor_tensor(out=bs.t[:, 0:1], in0=bs.t[:, 5:6], scalar=bs.t[:, 4:5], in1=bs.t[:, 0:1],
                                                         op0=ALU.mult, op1=ALU.add), reads=[bs], writes=[bs])
            k.op("dve", lambda e: e.scalar_tensor_tensor(out=bs.t[:, 1:2], in0=bs.t[:, 6:7], scalar=bs.t[:, 4:5], in1=bs.t[:, 2:3],
                                                         op0=ALU.mult, op1=ALU.add), reads=[bs], writes=[bs])
        Mt = k.sb("Mt", [16, NLAT], F32)
        k.op("dve", lambda e: e.tensor_scalar(out=Mt.t[:], in0=affT.t[:], scalar1=bs.t[:, 0:1], scalar2=None, op0=ALU.is_ge),
             reads=[affT, bs], writes=[Mt])
        k.op("dve", lambda e: e.memset(junkT.t[:], 1.0), writes=[junkT])
        cumM = k.sb("cumM", [16, NLAT], F32)
        k.op("dve", lambda e: e.tensor_tensor_scan(out=cumM.t[:], data0=junkT.t[:], data1=Mt.t[:], initial=0.0, op0=ALU.mult,
                                                   op1=ALU.add), reads=[junkT, Mt], writes=[cumM])
        k.op("dve", lambda e: e.scalar_tensor_tensor(out=cumM.t[:], in0=Mt.t[:], scalar=-8193.0, in1=cumM.t[:], op0=ALU.mult,
                                                     op1=ALU.add), reads=[Mt, cumM], writes=[cumM])
        k.op("dve", lambda e: e.tensor_scalar(out=cumM.t[:], in0=cumM.t[:], scalar1=8192.0, scalar2=None, op0=ALU.add),
             reads=[cumM], writes=[cumM])
        slotTM = k.sb("slotTM", [128, 32, NE], F32)
        pb = PS[2]
        for t in range(32):
            k.op("pe", lambda e: e.transpose(pb.t[:, t * 16:(t + 1) * 16], cumM.t[:, t * 128:(t + 1) * 128], identf.t[0:16, 0:16]),
                 reads=[cumM, identf], writes=[pb])
        k.op("act", lambda e: e.activation(out=slotTM.t[:].rearrange("p t e -> p (t e)"), in_=pb.t[:], func=AF.Copy),
             reads=[pb], writes=[slotTM])
        vals = k.sb("vals", [128, 32, NE, 4], F32)
        tokv = k.sb("tokv", [128, 32, 2], F32)
        k.dma("sp", tokv.t[:], c_tokv.ap, writes=[tokv])
        for e_ in range(NE):
            k.op("pool", lambda e: e.tensor_copy(out=vals.t[:, :, e_, 0:2], in_=tokv.t[:]), reads=[tokv], writes=[vals])
        k.op("pool", lambda e: e.tensor_copy(out=vals.t[:, :, :, 2], in_=affall.t[:]), reads=[affall], writes=[vals])
        k.op("pool", lambda e: e.tensor_copy(out=vals.t[:, :, :, 3], in_=affall.t[:]), reads=[affall], writes=[vals])
        iot = k.sb("iot", [128, 512], F32)
        k.dma("sp", iot.t[:], c_iota.ap, writes=[iot])
        Sel = [k.sb("Sel%d" % i, [128, 512], F32) for i in range(3)]
        res = k.sb("resF", [128, 4, 4], F32)
        idf = k.sb("idf", [128, 4], F32)
        csel = 0
        for e_ in range(NE):
            for t in range(32):
                sl = Sel[csel % 3]
                csel += 1
                k.op("dve", lambda e: e.tensor_scalar(out=sl.t[:], in0=iot.t[:], scalar1=slotTM.t[:, t, e_:e_ + 1], scalar2=None,
                                                      op0=ALU.is_equal), reads=[iot, slotTM], writes=[sl])
                for st in range(4):
                    k.op("pe", lambda e: e.matmul(PS[4 + st].t[:, 0:4], sl.t[:, st * 128:(st + 1) * 128], vals.t[:, t, e_, :],
                                                  start=(t == 0), stop=(t == 31)), reads=[sl, vals], writes=[PS[4 + st]])
            for st in range(4):
                k.op("act", lambda e: e.activation(out=res.t[:, st, :], in_=PS[4 + st].t[:, 0:4], func=AF.Copy),
                     reads=[PS[4 + st]], writes=[res])
            k.op("dve", lambda e: e.scalar_tensor_tensor(out=idf.t[:], in0=res.t[:, :, 0], scalar=64.0, in1=res.t[:, :, 1],
                                                         op0=ALU.mult, op1=ALU.add), reads=[res], writes=[idf])
            k.op("dve", lambda e: e.tensor_copy(out=idxI.t[:, e_, :], in_=idf.t[:]), reads=[idf], writes=[idxI])
            k.op("dve", lambda e: e.tensor_copy(out=gateS.t[:, e_, :], in_=res.t[:, :, 2]), reads=[res], writes=[gateS])
        k.pop()
        if DEBUG and "IDXD" in DEBUG_NAMES:
            k.dma("sp", IDXD, idxI.t[:], reads=[idxI], writes=[bAFF])
            k.dma("sp", GATED, gateS.t[:], reads=[gateS], writes=[bAFF])
        if STOP_AFTER == "F":
            return finish(nc, k, out)

        k.push()
        zt = k.sb("zt", [128, D], F32)
        k.op("pool", lambda e: e.memset(zt.t[:], 0.0), writes=[zt])
        for t in range(32):
            k.dma("sp", FACC[t * 128:(t + 1) * 128, :], zt.t[:], reads=[zt], writes=[bFACC])
        xsT = k.sb("xsT", [128, 16, 512], F32R)
        hidT = k.sb("hidT", [128, 16, 512], F32R)
        gsl = [k.sb("gsl%d" % i, [128, 16, 512], F32R) for i in range(2)]
        xg = k.sb("xg", [128, D], F32)
        yst = [k.sb("yst%d" % i, [128, D], F32) for i in range(4)]
        s1 = k.sb("s1", [128, 4, 512], F32)
        t3 = [k.sb("t3_%d" % i, [128, 512], F32) for i in range(2)]
        cg = 0
        cpp = 0
        scat_prev = [None]
        NEX = NE if STOP_AFTER != "G1" else 1
        for e_ in range(NEX):
            for st in range(4):
                k.idma(lambda e: e.indirect_dma_start(out=xg.t[:], out_offset=None, in_=X1,
                                                      in_offset=bass.IndirectOffsetOnAxis(ap=idxI.t[:, e_, st:st + 1], axis=0)),
                       reads=[bX1, idxI], writes=[xg])
                for c in range(16):
                    pb = PS[c // 4]
                    k.op("pe", lambda e: e.transpose(pb.t[:, (c % 4) * 128:(c % 4 + 1) * 128], xg.t[:, c * 128:(c + 1) * 128],
                                                     identf.t[:]), reads=[xg, identf], writes=[pb])
                for c in range(16):
                    pb = PS[c // 4]
                    k.op("act", lambda e: e.activation(out=xsT.t[:, c, st * 128:(st + 1) * 128],
                                                       in_=pb.t[:, (c % 4) * 128:(c % 4 + 1) * 128], func=AF.Identity,
                                                       scale=modT.t[:, 0, 64 + c:65 + c], bias=modT.t[:, 0, 48 + c:49 + c]),
                         reads=[pb, modT], writes=[xsT])
            for fb in range(4):
                for wi, wsrc in enumerate((w1, w3)):
                    sl = gsl[cg % 2]
                    cg += 1
                    k.dma("pool", sl.t[:], wsrc.ap[e_, :, fb * 512:(fb + 1) * 512].rearrange("(c p) f -> p c f", p=128), writes=[sl])
                    for fc in range(4):
                        pa = PS[4 + cpp % 4]
                        cpp += 1
                        for c in range(16):
                            k.op("pe", lambda e: e.matmul(pa.t[:], sl.t[:, c, fc * 128:(fc + 1) * 128], xsT.t[:, c, :],
                                                          start=(c == 0), stop=(c == 15)), reads=[sl, xsT], writes=[pa])
                        if wi == 0:
                            k.op("act", lambda e: e.activation(out=s1.t[:, fc, :], in_=pa.t[:], func=AF.Silu), reads=[pa], writes=[s1])
                        else:
                            tt = t3[fc % 2]
                            k.op("act", lambda e: e.activation(out=tt.t[:], in_=pa.t[:], func=AF.Copy), reads=[pa], writes=[tt])
                            k.op("dve", lambda e: e.tensor_tensor(out=hidT.t[:, fb * 4 + fc, :], in0=tt.t[:], in1=s1.t[:, fc, :],
                                                                  op=ALU.mult), reads=[tt, s1], writes=[hidT])
            for db in range(4):
                sl = gsl[cg % 2]
                cg += 1
                k.dma("pool", sl.t[:], w2.ap[e_, :, db * 512:(db + 1) * 512].rearrange("(c p) f -> p c f", p=128), writes=[sl])
                for st in range(4):
                    pa = PS[4 + cpp % 4]
                    cpp += 1
                    for c in range(16):
                        k.op("pe", lambda e: e.matmul(pa.t[:], hidT.t[:, c, st * 128:(st + 1) * 128], sl.t[:, c, :],
                                                      start=(c == 0), stop=(c == 15)), reads=[sl, hidT], writes=[pa])
                    k.op("act", lambda e: e.activation(out=yst[st].t[:, db * 512:(db + 1) * 512], in_=pa.t[:], func=AF.Copy,
                                                       scale=gateS.t[:, e_, st:st + 1]), reads=[pa, gateS], writes=[yst[st]])
            for st in range(4):
                k.idma(lambda e: e.indirect_dma_start(out=FACC, out_offset=bass.IndirectOffsetOnAxis(ap=idxI.t[:, e_, st:st + 1], axis=0),
                                                      in_=yst[st].t[:], in_offset=None, compute_op=ALU.add),
                       reads=[yst[st], idxI, bFACCs, bFACC], writes=[bFACCs])
        k.pop()
        if STOP_AFTER in ("G", "G1"):
            return finish(nc, k, out)

        k.push()
        g2bc = bcast_tile("g2bc", MOD[0:1, 5 * D:6 * D])
        l2g = bcast_tile("l2g", ln2_g.ap)
        l2b = bcast_tile("l2b", ln2_b.ap)
        xh = [k.sb("xh%d" % i, [128, D], F32) for i in range(2)]
        fh = [k.sb("fh%d" % i, [128, D], F32) for i in range(2)]
        oh = [k.sb("oh%d" % i, [128, D], F32) for i in range(2)]
        junkH = k.sb("junkH", [128, D], F32)
        stH = k.sb("stH", [128, 8], F32)
        for t in range(32):
            b = t % 2
            k.dma("sp", xh[b].t[:], X1[t * 128:(t + 1) * 128, :], reads=[bX1], writes=[xh[b]])
            k.dma("sp", fh[b].t[:], FACC[t * 128:(t + 1) * 128, :], reads=[bFACC, bFACCs], writes=[fh[b]])
            k.op("dve", lambda e: e.tensor_tensor(out=fh[b].t[:], in0=fh[b].t[:], in1=g2bc.t[:], op=ALU.mult),
                 reads=[fh[b], g2bc], writes=[fh[b]])
            k.op("dve", lambda e: e.scalar_tensor_tensor(out=xh[b].t[:], in0=xh[b].t[:], scalar=ALPHA, in1=fh[b].t[:],
                                                         op0=ALU.mult, op1=ALU.add), reads=[xh[b], fh[b]], writes=[xh[b]])
            layer_norm(xh[b], l2g, l2b, oh[b], stH, junkH)
            k.dma("sp", out[t * 128:(t + 1) * 128, :], oh[b].t[:], reads=[oh[b]], writes=[bOUT])
        k.pop()

        finish(nc, k, out)
    return nc


def finish(nc, k, out):
    k.barrier(["sp"])
    return nc


_PROGRAM = None


def host_constants():
    import ml_dtypes
    c = {}
    c["c_identf"] = np.eye(128, dtype=np.float32)
    c["c_identb"] = np.eye(128, dtype=np.float32).astype(ml_dtypes.bfloat16)
    perm = np.zeros((128, 128), np.float32)
    for m in range(128):
        s = m + 32 if (m % 64) < 32 else m - 32
        perm[s, m] = 1.0
    c["c_perm"] = perm.astype(ml_dtypes.bfloat16)
    n = np.arange(NLAT)
    row = (n // 64).astype(np.float32)
    col = (n % 64).astype(np.float32)
    inv = (np.float32(10000.0) ** (-np.arange(16, dtype=np.float32) / np.float32(16))).astype(np.float32)
    ang = np.concatenate([row[:, None] * inv[None, :], col[:, None] * inv[None, :]], axis=-1).astype(np.float32)
    cs = np.cos(ang).astype(np.float32).T
    sn = np.sin(ang).astype(np.float32).T
    c["c_ropec"] = np.ascontiguousarray(np.concatenate([cs, cs, cs, cs], axis=0))
    c["c_ropes"] = np.ascontiguousarray(np.concatenate([-sn, sn, -sn, sn], axis=0))
    rs = np.ones((128, 512), np.float32)
    rs[:, ::128] = 0.0
    c["c_reset"] = rs
    j = np.arange(128)[:, None]
    i = np.arange(128)[None, :]
    c["c_maskf"] = (j <= i).astype(np.float32).astype(ml_dtypes.bfloat16)
    c["c_maskb"] = (j >= i).astype(np.float32).astype(ml_dtypes.bfloat16)
    c["c_iota"] = np.tile(np.arange(512, dtype=np.float32)[None, :], (128, 1))
    tok = (np.arange(32)[None, :] * 128 + np.arange(128)[:, None])
    c["c_tokv"] = np.stack([(tok // 64).astype(np.float32), (tok % 64).astype(np.float32)], axis=-1)
    return c


def make_in_maps(inp):
    consts = host_constants()
    f = lambda a: np.ascontiguousarray(np.asarray(a, dtype=np.float32))
    shared = {
        "w_ada": f(inp["w_ada"][0]), "b_ada": f(inp["b_ada"][0]).reshape(1, -1), "w_in": f(inp["w_in"][0]),
        "w_gate2": f(inp["w_gate2"][0]), "b_gate": np.ascontiguousarray(f(inp["b_gate"][0]).reshape(2, 4, 128).transpose(2, 0, 1)),
        "gla_norm_g": f(inp["gla_norm_g"][0]).reshape(1, -1), "diff_lambda": f(inp["diff_lambda"][0]).reshape(1, -1),
        "diff_norm_g": f(inp["diff_norm_g"][0]).reshape(1, -1), "w_o": f(inp["w_o"][0]),
        "ln1_g": f(inp["ln1_g"][0]).reshape(1, -1), "ln1_b": f(inp["ln1_b"][0]).reshape(1, -1),
        "w_router": f(inp["w_router"][0]), "w1": f(inp["w1"][0]), "w3": f(inp["w3"][0]), "w2": f(inp["w2"][0]),
        "ln2_g": f(inp["ln2_g"][0]).reshape(1, -1), "ln2_b": f(inp["ln2_b"][0]).reshape(1, -1),
    }
    shared.update(consts)
    maps = []
    for core in range(8):
        b = core % 4
        m = dict(shared)
        m["x"] = f(inp["x"][b])
        m["ctx"] = f(inp["ctx"][b])
        m["cvec"] = np.ascontiguousarray(np.stack([np.asarray(inp["c"][b], np.float32), np.asarray(inp["c_ctx"], np.float32)]))
        maps.append(m)
    return maps


def kernel(**inputs):
    nc = build_program()
    in_maps = make_in_maps(inputs)
    in_maps = [{kk: v for kk, v in m.items() if kk in nc.used_inputs} for m in in_maps]
    res = run_bass_kernel_spmd(nc, in_maps, core_ids=list(range(8)))
    outs = [np.asarray(res.results[b]["out"]) for b in range(4)]
    return np.stack(outs, axis=0).astype(np.float32)
```

```python
import math
from contextlib import ExitStack

import numpy as np
import concourse.bass as bass
import concourse.mybir as mybir
from concourse.bass_utils import run_bass_kernel_spmd

F32 = mybir.dt.float32
F32R = mybir.dt.float32r
BF16 = mybir.dt.bfloat16
I32 = mybir.dt.int32
AF = mybir.ActivationFunctionType
ALU = mybir.AluOpType
AX = mybir.AxisListType

D = 2048
NLAT = 4096
NCTX = 256
NTOK = NLAT + NCTX
DIN = 6176
NE = 16
CAP = 512
ALPHA = 2.0 ** 0.25
LAM_INIT = 0.2
NDSEM = 80

STOP_AFTER = "Z"
DEBUG = False
DEBUG_NAMES = ()
B_KINDS = None
ROPE_STAGE = 9
C_HEADS = 8
N_FILL = 0


class Buf:
    def __init__(self, t=None, waw=True):
        self.t = t
        self.w = {}
        self.r = {}
        self.waw = waw


class K:
    def __init__(self, nc, es):
        self.nc = nc
        self.es = es
        self.E = {"pe": nc.tensor, "act": nc.scalar, "dve": nc.vector, "pool": nc.gpsimd, "sp": nc.sync}
        self.sem = {e: es.enter_context(nc.semaphore("s_" + e)) for e in self.E}
        self.seq = {e: 0 for e in self.E}
        self.known = {e: {} for e in self.E}
        self.dsem = [es.enter_context(nc.semaphore("d%d" % i)) for i in range(NDSEM)]
        self.duse = [0] * NDSEM
        self.dcount = 0
        self.dcount_sw = 0
        self.scopes = []
        self.n = 0

    def push(self):
        self.scopes.append(ExitStack())

    def pop(self):
        self.barrier()
        self.scopes.pop().close()

    def sb(self, name, shape, dtype):
        st = self.scopes[-1] if self.scopes else self.es
        return Buf(st.enter_context(self.nc.sbuf_tensor(name, list(shape), dtype)))

    def ps(self, name, shape, dtype):
        st = self.scopes[-1] if self.scopes else self.es
        return Buf(st.enter_context(self.nc.psum_tensor(name, list(shape), dtype)))

    def _semobj(self, key):
        return self.sem[key[1]] if key[0] == "e" else self.dsem[key[1]]

    def _wait(self, eng, key, val):
        if self.known[eng].get(key, 0) >= val:
            return
        self.E[eng].wait_ge(self._semobj(key), val)
        self.known[eng][key] = val

    def _deps(self, eng, reads, writes):
        for b in reads:
            for kk, v in b.w.items():
                if eng == "pe" and kk == ("e", "pe"):
                    continue
                self._wait(eng, kk, v)
        for b in writes:
            if b.waw:
                for kk, v in b.w.items():
                    if eng == "pe" and kk == ("e", "pe"):
                        continue
                    self._wait(eng, kk, v)
            for kk, v in b.r.items():
                if eng == "pe" and kk == ("e", "pe"):
                    continue
                self._wait(eng, kk, v)

    def _commit(self, key, val, reads, writes):
        for b in reads:
            b.r[key] = max(b.r.get(key, 0), val)
        for b in writes:
            if b.waw:
                b.w = {key: val}
                b.r = {}
            else:
                b.w[key] = max(b.w.get(key, 0), val)

    def op(self, eng, fn, reads=(), writes=()):
        self._deps(eng, reads, writes)
        inst = fn(self.E[eng])
        self.seq[eng] += 1
        inst.then_inc(self.sem[eng], 1)
        self._commit(("e", eng), self.seq[eng], reads, writes)
        self.n += 1

    def _dslot(self, q):
        half = NDSEM // 2
        if q == "pool":
            i = half + self.dcount_sw % half
            self.dcount_sw += 1
        else:
            i = self.dcount % half
            self.dcount += 1
        if self.duse[i] > 0:
            self._wait(q, ("d", i), self.duse[i] * 16)
        return i

    def dma(self, q, out, in_, reads=(), writes=(), **kw):
        self._deps(q, reads, writes)
        i = self._dslot(q)
        inst = self.E[q].dma_start(out=out, in_=in_, **kw)
        self.duse[i] += 1
        inst.then_inc(self.dsem[i], 16)
        self._commit(("d", i), self.duse[i] * 16, reads, writes)
        self.n += 1

    def idma(self, fn, reads=(), writes=()):
        q = "pool"
        self._deps(q, reads, writes)
        i = self._dslot(q)
        inst = fn(self.E[q])
        self.duse[i] += 1
        inst.then_inc(self.dsem[i], 16)
        self._commit(("d", i), self.duse[i] * 16, reads, writes)
        self.n += 1

    def barrier(self, engines=None):
        for e in (engines or list(self.E)):
            for f in self.E:
                if self.seq[f] > 0 and not (e == f == "pe"):
                    self._wait(e, ("e", f), self.seq[f])
            for i in range(NDSEM):
                if self.duse[i] > 0:
                    self._wait(e, ("d", i), self.duse[i] * 16)


def build_program():
    nc = bass.Bass("TRN2", target_bir_lowering=False)

    used_inputs = []

    class Lazy:
        def __init__(self, name, shape, dt=F32):
            self.name, self.shape, self.dt, self._ap = name, list(shape), dt, None

        @property
        def ap(self):
            if self._ap is None:
                self._ap = nc.dram_tensor(self.name, self.shape, self.dt, kind="ExternalInput").ap()
                used_inputs.append(self.name)
            return self._ap

    def din(name, shape, dt=F32):
        return Lazy(name, shape, dt)

    def dscr(name, shape, dt=F32):
        kind = "ExternalOutput" if (DEBUG and name in DEBUG_NAMES) else "Internal"
        return nc.dram_tensor(name, list(shape), dt, kind=kind).ap()

    x = din("x", [NLAT, D])
    ctx = din("ctx", [NCTX, D])
    cvec = din("cvec", [2, D])
    w_ada = din("w_ada", [D, 6 * D])
    b_ada = din("b_ada", [1, 6 * D])
    w_in = din("w_in", [D, DIN])
    w_gate2 = din("w_gate2", [2, 16, 512])
    b_gate = din("b_gate", [128, 2, 4])
    gla_norm_g = din("gla_norm_g", [1, 256])
    diff_lambda = din("diff_lambda", [1, 256])
    diff_norm_g = din("diff_norm_g", [1, 128])
    w_o = din("w_o", [D, D])
    ln1_g = din("ln1_g", [1, D])
    ln1_b = din("ln1_b", [1, D])
    w_router = din("w_router", [D, NE])
    w1 = din("w1", [NE, D, D])
    w3 = din("w3", [NE, D, D])
    w2 = din("w2", [NE, D, D])
    ln2_g = din("ln2_g", [1, D])
    ln2_b = din("ln2_b", [1, D])
    c_identf = din("c_identf", [128, 128])
    c_identb = din("c_identb", [128, 128], BF16)
    c_perm = din("c_perm", [128, 128], BF16)
    c_ropec = din("c_ropec", [128, NLAT])
    c_ropes = din("c_ropes", [128, NLAT])
    c_reset = din("c_reset", [128, 512])
    c_maskf = din("c_maskf", [128, 128], BF16)
    c_maskb = din("c_maskb", [128, 128], BF16)
    c_iota = din("c_iota", [128, 512])
    c_tokv = din("c_tokv", [128, 32, 2])
    out = nc.dram_tensor("out", [NLAT, D], F32, kind="ExternalOutput").ap()
    nc.used_inputs = used_inputs

    MOD = dscr("MOD", [2, 6 * D])
    AT = dscr("AT", [2, 4, 128, NTOK], BF16)
    BT = dscr("BT", [2, 4, 128, NTOK], BF16)
    ETOT = dscr("ETOT", [2, 128, 4, 34])
    VG = dscr("VG", [NTOK, 1024], BF16)
    RG = dscr("RG", [NLAT, 1024])
    QDT = dscr("QDT", [8, 128, NLAT], BF16)
    KDT = dscr("KDT", [8, 128, NTOK], BF16)
    VD = dscr("VD", [NTOK, 1024], BF16)
    OFS = dscr("OFS", [NLAT, 1024])
    AL = dscr("AL", [NLAT, D])
    X1 = dscr("X1", [NLAT, D])
    H = dscr("H", [NLAT, D])
    AFF = dscr("AFF", [NLAT, NE])
    FACC = dscr("FACC", [NLAT, D])
    ALT = dscr("ALT", [1024, NLAT])
    IDXD = dscr("IDXD", [128, NE, 4], I32)
    GATED = dscr("GATED", [128, NE, 4])
    bMOD, bAT, bBT, bETOT, bVG, bRG, bQDT, bKDT, bVD, bOFS, bAL, bX1, bH, bAFF, bFACC, bOUT = [
        Buf(None, waw=False) for _ in range(16)]
    bALT = Buf(None, waw=False)
    bFACCs = Buf(None, waw=True)

    with ExitStack() as es:
        k = K(nc, es)
        PS = [k.ps("psb%d" % i, [128, 512], F32) for i in range(8)]
        identf = k.sb("identf", [128, 128], F32)
        identb = k.sb("identb", [128, 128], BF16)
        k.dma("sp", identf.t[:], c_identf.ap, writes=[identf])
        k.dma("sp", identb.t[:], c_identb.ap, writes=[identb])
        modT = k.sb("modT", [128, 2, 96], F32)

        k.push()
        cT = k.sb("cT", [128, 16, 128], F32)
        k.op("pool", lambda e: e.memset(cT.t[:], 0.0), writes=[cT])
        with nc.allow_non_contiguous_dma(reason="tiny transposed vector load"):
            for r in range(2):
                k.dma("sp", cT.t[:, :, r], cvec.ap[r, :].rearrange("(c p) -> p c", p=128), writes=[cT])
        scT = k.sb("scT", [128, 16, 128], F32R)
        k.op("act", lambda e: e.activation(out=scT.t[:], in_=cT.t[:], func=AF.Silu), reads=[cT], writes=[scT])
        bada2 = k.sb("bada2", [2, 6 * D], F32)
        k.dma("sp", bada2.t[:], b_ada.ap.partition_broadcast(2), writes=[bada2])
        mod_sb = k.sb("mod_sb", [2, 6 * D], F32)
        slabs = [k.sb("aslab%d" % i, [128, 16, 512], F32R) for i in range(2)]
        for blk in range(24):
            slab = slabs[blk % 2]
            k.dma("pool", slab.t[:], w_ada.ap[:, blk * 512:(blk + 1) * 512].rearrange("(c p) f -> p c f", p=128),
                  writes=[slab])
            ps = PS[blk % 2]
            for c in range(16):
                k.op("pe", lambda e: e.matmul(ps.t[:], scT.t[:, c, :], slab.t[:, c, :], start=(c == 0), stop=(c == 15)),
                     reads=[scT, slab], writes=[ps])
            k.op("dve", lambda e: e.tensor_tensor(out=mod_sb.t[:, blk * 512:(blk + 1) * 512], in0=ps.t[0:2, :],
                                                  in1=bada2.t[:, blk * 512:(blk + 1) * 512], op=ALU.add),
                 reads=[ps, bada2], writes=[mod_sb])
        k.dma("sp", MOD, mod_sb.t[:], reads=[mod_sb], writes=[bMOD])
        with nc.allow_non_contiguous_dma(reason="small transposed reload of modulation vectors"):
            k.dma("sp", modT.t[:], MOD.rearrange("r (j p) -> p r j", p=128), reads=[bMOD], writes=[modT])
        for j0 in (16, 64):
            k.op("dve", lambda e: e.tensor_scalar(out=modT.t[:, :, j0:j0 + 16], in0=modT.t[:, :, j0:j0 + 16],
                                                  scalar1=1.0, scalar2=None, op0=ALU.add),
                 reads=[modT], writes=[modT])
        k.pop()
        if STOP_AFTER == "A":
            return finish(nc, k, out)


        k.push()
        Cblk = k.sb("Cblk", [128, 512], F32)
        Sblk = k.sb("Sblk", [128, 512], F32)
        qraw = [k.sb("qraw%d" % i, [128, 512], BF16) for i in range(2)]
        uT = k.sb("uT", [128, 16, 512], F32R)
        xt = [k.sb("xt%d" % i, [128, D], F32) for i in range(2)]
        slabs = [k.sb("bslab%d" % i, [128, 16, 512], F32R) for i in range(2)]
        wg2 = k.sb("wg2", [48, 512], F32)
        k.dma("sp", wg2.t[0:16, :], w_gate2.ap[0], writes=[wg2])
        k.dma("sp", wg2.t[32:48, :], w_gate2.ap[1], writes=[wg2])
        negbg = k.sb("negbg", [128, 2, 4], F32)
        k.dma("sp", negbg.t[:], b_gate.ap, writes=[negbg])
        k.op("dve", lambda e: e.tensor_scalar(out=negbg.t[:], in0=negbg.t[:], scalar1=-1.0, scalar2=None, op0=ALU.mult),
             reads=[negbg], writes=[negbg])
        perm = k.sb("perm", [128, 128], BF16)
        k.dma("sp", perm.t[:], c_perm.ap, writes=[perm])
        resetm = k.sb("resetm", [128, 512], F32)
        k.dma("sp", resetm.t[:], c_reset.ap, writes=[resetm])
        lrT = k.sb("lrT", [48, 512], F32)
        zf = xt[0]
        k.op("pool", lambda e: e.memset(zf.t[:], 0.0), writes=[zf])
        lrslab = k.sb("lrslab", [128, 16, 128], F32R)
        k.op("act", lambda e: e.activation(out=lrslab.t[:].rearrange("p c f -> p (c f)"), in_=zf.t[:], func=AF.Copy),
             reads=[zf], writes=[lrslab])
        with nc.allow_non_contiguous_dma(reason="small low-rank gate columns"):
            for d in range(2):
                k.dma("pool", lrslab.t[:, :, d * 32:d * 32 + 16],
                      w_in.ap[:, 3072 + d * 16:3072 + d * 16 + 16].rearrange("(c p) f -> p c f", p=128), writes=[lrslab])
        EA = k.sb("EA", [128, 2, 4, 512], BF16)
        EB = k.sb("EB", [128, 2, 4, 512], BF16)
        ABst = k.sb("ABst", [128, 2, 2, 4, 512], BF16)
        etst = k.sb("etst", [128, 2, 4, 4], F32)
        tmpE = [k.sb("tmpE%d" % i, [128, 512], F32) for i in range(2)]
        spt = [k.sb("spt%d" % i, [128, 512], F32) for i in range(2)]
        cumt = [k.sb("cumt%d" % i, [128, 512], F32) for i in range(2)]
        argt = [k.sb("argt%d" % i, [128, 512], F32) for i in range(2)]
        Vst = [k.sb("Vst%d" % i, [128, 4, 512], BF16) for i in range(2)]
        Rst = [k.sb("Rst0", [128, 4, 512], F32)] * 2
        QKst = [k.sb("QKst%d" % i, [128, 4, 512], BF16) for i in range(2)]
        t1 = tmpE
        t2 = spt
        SLABS = [("glr", 3072, 32), ("gq", 0, 512), ("gk", 512, 512), ("gv", 1024, 512), ("gv", 1536, 512),
                 ("gr", 2048, 512), ("gr", 2560, 512), ("dq", 3104, 512), ("dq", 3616, 512),
                 ("dk", 4128, 512), ("dk", 4640, 512), ("dv", 5152, 512), ("dv", 5664, 512)]
        cnt = {"x": 0, "slab": 0, "acc": 0, "g": 0, "st": 0, "r": 0}
        NBLK = 9 if STOP_AFTER != "B1" else 2
        for tb in range(NBLK):
            isctx = tb == 0
            NT = 256 if isctx else 512
            tok0 = 0 if isctx else 256 + (tb - 1) * 512
            lat0 = 0 if isctx else (tb - 1) * 512
            mr = 1 if isctx else 0
            src = ctx.ap if isctx else x.ap[lat0:lat0 + 512, :]
            nt = NT // 128
            for t in range(nt):
                xb = xt[cnt["x"] % 2]
                cnt["x"] += 1
                k.dma("sp", xb.t[:], src[t * 128:(t + 1) * 128, :], writes=[xb])
                for c in range(16):
                    pb = PS[c // 4]
                    k.op("pe", lambda e: e.transpose(pb.t[:, (c % 4) * 128:(c % 4 + 1) * 128], xb.t[:, c * 128:(c + 1) * 128],
                                                     identf.t[:]), reads=[xb, identf], writes=[pb])
                for c in range(16):
                    pb = PS[c // 4]
                    k.op("act", lambda e: e.activation(out=uT.t[:, c, t * 128:(t + 1) * 128],
                                                       in_=pb.t[:, (c % 4) * 128:(c % 4 + 1) * 128], func=AF.Identity,
                                                       scale=modT.t[:, mr, 16 + c:17 + c], bias=modT.t[:, mr, c:c + 1]),
                         reads=[pb, modT], writes=[uT])
            if not isctx:
                k.dma("sp", Cblk.t[:], c_ropec.ap[:, lat0:lat0 + 512], writes=[Cblk])
                k.dma("sp", Sblk.t[:], c_ropes.ap[:, lat0:lat0 + 512], writes=[Sblk])
            half = {}
            for (kind, c0, ncols) in SLABS:
                hf = half.get(kind, 0)
                half[kind] = hf + 1
                if isctx and kind in ("gr", "dq"):
                    continue
                if B_KINDS is not None and kind not in B_KINDS:
                    continue
                if kind != "glr":
                    slab = slabs[cnt["slab"] % 2]
                    cnt["slab"] += 1
                if kind == "glr":
                    slab = lrslab
                else:
                    k.dma("pool", slab.t[:], w_in.ap[:, c0:c0 + 512].rearrange("(c p) f -> p c f", p=128), writes=[slab])

                def acc_feat(j):
                    pa = PS[4 + cnt["acc"] % 2]
                    cnt["acc"] += 1
                    for c in range(16):
                        k.op("pe", lambda e: e.matmul(pa.t[:, :NT], slab.t[:, c, j * 128:(j + 1) * 128], uT.t[:, c, :NT],
                                                      start=(c == 0), stop=(c == 15)), reads=[slab, uT], writes=[pa])
                    return pa

                def acc_tok(t):
                    pa = PS[4 + cnt["acc"] % 2]
                    cnt["acc"] += 1
                    for c in range(16):
                        k.op("pe", lambda e: e.matmul(pa.t[:], uT.t[:, c, t * 128:(t + 1) * 128], slab.t[:, c, :],
                                                      start=(c == 0), stop=(c == 15)), reads=[slab, uT], writes=[pa])
                    return pa

                if kind == "glr":
                    pa = acc_feat(0)
                    k.op("act", lambda e: e.activation(out=lrT.t[:, :NT], in_=pa.t[0:48, :NT], func=AF.Copy),
                         reads=[pa], writes=[lrT])
                    nch = NT // 128
                    for d in range(2):
                        for j in range(4):
                            g = cnt["g"] % 2
                            cnt["g"] += 1
                            pz = PS[6 + g]
                            k.op("pe", lambda e: e.matmul(pz.t[:, :NT], wg2.t[d * 32:d * 32 + 16, j * 128:(j + 1) * 128],
                                                          lrT.t[d * 32:d * 32 + 16, :NT], start=True, stop=True),
                                 reads=[wg2, lrT], writes=[pz])
                            k.op("act", lambda e: e.activation(out=tmpE[g].t[:, :NT], in_=pz.t[:, :NT], func=AF.Exp,
                                                               scale=-1.0, bias=negbg.t[:, d, j:j + 1]),
                                 reads=[pz, negbg], writes=[tmpE[g]])
                            k.op("act", lambda e: e.activation(out=spt[g].t[:, :NT], in_=tmpE[g].t[:, :NT], func=AF.Ln,
                                                               scale=1.0, bias=1.0), reads=[tmpE[g]], writes=[spt[g]])
                            k.op("dve", lambda e: e.tensor_tensor_scan(out=cumt[g].t[:, :NT], data0=resetm.t[:, :NT],
                                                                       data1=spt[g].t[:, :NT], initial=0.0, op0=ALU.mult,
                                                                       op1=ALU.add), reads=[resetm, spt[g]], writes=[cumt[g]])
                            if d == 0:
                                arg = cumt[g]
                            else:
                                arg = argt[g]
                                k.op("dve", lambda e: e.tensor_tensor(out=arg.t[:, :NT], in0=cumt[g].t[:, :NT],
                                                                      in1=spt[g].t[:, :NT], op=ALU.subtract),
                                     reads=[cumt[g], spt[g]], writes=[arg])
                            sa = -1.0 / 16 if d == 0 else 1.0 / 16
                            k.op("act", lambda e: e.activation(out=EA.t[:, d, j, :NT], in_=arg.t[:, :NT], func=AF.Exp, scale=sa),
                                 reads=[arg], writes=[EA])
                            k.op("act", lambda e: e.activation(out=EB.t[:, d, j, :NT], in_=arg.t[:, :NT], func=AF.Exp, scale=-sa),
                                 reads=[arg], writes=[EB])
                            k.op("act", lambda e: e.activation(out=etst.t[:, d, j, 0:nch], in_=cumt[g].t[:, 127:NT:128],
                                                               func=AF.Exp, scale=-1.0 / 16), reads=[cumt[g]], writes=[etst])
                    ch0 = tok0 // 128
                    with nc.allow_non_contiguous_dma(reason="tiny per-chunk decay totals"):
                        for d in range(2):
                            k.dma("sp", ETOT[d, :, :, ch0:ch0 + nch], etst.t[:, d, :, 0:nch], reads=[etst], writes=[bETOT])
                elif kind in ("gq", "gk"):
                    isq = kind == "gq"
                    for j in range(4):
                        pa = acc_feat(j)
                        for d in range(2):
                            if isq:
                                k.op("dve", lambda e: e.scalar_tensor_tensor(out=ABst.t[:, 0, d, j, :NT], in0=pa.t[:, :NT],
                                                                             scalar=128.0 ** -0.5, in1=EA.t[:, d, j, :NT],
                                                                             op0=ALU.mult, op1=ALU.mult),
                                     reads=[pa, EA], writes=[ABst])
                            else:
                                k.op("dve", lambda e: e.tensor_tensor(out=ABst.t[:, 1, d, j, :NT], in0=pa.t[:, :NT],
                                                                      in1=EB.t[:, d, j, :NT], op=ALU.mult),
                                     reads=[pa, EB], writes=[ABst])
                    dst = AT if isq else BT
                    k.dma("sp", dst.rearrange("d j p n -> p d j n")[:, :, :, tok0:tok0 + NT],
                          ABst.t[:, 0 if isq else 1, :, :, :NT], reads=[ABst], writes=[bAT if isq else bBT])
                elif kind in ("gv", "dv", "gr"):
                    if kind == "gr":
                        st = Rst[cnt["r"] % 2]
                        cnt["r"] += 1
                    else:
                        st = Vst[cnt["st"] % 2]
                        cnt["st"] += 1
                    for t in range(nt):
                        pa = acc_tok(t)
                        fn = AF.Silu if kind == "gr" else AF.Copy
                        k.op("act", lambda e: e.activation(out=st.t[:, t, :], in_=pa.t[:], func=fn), reads=[pa], writes=[st])
                    if kind == "gr":
                        k.dma("sp", RG[lat0:lat0 + 512, hf * 512:(hf + 1) * 512].rearrange("(t p) f -> p t f", p=128),
                              st.t[:, 0:nt, :], reads=[st], writes=[bRG])
                    else:
                        dst, bd = (VG, bVG) if kind == "gv" else (VD, bVD)
                        k.dma("sp", dst[tok0:tok0 + NT, hf * 512:(hf + 1) * 512].rearrange("(t p) f -> p t f", p=128),
                              st.t[:, 0:nt, :], reads=[st], writes=[bd])
                elif kind in ("dq", "dk"):
                    st = QKst[cnt["st"] % 2]
                    cnt["st"] += 1
                    sc = 0.125 if kind == "dq" else 1.0
                    for jj in range(4):
                        pa = acc_feat(jj)
                        if isctx:
                            k.op("act", lambda e: e.activation(out=st.t[:, jj, :NT], in_=pa.t[:, :NT], func=AF.Copy),
                                 reads=[pa], writes=[st])
                            continue
                        g = cnt["g"] % 2
                        cnt["g"] += 1
                        k.op("act", lambda e: e.activation(out=qraw[g].t[:], in_=pa.t[:], func=AF.Copy), reads=[pa], writes=[qraw[g]])
                        pz = PS[6 + g]
                        if ROPE_STAGE >= 1:
                            k.op("pe", lambda e: e.matmul(pz.t[:], perm.t[:], qraw[g].t[:], start=True, stop=True),
                                 reads=[perm, qraw[g]], writes=[pz])
                        qf, pzf = argt[g], cumt[g]
                        if ROPE_STAGE >= 2:
                            k.op("act", lambda e: e.activation(out=qf.t[:], in_=pa.t[:], func=AF.Copy), reads=[pa], writes=[qf])
                            k.op("dve", lambda e: e.tensor_tensor(out=t1[g].t[:], in0=qf.t[:], in1=Cblk.t[:], op=ALU.mult),
                                 reads=[qf, Cblk], writes=[t1[g]])
                        if ROPE_STAGE >= 3:
                            k.op("act", lambda e: e.activation(out=pzf.t[:], in_=pz.t[:], func=AF.Copy), reads=[pz], writes=[pzf])
                            k.op("dve", lambda e: e.tensor_tensor(out=t2[g].t[:], in0=pzf.t[:], in1=Sblk.t[:], op=ALU.mult),
                                 reads=[pzf, Sblk], writes=[t2[g]])
                        if ROPE_STAGE >= 4:
                            k.op("dve", lambda e: e.tensor_tensor(out=t1[g].t[:], in0=t1[g].t[:], in1=t2[g].t[:], op=ALU.add),
                                 reads=[t1[g], t2[g]], writes=[t1[g]])
                            k.op("act", lambda e: e.activation(out=st.t[:, jj, :], in_=t1[g].t[:], func=AF.Copy, scale=sc),
                                 reads=[t1[g]], writes=[st])
                        else:
                            k.op("act", lambda e: e.activation(out=st.t[:, jj, :], in_=pa.t[:], func=AF.Copy), reads=[pa], writes=[st])
                    if kind == "dq":
                        k.dma("sp", QDT[hf * 4:hf * 4 + 4, :, lat0:lat0 + 512].rearrange("h p n -> p h n"), st.t[:],
                              reads=[st], writes=[bQDT])
                    else:
                        k.dma("sp", KDT[hf * 4:hf * 4 + 4, :, tok0:tok0 + NT].rearrange("h p n -> p h n"), st.t[:, :, :NT],
                              reads=[st], writes=[bKDT])
        k.pop()
        if STOP_AFTER in ("B", "B1"):
            return finish(nc, k, out)


        k.push()
        KT = k.sb("KT", [128, NTOK], BF16)
        Vh = k.sb("Vh", [128, 34, 128], BF16)
        onesb = k.sb("onesb", [128, 128], BF16)
        k.op("pool", lambda e: e.memset(onesb.t[:], 1.0), writes=[onesb])
        onesf = k.sb("onesf", [128, 128], F32)
        k.op("pool", lambda e: e.memset(onesf.t[:], 1.0), writes=[onesf])
        Pacc = [k.sb("Pacc%d" % i, [128, 512], F32) for i in range(2)]
        QT = [[k.sb("QT%d_%d" % (i, m), [128, 512], BF16) for m in range(2)] for i in range(2)]
        for i in range(2):
            for m in range(2):
                k.op("pool", lambda e: e.memset(QT[i][m].t[:], 0.0), writes=[QT[i][m]])
        PT = [k.sb("PT%d" % i, [128, 512], BF16) for i in range(3)]
        On = [k.sb("On%d" % i, [128, 512], F32) for i in range(2)]
        Rt = k.sb("Rt", [128, 512], F32)
        dd = k.sb("dd", [128, 512], F32)
        sqb = k.sb("sqb", [128, 512], BF16)
        rs = k.sb("rs", [128, 512], F32)
        ost = [k.sb("ost%d" % i, [128, 512], F32) for i in range(2)]
        dl = k.sb("dl", [128, 256], F32)
        k.dma("sp", dl.t[:], diff_lambda.ap.partition_broadcast(128), writes=[dl])
        prod = k.sb("prod", [128, 2, 64], F32)
        for i in range(2):
            k.op("dve", lambda e: e.tensor_tensor(out=prod.t[:, i, :], in0=dl.t[:, i * 128:i * 128 + 64],
                                                  in1=dl.t[:, i * 128 + 64:i * 128 + 128], op=ALU.mult), reads=[dl], writes=[prod])
        sums = k.sb("sums", [128, 2], F32)
        k.op("dve", lambda e: e.tensor_reduce(out=sums.t[:], in_=prod.t[:], axis=AX.X, op=ALU.add), reads=[prod], writes=[sums])
        exl = k.sb("exl", [128, 2], F32)
        k.op("act", lambda e: e.activation(out=exl.t[:], in_=sums.t[:], func=AF.Exp), reads=[sums], writes=[exl])
        neglam = k.sb("neglam", [128, 1], F32)
        k.op("dve", lambda e: e.tensor_tensor(out=neglam.t[:], in0=exl.t[:, 1:2], in1=exl.t[:, 0:1], op=ALU.subtract),
             reads=[exl], writes=[neglam])
        k.op("dve", lambda e: e.tensor_scalar(out=neglam.t[:], in0=neglam.t[:], scalar1=-LAM_INIT, scalar2=None, op0=ALU.add),
             reads=[neglam], writes=[neglam])
        g8c = k.sb("g8c", [128, 1], F32)
        with nc.allow_non_contiguous_dma(reason="tiny per-partition gain vector"):
            k.dma("sp", g8c.t[:], diff_norm_g.ap.rearrange("o (p u) -> (o p) u", u=1), writes=[g8c])
        k.op("dve", lambda e: e.tensor_scalar(out=g8c.t[:], in0=g8c.t[:], scalar1=1.0 - LAM_INIT, scalar2=None, op0=ALU.mult),
             reads=[g8c], writes=[g8c])
        cq = 0
        cp = 0
        cm = 0
        NH = C_HEADS if STOP_AFTER != "C1" else 1
        for h in range(NH):
            k.dma("sp", KT.t[:], KDT[h], reads=[bKDT], writes=[KT])
            k.dma("sp", Vh.t[:], VD[:, h * 128:(h + 1) * 128].rearrange("(t p) f -> p t f", p=128), reads=[bVD], writes=[Vh])
            for qb in range(8):
                QTb = QT[cq % 2]
                osb = ost[cq % 2]
                cq += 1
                for m in range(2):
                    k.dma("sp", QTb[m].t[64 * m:64 * m + 64, :], QDT[h, 64 * m:64 * m + 64, qb * 512:(qb + 1) * 512],
                          reads=[bQDT], writes=[QTb[m]])
                steps = [(m, kt) for m in range(2) for kt in range(34)]

                def emit_qk(i):
                    m, kt = steps[i]
                    pS = PS[i % 3]
                    k.op("pe", lambda e: e.matmul(pS.t[:], KT.t[:, kt * 128:(kt + 1) * 128], QTb[m].t[:], start=True, stop=True),
                         reads=[KT, QTb[m]], writes=[pS])

                emit_qk(0)
                emit_qk(1)
                for i, (m, kt) in enumerate(steps):
                    if i + 2 < len(steps):
                        emit_qk(i + 2)
                    if kt == 0:
                        pO = PS[3 + 2 * (cm % 2)]
                        pL = PS[4 + 2 * (cm % 2)]
                        cm += 1
                    pS = PS[i % 3]
                    PTb = PT[cp % 3]
                    cp += 1
                    for _f in range(N_FILL):
                        k.op("pe", lambda e: e.matmul(PS[7].t[:], onesb.t[:], KT.t[:, 0:512], start=True, stop=True),
                             reads=[onesb, KT], writes=[PS[7]])
                    k.op("act", lambda e: e.activation(out=PTb.t[:], in_=pS.t[:], func=AF.Exp), reads=[pS], writes=[PTb])
                    k.op("pe", lambda e: e.matmul(pO.t[:], Vh.t[:, kt, :], PTb.t[:], start=(kt == 0), stop=(kt == 33)),
                         reads=[PTb, Vh], writes=[pO])
                    if kt == 0:
                        k.op("dve", lambda e: e.tensor_copy(out=Pacc[m].t[:], in_=PTb.t[:]), reads=[PTb], writes=[Pacc[m]])
                    else:
                        k.op("dve", lambda e: e.tensor_tensor(out=Pacc[m].t[:], in0=Pacc[m].t[:], in1=PTb.t[:], op=ALU.add),
                             reads=[Pacc[m], PTb], writes=[Pacc[m]])
                    if kt == 33:
                        k.op("pe", lambda e: e.matmul(pL.t[:], onesf.t[:], Pacc[m].t[:], start=True, stop=True),
                             reads=[Pacc[m], onesf], writes=[pL])
                        k.op("act", lambda e: e.activation(out=On[m].t[:], in_=pO.t[:], func=AF.Copy), reads=[pO], writes=[On[m]])
                        k.op("dve", lambda e: e.reciprocal(out=Rt.t[:], in_=pL.t[:]), reads=[pL], writes=[Rt])
                        k.op("dve", lambda e: e.tensor_tensor(out=On[m].t[:], in0=On[m].t[:], in1=Rt.t[:], op=ALU.mult),
                             reads=[On[m], Rt], writes=[On[m]])
                k.op("dve", lambda e: e.scalar_tensor_tensor(out=dd.t[:], in0=On[1].t[:], scalar=neglam.t[:, 0:1], in1=On[0].t[:],
                                                             op0=ALU.mult, op1=ALU.add), reads=[On[0], On[1], neglam], writes=[dd])
                k.op("dve", lambda e: e.tensor_tensor(out=sqb.t[:], in0=dd.t[:], in1=dd.t[:], op=ALU.mult), reads=[dd], writes=[sqb])
                pR = PS[7]
                k.op("pe", lambda e: e.matmul(pR.t[:], onesb.t[:], sqb.t[:], start=True, stop=True), reads=[onesb, sqb], writes=[pR])
                k.op("dve", lambda e: e.tensor_scalar(out=rs.t[:], in0=pR.t[:], scalar1=1.0 / 128, scalar2=1e-6, op0=ALU.mult,
                                                      op1=ALU.add), reads=[pR], writes=[rs])
                k.op("act", lambda e: e.activation(out=rs.t[:], in_=rs.t[:], func=AF.Ln), reads=[rs], writes=[rs])
                k.op("act", lambda e: e.activation(out=rs.t[:], in_=rs.t[:], func=AF.Exp, scale=-0.5), reads=[rs], writes=[rs])
                k.op("dve", lambda e: e.scalar_tensor_tensor(out=osb.t[:], in0=dd.t[:], scalar=g8c.t[:, 0:1], in1=rs.t[:],
                                                             op0=ALU.mult, op1=ALU.mult), reads=[dd, g8c, rs], writes=[osb])
                k.dma("sp", ALT[h * 128:(h + 1) * 128, qb * 512:(qb + 1) * 512], osb.t[:], reads=[osb], writes=[bALT])
        k.pop()
        if STOP_AFTER in ("C", "C1"):
            return finish(nc, k, out)

        k.push()
        etot = k.sb("etot", [128, 2, 4, 34], F32)
        for d in range(2):
            k.dma("sp", etot.t[:, d], ETOT[d], reads=[bETOT], writes=[etot])
        masks = [k.sb("mask%d" % d, [128, 128], BF16) for d in range(2)]
        k.dma("sp", masks[0].t[:], c_maskf.ap, writes=[masks[0]])
        k.dma("sp", masks[1].t[:], c_maskb.ap, writes=[masks[1]])
        gn = k.sb("gn", [128, 256], F32)
        k.dma("sp", gn.t[:], gla_norm_g.ap.partition_broadcast(128), writes=[gn])
        S32 = [k.sb("S32_%d" % j, [128, 256], F32) for j in range(4)]
        Sb = [k.sb("Sb_%d" % j, [128, 256], BF16) for j in range(4)]
        Ab = [k.sb("Ab%d" % i, [128, 4, 128], BF16) for i in range(2)]
        Bb = [k.sb("Bb%d" % i, [128, 4, 128], BF16) for i in range(2)]
        Vg = [k.sb("Vg%d" % i, [128, 1024], BF16) for i in range(2)]
        OFb = [k.sb("OFb%d" % i, [128, 1024], F32) for i in range(2)]
        Rb = [k.sb("Rb%d" % i, [128, 1024], F32) for i in range(2)]
        OFst = [k.sb("OFst%d" % i, [128, 1024], F32) for i in range(2)]
        gl = [k.sb("gl%d" % i, [128, 1024], F32) for i in range(2)]
        Btok = [k.sb("Btok%d" % i, [128, 128], BF16) for i in range(2)]
        attT = [k.sb("attT%d" % i, [128, 128], BF16) for i in range(2)]
        ste = [k.sb("ste%d" % i, [128, 256], F32) for i in range(2)]
        osb2 = [k.sb("osb2_%d" % i, [128, 256], F32) for i in range(2)]
        junk2 = k.sb("junk2", [128, 256], F32)
        ssq2 = k.sb("ssq2", [128, 4], F32)
        rstd2 = k.sb("rstd2", [128, 4], F32)
        cs = 0
        cx = 0
        for d in range(2):
            for j in range(4):
                k.op("pool", lambda e: e.memset(S32[j].t[:], 0.0), writes=[S32[j]])
                k.op("pool", lambda e: e.memset(Sb[j].t[:], 0.0), writes=[Sb[j]])
            order = list(range(34)) if d == 0 else [1, 0] + list(range(33, 1, -1))
            if STOP_AFTER == "D1":
                order = order[:6]
            for ci in order:
                isctx = ci < 2
                tok0 = ci * 128
                lat0 = tok0 - 256
                b = cs % 2
                cs += 1
                k.dma("sp", Ab[b].t[:], AT[d, :, :, tok0:tok0 + 128].rearrange("j p n -> p j n"), reads=[bAT], writes=[Ab[b]])
                k.dma("sp", Bb[b].t[:], BT[d, :, :, tok0:tok0 + 128].rearrange("j p n -> p j n"), reads=[bBT], writes=[Bb[b]])
                k.dma("sp", Vg[b].t[:], VG[tok0:tok0 + 128, :], reads=[bVG], writes=[Vg[b]])
                if d == 1 and not isctx:
                    k.dma("sp", OFb[b].t[:], OFS[lat0:lat0 + 128, :], reads=[bOFS], writes=[OFb[b]])
                    k.dma("sp", Rb[b].t[:], RG[lat0:lat0 + 128, :], reads=[bRG], writes=[Rb[b]])
                for j in range(4):
                    x2 = cx % 2
                    cx += 1
                    et = etot.t[:, d, j, ci:ci + 1]
                    pT = PS[x2]
                    k.op("pe", lambda e: e.transpose(pT.t[:].bitcast(BF16)[:, 0:128], Bb[b].t[:, j, :], identb.t[:]),
                         reads=[Bb[b], identb], writes=[pT])
                    k.op("act", lambda e: e.activation(out=Btok[x2].t[:], in_=pT.t[:].bitcast(BF16)[:, 0:128], func=AF.Copy),
                         reads=[pT], writes=[Btok[x2]])
                    if d == 1:
                        k.op("dve", lambda e: e.tensor_scalar(out=S32[j].t[:], in0=S32[j].t[:], scalar1=et, scalar2=None,
                                                              op0=ALU.mult), reads=[S32[j], etot], writes=[S32[j]])
                        k.op("act", lambda e: e.activation(out=Sb[j].t[:], in_=S32[j].t[:], func=AF.Copy),
                             reads=[S32[j]], writes=[Sb[j]])
                    if not isctx:
                        pA = PS[2 + x2]
                        k.op("pe", lambda e: e.matmul(pA.t[:, 0:128], Bb[b].t[:, j, :], Ab[b].t[:, j, :], start=True, stop=True),
                             reads=[Ab[b], Bb[b]], writes=[pA])
                        k.op("dve", lambda e: e.tensor_tensor(out=attT[x2].t[:], in0=pA.t[:, 0:128], in1=masks[d].t[:], op=ALU.mult),
                             reads=[pA, masks[d]], writes=[attT[x2]])
                        pO = PS[4 + x2]
                        k.op("pe", lambda e: e.matmul(pO.t[:, 0:256], attT[x2].t[:], Vg[b].t[:, j * 256:(j + 1) * 256],
                                                      start=True, stop=False), reads=[attT[x2], Vg[b]], writes=[pO])
                        k.op("pe", lambda e: e.matmul(pO.t[:, 0:256], Ab[b].t[:, j, :], Sb[j].t[:], start=False, stop=True),
                             reads=[Ab[b], Sb[j]], writes=[pO])
                        if d == 0:
                            k.op("act", lambda e: e.activation(out=OFst[b].t[:, j * 256:(j + 1) * 256], in_=pO.t[:, 0:256],
                                                               func=AF.Copy), reads=[pO], writes=[OFst[b]])
                        else:
                            k.op("act", lambda e: e.activation(out=osb2[x2].t[:], in_=pO.t[:, 0:256], func=AF.Copy),
                                 reads=[pO], writes=[osb2[x2]])
                            k.op("dve", lambda e: e.tensor_tensor(out=gl[b].t[:, j * 256:(j + 1) * 256], in0=osb2[x2].t[:],
                                                                  in1=OFb[b].t[:, j * 256:(j + 1) * 256], op=ALU.add),
                                 reads=[osb2[x2], OFb[b]], writes=[gl[b]])
                            k.op("act", lambda e: e.activation(out=junk2.t[:], in_=gl[b].t[:, j * 256:(j + 1) * 256], func=AF.Square,
                                                               accum_out=ssq2.t[:, j:j + 1]), reads=[gl[b]], writes=[junk2, ssq2])
                    pS2 = PS[6 + x2]
                    k.op("pe", lambda e: e.matmul(pS2.t[:, 0:256], Btok[x2].t[:], Vg[b].t[:, j * 256:(j + 1) * 256],
                                                  start=True, stop=True), reads=[Btok[x2], Vg[b]], writes=[pS2])
                    if d == 0:
                        k.op("act", lambda e: e.activation(out=ste[x2].t[:], in_=pS2.t[:, 0:256], func=AF.Copy, scale=et),
                             reads=[pS2, etot], writes=[ste[x2]])
                        k.op("dve", lambda e: e.scalar_tensor_tensor(out=S32[j].t[:], in0=S32[j].t[:], scalar=et, in1=ste[x2].t[:],
                                                                     op0=ALU.mult, op1=ALU.add),
                             reads=[S32[j], ste[x2], etot], writes=[S32[j]])
                        k.op("act", lambda e: e.activation(out=Sb[j].t[:], in_=S32[j].t[:], func=AF.Copy),
                             reads=[S32[j]], writes=[Sb[j]])
                    else:
                        k.op("act", lambda e: e.activation(out=ste[x2].t[:], in_=pS2.t[:, 0:256], func=AF.Copy),
                             reads=[pS2], writes=[ste[x2]])
                        k.op("dve", lambda e: e.tensor_tensor(out=S32[j].t[:], in0=S32[j].t[:], in1=ste[x2].t[:], op=ALU.add),
                             reads=[S32[j], ste[x2]], writes=[S32[j]])
                if not isctx:
                    if d == 0:
                        k.dma("sp", OFS[lat0:lat0 + 128, :], OFst[b].t[:], reads=[OFst[b]], writes=[bOFS])
                    else:
                        k.op("dve", lambda e: e.tensor_scalar(out=rstd2.t[:], in0=ssq2.t[:], scalar1=1.0 / 256, scalar2=1e-6,
                                                              op0=ALU.mult, op1=ALU.add), reads=[ssq2], writes=[rstd2])
                        k.op("act", lambda e: e.activation(out=rstd2.t[:], in_=rstd2.t[:], func=AF.Sqrt), reads=[rstd2], writes=[rstd2])
                        k.op("dve", lambda e: e.reciprocal(out=rstd2.t[:], in_=rstd2.t[:]), reads=[rstd2], writes=[rstd2])
                        for j in range(4):
                            k.op("dve", lambda e: e.scalar_tensor_tensor(out=gl[b].t[:, j * 256:(j + 1) * 256],
                                                                         in0=gl[b].t[:, j * 256:(j + 1) * 256],
                                                                         scalar=rstd2.t[:, j:j + 1], in1=gn.t[:], op0=ALU.mult,
                                                                         op1=ALU.mult), reads=[gl[b], rstd2, gn], writes=[gl[b]])
                        k.op("pool", lambda e: e.tensor_tensor(out=gl[b].t[:], in0=gl[b].t[:], in1=Rb[b].t[:], op=ALU.mult),
                             reads=[gl[b], Rb[b]], writes=[gl[b]])
                        k.dma("sp", AL[lat0:lat0 + 128, 0:1024], gl[b].t[:], reads=[gl[b]], writes=[bAL])
        k.pop()
        if STOP_AFTER in ("D", "D1"):
            return finish(nc, k, out)


        def bcast_tile(name, src_ap):
            t = k.sb(name, [128, D], F32)
            k.dma("sp", t.t[:], src_ap.partition_broadcast(128), writes=[t])
            return t

        def layer_norm(y, gbc, bbc, outb, st, junkb):
            k.op("act", lambda e: e.activation(out=junkb.t[:], in_=y.t[:], func=AF.Copy, accum_out=st.t[:, 0:1]),
                 reads=[y], writes=[junkb, st])
            k.op("act", lambda e: e.activation(out=junkb.t[:], in_=y.t[:], func=AF.Square, accum_out=st.t[:, 1:2]),
                 reads=[y], writes=[junkb, st])
            k.op("dve", lambda e: e.tensor_scalar(out=st.t[:, 0:2], in0=st.t[:, 0:2], scalar1=1.0 / D, scalar2=None, op0=ALU.mult),
                 reads=[st], writes=[st])
            k.op("dve", lambda e: e.tensor_tensor(out=st.t[:, 2:3], in0=st.t[:, 0:1], in1=st.t[:, 0:1], op=ALU.mult),
                 reads=[st], writes=[st])
            k.op("dve", lambda e: e.tensor_tensor(out=st.t[:, 3:4], in0=st.t[:, 1:2], in1=st.t[:, 2:3], op=ALU.subtract),
                 reads=[st], writes=[st])
            k.op("dve", lambda e: e.tensor_scalar(out=st.t[:, 3:4], in0=st.t[:, 3:4], scalar1=1e-5, scalar2=None, op0=ALU.add),
                 reads=[st], writes=[st])
            k.op("act", lambda e: e.activation(out=st.t[:, 3:4], in_=st.t[:, 3:4], func=AF.Sqrt), reads=[st], writes=[st])
            k.op("dve", lambda e: e.reciprocal(out=st.t[:, 4:5], in_=st.t[:, 3:4]), reads=[st], writes=[st])
            k.op("dve", lambda e: e.scalar_tensor_tensor(out=st.t[:, 5:6], in0=st.t[:, 0:1], scalar=-1.0, in1=st.t[:, 4:5],
                                                         op0=ALU.mult, op1=ALU.mult), reads=[st], writes=[st])
            k.op("act", lambda e: e.activation(out=outb.t[:], in_=y.t[:], func=AF.Identity, scale=st.t[:, 4:5], bias=st.t[:, 5:6]),
                 reads=[y, st], writes=[outb])
            k.op("dve", lambda e: e.tensor_tensor(out=outb.t[:], in0=outb.t[:], in1=gbc.t[:], op=ALU.mult),
                 reads=[outb, gbc], writes=[outb])
            k.op("pool", lambda e: e.tensor_tensor(out=outb.t[:], in0=outb.t[:], in1=bbc.t[:], op=ALU.add),
                 reads=[outb, bbc], writes=[outb])

        affall = k.sb("affall", [128, 32, NE], F32)
        k.push()
        g1bc = bcast_tile("g1bc", MOD[0:1, 2 * D:3 * D])
        l1g = bcast_tile("l1g", ln1_g.ap)
        l1b = bcast_tile("l1b", ln1_b.ap)
        aT = k.sb("aT", [128, 16, 512], F32R)
        oslab = [k.sb("oslab%d" % i, [128, 16, 256], F32R) for i in range(2)]
        osb = k.sb("osbE", [128, 4, D], F32)
        alt = [k.sb("alt%d" % i, [128, D], F32) for i in range(2)]
        xe = k.sb("xe", [128, D], F32)
        x1t = [k.sb("x1t%d" % i, [128, D], F32) for i in range(2)]
        junkE = k.sb("junkE", [128, D], F32)
        stE = k.sb("stE", [128, 8], F32)
        hT = k.sb("hT", [128, 16, 128], F32R)
        wr = k.sb("wr", [128, 16, NE], F32R)
        k.dma("pool", wr.t[:], w_router.ap.rearrange("(c p) e -> p c e", p=128), writes=[wr])
        sm = k.sb("sm", [128, 4], F32)
        ee = k.sb("ee", [128, NE], F32)
        ca = 0
        cso = 0
        NBE = 8 if STOP_AFTER != "E1" else 1
        for tb in range(NBE):
            k.dma("pool", aT.t[:, 8:16, :], ALT[:, tb * 512:(tb + 1) * 512].rearrange("(c p) n -> p c n", p=128),
                  reads=[bALT], writes=[aT])
            for t in range(4):
                al = alt[ca % 2]
                ca += 1
                r0 = tb * 512 + t * 128
                k.dma("sp", al.t[:, 0:1024], AL[r0:r0 + 128, 0:1024], reads=[bAL], writes=[al])
                for c in range(8):
                    pb = PS[c // 4]
                    k.op("pe", lambda e: e.transpose(pb.t[:, (c % 4) * 128:(c % 4 + 1) * 128], al.t[:, c * 128:(c + 1) * 128],
                                                     identf.t[:]), reads=[al, identf], writes=[pb])
                for q4 in range(2):
                    pb = PS[q4]
                    k.op("act", lambda e: e.activation(out=aT.t[:, q4 * 4:q4 * 4 + 4, t * 128:(t + 1) * 128],
                                                       in_=pb.t[:].rearrange("p (c n) -> p c n", c=4), func=AF.Copy),
                         reads=[pb], writes=[aT])
            for cb in range(8):
                sl = oslab[cso % 2]
                cso += 1
                k.dma("pool", sl.t[:], w_o.ap[:, cb * 256:(cb + 1) * 256].rearrange("(c p) f -> p c f", p=128), writes=[sl])
                for t in range(4):
                    pa = PS[4 + (t % 4)]
                    for c in range(16):
                        k.op("pe", lambda e: e.matmul(pa.t[:, 0:256], aT.t[:, c, t * 128:(t + 1) * 128], sl.t[:, c, :],
                                                      start=(c == 0), stop=(c == 15)), reads=[aT, sl], writes=[pa])
                    k.op("act", lambda e: e.activation(out=osb.t[:, t, cb * 256:(cb + 1) * 256], in_=pa.t[:, 0:256], func=AF.Copy),
                         reads=[pa], writes=[osb])
            for t in range(4):
                r0 = tb * 512 + t * 128
                tile_i = tb * 4 + t
                x1 = x1t[tile_i % 2]
                k.dma("sp", xe.t[:], x.ap[r0:r0 + 128, :], writes=[xe])
                k.op("dve", lambda e: e.tensor_tensor(out=osb.t[:, t, :], in0=osb.t[:, t, :], in1=g1bc.t[:], op=ALU.mult),
                     reads=[osb, g1bc], writes=[osb])
                k.op("dve", lambda e: e.scalar_tensor_tensor(out=xe.t[:], in0=xe.t[:], scalar=ALPHA, in1=osb.t[:, t, :],
                                                             op0=ALU.mult, op1=ALU.add), reads=[xe, osb], writes=[xe])
                layer_norm(xe, l1g, l1b, x1, stE, junkE)
                k.dma("sp", X1[r0:r0 + 128, :], x1.t[:], reads=[x1], writes=[bX1])
                for c in range(16):
                    pb = PS[c // 4]
                    k.op("pe", lambda e: e.transpose(pb.t[:, (c % 4) * 128:(c % 4 + 1) * 128], x1.t[:, c * 128:(c + 1) * 128],
                                                     identf.t[:]), reads=[x1, identf], writes=[pb])
                for c in range(16):
                    pb = PS[c // 4]
                    k.op("act", lambda e: e.activation(out=hT.t[:, c, :], in_=pb.t[:, (c % 4) * 128:(c % 4 + 1) * 128],
                                                       func=AF.Identity, scale=modT.t[:, 0, 64 + c:65 + c],
                                                       bias=modT.t[:, 0, 48 + c:49 + c]), reads=[pb, modT], writes=[hT])
                pl = PS[4]
                for c in range(16):
                    k.op("pe", lambda e: e.matmul(pl.t[:, 0:NE], hT.t[:, c, :], wr.t[:, c, :], start=(c == 0), stop=(c == 15)),
                         reads=[hT, wr], writes=[pl])
                k.op("dve", lambda e: e.tensor_reduce(out=sm.t[:, 0:1], in_=pl.t[:, 0:NE], axis=AX.X, op=ALU.max),
                     reads=[pl], writes=[sm])
                k.op("dve", lambda e: e.tensor_scalar(out=sm.t[:, 1:2], in0=sm.t[:, 0:1], scalar1=-1.0, scalar2=None, op0=ALU.mult),
                     reads=[sm], writes=[sm])
                k.op("act", lambda e: e.activation(out=ee.t[:], in_=pl.t[:, 0:NE], func=AF.Exp, bias=sm.t[:, 1:2],
                                                   accum_out=sm.t[:, 2:3]), reads=[pl, sm], writes=[ee, sm])
                k.op("dve", lambda e: e.reciprocal(out=sm.t[:, 3:4], in_=sm.t[:, 2:3]), reads=[sm], writes=[sm])
                k.op("dve", lambda e: e.tensor_scalar(out=affall.t[:, tile_i, :], in0=ee.t[:], scalar1=sm.t[:, 3:4], scalar2=None,
                                                      op0=ALU.mult), reads=[ee, sm], writes=[affall])
        k.pop()
        if DEBUG and "AFF" in DEBUG_NAMES:
            k.dma("sp", AFF.rearrange("(t p) e -> p t e", p=128), affall.t[:], reads=[affall], writes=[bAFF])
        if STOP_AFTER in ("E", "E1"):
            return finish(nc, k, out)

        idxI = k.sb("idxI", [128, NE, 4], I32)
        gateS = k.sb("gateS", [128, NE, 4], F32)
        k.push()
        affT = k.sb("affT", [16, NLAT], F32)
        for g8i in range(8):
            pb = PS[g8i % 2]
            for t4 in range(4):
                t = g8i * 4 + t4
                k.op("pe", lambda e: e.transpose(pb.t[0:16, t4 * 128:(t4 + 1) * 128], affall.t[:, t, :], identf.t[:]),
                     reads=[affall, identf], writes=[pb])
            k.op("act", lambda e: e.activation(out=affT.t[:, g8i * 512:(g8i + 1) * 512], in_=pb.t[0:16, :], func=AF.Copy),
                 reads=[pb], writes=[affT])
        bs = k.sb("bs", [16, 8], F32)
        junkT = k.sb("junkT", [16, NLAT], F32)
        k.op("dve", lambda e: e.memset(bs.t[:], 0.0), writes=[bs])
        k.op("dve", lambda e: e.memset(bs.t[:, 1:2], 1.0), reads=[bs], writes=[bs])
        for it in range(30):
            k.op("dve", lambda e: e.tensor_scalar(out=bs.t[:, 5:6], in0=bs.t[:, 1:2], scalar1=0.5, scalar2=None, op0=ALU.mult),
                 reads=[bs], writes=[bs])
            k.op("dve", lambda e: e.scalar_tensor_tensor(out=bs.t[:, 2:3], in0=bs.t[:, 0:1], scalar=0.5, in1=bs.t[:, 5:6],
                                                         op0=ALU.mult, op1=ALU.add), reads=[bs], writes=[bs])
            k.op("dve", lambda e: e.tensor_scalar(out=junkT.t[:], in0=affT.t[:], scalar1=bs.t[:, 2:3], scalar2=0.0, op0=ALU.is_ge,
                                                  op1=ALU.add, accum_out=bs.t[:, 3:4]), reads=[affT, bs], writes=[junkT, bs])
            k.op("dve", lambda e: e.tensor_scalar(out=bs.t[:, 4:5], in0=bs.t[:, 3:4], scalar1=float(CAP), scalar2=None,
                                                  op0=ALU.is_ge), reads=[bs], writes=[bs])
            k.op("dve", lambda e: e.tensor_tensor(out=bs.t[:, 5:6], in0=bs.t[:, 2:3], in1=bs.t[:, 0:1], op=ALU.subtract),
                 reads=[bs], writes=[bs])
            k.op("dve", lambda e: e.tensor_tensor(out=bs.t[:, 6:7], in0=bs.t[:, 1:2], in1=bs.t[:, 2:3], op=ALU.subtract),
                 reads=[bs], writes=[bs])
            k.op("dve", lambda e: e.scalar_tensor_tensor(out=bs.t[:, 0:1], in0=bs.t[:, 5:6], scalar=bs.t[:, 4:5], in1=bs.t[:, 0:1],
                                                         op0=ALU.mult, op1=ALU.add), reads=[bs], writes=[bs])
            k.op("dve", lambda e: e.scalar_tensor_tensor(out=bs.t[:, 1:2], in0=bs.t[:, 6:7], scalar=bs.t[:, 4:5], in1=bs.t[:, 2:3],
                                                         op0=ALU.mult, op1=ALU.add), reads=[bs], writes=[bs])
        Mt = k.sb("Mt", [16, NLAT], F32)
        k.op("dve", lambda e: e.tensor_scalar(out=Mt.t[:], in0=affT.t[:], scalar1=bs.t[:, 0:1], scalar2=None, op0=ALU.is_ge),
             reads=[affT, bs], writes=[Mt])
        k.op("dve", lambda e: e.memset(junkT.t[:], 1.0), writes=[junkT])
        cumM = k.sb("cumM", [16, NLAT], F32)
        k.op("dve", lambda e: e.tensor_tensor_scan(out=cumM.t[:], data0=junkT.t[:], data1=Mt.t[:], initial=0.0, op0=ALU.mult,
                                                   op1=ALU.add), reads=[junkT, Mt], writes=[cumM])
        k.op("dve", lambda e: e.scalar_tensor_tensor(out=cumM.t[:], in0=Mt.t[:], scalar=-8193.0, in1=cumM.t[:], op0=ALU.mult,
                                                     op1=ALU.add), reads=[Mt, cumM], writes=[cumM])
        k.op("dve", lambda e: e.tensor_scalar(out=cumM.t[:], in0=cumM.t[:], scalar1=8192.0, scalar2=None, op0=ALU.add),
             reads=[cumM], writes=[cumM])
        slotTM = k.sb("slotTM", [128, 32, NE], F32)
        pb = PS[2]
        for t in range(32):
            k.op("pe", lambda e: e.transpose(pb.t[:, t * 16:(t + 1) * 16], cumM.t[:, t * 128:(t + 1) * 128], identf.t[0:16, 0:16]),
                 reads=[cumM, identf], writes=[pb])
        k.op("act", lambda e: e.activation(out=slotTM.t[:].rearrange("p t e -> p (t e)"), in_=pb.t[:], func=AF.Copy),
             reads=[pb], writes=[slotTM])
        vals = k.sb("vals", [128, 32, NE, 4], F32)
        tokv = k.sb("tokv", [128, 32, 2], F32)
        k.dma("sp", tokv.t[:], c_tokv.ap, writes=[tokv])
        for e_ in range(NE):
            k.op("pool", lambda e: e.tensor_copy(out=vals.t[:, :, e_, 0:2], in_=tokv.t[:]), reads=[tokv], writes=[vals])
        k.op("pool", lambda e: e.tensor_copy(out=vals.t[:, :, :, 2], in_=affall.t[:]), reads=[affall], writes=[vals])
        k.op("pool", lambda e: e.tensor_copy(out=vals.t[:, :, :, 3], in_=affall.t[:]), reads=[affall], writes=[vals])
        iot = k.sb("iot", [128, 512], F32)
        k.dma("sp", iot.t[:], c_iota.ap, writes=[iot])
        Sel = [k.sb("Sel%d" % i, [128, 512], F32) for i in range(3)]
        res = k.sb("resF", [128, 4, 4], F32)
        idf = k.sb("idf", [128, 4], F32)
        csel = 0
        for e_ in range(NE):
            for t in range(32):
                sl = Sel[csel % 3]
                csel += 1
                k.op("dve", lambda e: e.tensor_scalar(out=sl.t[:], in0=iot.t[:], scalar1=slotTM.t[:, t, e_:e_ + 1], scalar2=None,
                                                      op0=ALU.is_equal), reads=[iot, slotTM], writes=[sl])
                for st in range(4):
                    k.op("pe", lambda e: e.matmul(PS[4 + st].t[:, 0:4], sl.t[:, st * 128:(st + 1) * 128], vals.t[:, t, e_, :],
                                                  start=(t == 0), stop=(t == 31)), reads=[sl, vals], writes=[PS[4 + st]])
            for st in range(4):
                k.op("act", lambda e: e.activation(out=res.t[:, st, :], in_=PS[4 + st].t[:, 0:4], func=AF.Copy),
                     reads=[PS[4 + st]], writes=[res])
            k.op("dve", lambda e: e.scalar_tensor_tensor(out=idf.t[:], in0=res.t[:, :, 0], scalar=64.0, in1=res.t[:, :, 1],
                                                         op0=ALU.mult, op1=ALU.add), reads=[res], writes=[idf])
            k.op("dve", lambda e: e.tensor_copy(out=idxI.t[:, e_, :], in_=idf.t[:]), reads=[idf], writes=[idxI])
            k.op("dve", lambda e: e.tensor_copy(out=gateS.t[:, e_, :], in_=res.t[:, :, 2]), reads=[res], writes=[gateS])
        k.pop()
        if DEBUG and "IDXD" in DEBUG_NAMES:
            k.dma("sp", IDXD, idxI.t[:], reads=[idxI], writes=[bAFF])
            k.dma("sp", GATED, gateS.t[:], reads=[gateS], writes=[bAFF])
        if STOP_AFTER == "F":
            return finish(nc, k, out)

        k.push()
        zt = k.sb("zt", [128, D], F32)
        k.op("pool", lambda e: e.memset(zt.t[:], 0.0), writes=[zt])
        for t in range(32):
            k.dma("sp", FACC[t * 128:(t + 1) * 128, :], zt.t[:], reads=[zt], writes=[bFACC])
        xsT = k.sb("xsT", [128, 16, 512], F32R)
        hidT = k.sb("hidT", [128, 16, 512], F32R)
        gsl = [k.sb("gsl%d" % i, [128, 16, 512], F32R) for i in range(2)]
        xg = k.sb("xg", [128, D], F32)
        yst = [k.sb("yst%d" % i, [128, D], F32) for i in range(4)]
        s1 = k.sb("s1", [128, 4, 512], F32)
        t3 = [k.sb("t3_%d" % i, [128, 512], F32) for i in range(2)]
        cg = [0]
        cpp = [0]
        NEX = NE if STOP_AFTER != "G1" else 1

        def prep_slot_tile(e_, st):
            k.idma(lambda e: e.indirect_dma_start(out=xg.t[:], out_offset=None, in_=X1,
                                                  in_offset=bass.IndirectOffsetOnAxis(ap=idxI.t[:, e_, st:st + 1], axis=0)),
                   reads=[bX1, idxI], writes=[xg])
            for c in range(16):
                pb = PS[c // 4]
                k.op("pe", lambda e: e.transpose(pb.t[:, (c % 4) * 128:(c % 4 + 1) * 128], xg.t[:, c * 128:(c + 1) * 128],
                                                 identf.t[:]), reads=[xg, identf], writes=[pb])
            for c in range(16):
                pb = PS[c // 4]
                k.op("act", lambda e: e.activation(out=xsT.t[:, c, st * 128:(st + 1) * 128],
                                                   in_=pb.t[:, (c % 4) * 128:(c % 4 + 1) * 128], func=AF.Identity,
                                                   scale=modT.t[:, 0, 64 + c:65 + c], bias=modT.t[:, 0, 48 + c:49 + c]),
                     reads=[pb, modT], writes=[xsT])

        def load_slab(wsrc, e_, cb):
            sl = gsl[cg[0] % 2]
            cg[0] += 1
            k.dma("pool", sl.t[:], wsrc.ap[e_, :, cb * 512:(cb + 1) * 512].rearrange("(c p) f -> p c f", p=128), writes=[sl])
            return sl

        for st in range(4):
            prep_slot_tile(0, st)
        pre = [load_slab(w1, 0, 0), load_slab(w3, 0, 0)]
        for e_ in range(NEX):
            for fb in range(4):
                for wi, wsrc in enumerate((w1, w3)):
                    if fb == 0:
                        sl = pre[wi]
                    else:
                        sl = load_slab(wsrc, e_, fb)
                    for fc in range(4):
                        pa = PS[4 + cpp[0] % 4]
                        cpp[0] += 1
                        for c in range(16):
                            k.op("pe", lambda e: e.matmul(pa.t[:], sl.t[:, c, fc * 128:(fc + 1) * 128], xsT.t[:, c, :],
                                                          start=(c == 0), stop=(c == 15)), reads=[sl, xsT], writes=[pa])
                        if wi == 0:
                            k.op("act", lambda e: e.activation(out=s1.t[:, fc, :], in_=pa.t[:], func=AF.Silu), reads=[pa], writes=[s1])
                        else:
                            tt = t3[fc % 2]
                            k.op("act", lambda e: e.activation(out=tt.t[:], in_=pa.t[:], func=AF.Copy), reads=[pa], writes=[tt])
                            k.op("dve", lambda e: e.tensor_tensor(out=hidT.t[:, fb * 4 + fc, :], in0=tt.t[:], in1=s1.t[:, fc, :],
                                                                  op=ALU.mult), reads=[tt, s1], writes=[hidT])
            for db in range(4):
                sl = load_slab(w2, e_, db)
                for st in range(4):
                    pa = PS[4 + cpp[0] % 4]
                    cpp[0] += 1
                    for c in range(16):
                        k.op("pe", lambda e: e.matmul(pa.t[:], hidT.t[:, c, st * 128:(st + 1) * 128], sl.t[:, c, :],
                                                      start=(c == 0), stop=(c == 15)), reads=[sl, hidT], writes=[pa])
                    k.op("act", lambda e: e.activation(out=yst[st].t[:, db * 512:(db + 1) * 512], in_=pa.t[:], func=AF.Copy,
                                                       scale=gateS.t[:, e_, st:st + 1]), reads=[pa, gateS], writes=[yst[st]])
                if e_ + 1 < NEX:
                    prep_slot_tile(e_ + 1, db)
            if e_ + 1 < NEX:
                pre = [load_slab(w1, e_ + 1, 0), load_slab(w3, e_ + 1, 0)]
            for st in range(4):
                k.idma(lambda e: e.indirect_dma_start(out=FACC, out_offset=bass.IndirectOffsetOnAxis(ap=idxI.t[:, e_, st:st + 1], axis=0),
                                                      in_=yst[st].t[:], in_offset=None, compute_op=ALU.add),
                       reads=[yst[st], idxI, bFACCs, bFACC], writes=[bFACCs])
        k.pop()
        if STOP_AFTER in ("G", "G1"):
            return finish(nc, k, out)

        k.push()
        g2bc = bcast_tile("g2bc", MOD[0:1, 5 * D:6 * D])
        l2g = bcast_tile("l2g", ln2_g.ap)
        l2b = bcast_tile("l2b", ln2_b.ap)
        xh = [k.sb("xh%d" % i, [128, D], F32) for i in range(2)]
        fh = [k.sb("fh%d" % i, [128, D], F32) for i in range(2)]
        oh = [k.sb("oh%d" % i, [128, D], F32) for i in range(2)]
        junkH = k.sb("junkH", [128, D], F32)
        stH = k.sb("stH", [128, 8], F32)
        for t in range(32):
            b = t % 2
            k.dma("sp", xh[b].t[:], X1[t * 128:(t + 1) * 128, :], reads=[bX1], writes=[xh[b]])
            k.dma("sp", fh[b].t[:], FACC[t * 128:(t + 1) * 128, :], reads=[bFACC, bFACCs], writes=[fh[b]])
            k.op("dve", lambda e: e.tensor_tensor(out=fh[b].t[:], in0=fh[b].t[:], in1=g2bc.t[:], op=ALU.mult),
                 reads=[fh[b], g2bc], writes=[fh[b]])
            k.op("dve", lambda e: e.scalar_tensor_tensor(out=xh[b].t[:], in0=xh[b].t[:], scalar=ALPHA, in1=fh[b].t[:],
                                                         op0=ALU.mult, op1=ALU.add), reads=[xh[b], fh[b]], writes=[xh[b]])
            layer_norm(xh[b], l2g, l2b, oh[b], stH, junkH)
            k.dma("sp", out[t * 128:(t + 1) * 128, :], oh[b].t[:], reads=[oh[b]], writes=[bOUT])
        k.pop()

        finish(nc, k, out)
    return nc


def finish(nc, k, out):
    k.barrier(["sp"])
    return nc


_PROGRAM = None


def host_constants():
    import ml_dtypes
    c = {}
    c["c_identf"] = np.eye(128, dtype=np.float32)
    c["c_identb"] = np.eye(128, dtype=np.float32).astype(ml_dtypes.bfloat16)
    perm = np.zeros((128, 128), np.float32)
    for m in range(128):
        s = m + 32 if (m % 64) < 32 else m - 32
        perm[s, m] = 1.0
    c["c_perm"] = perm.astype(ml_dtypes.bfloat16)
    n = np.arange(NLAT)
    row = (n // 64).astype(np.float32)
    col = (n % 64).astype(np.float32)
    inv = (np.float32(10000.0) ** (-np.arange(16, dtype=np.float32) / np.float32(16))).astype(np.float32)
    ang = np.concatenate([row[:, None] * inv[None, :], col[:, None] * inv[None, :]], axis=-1).astype(np.float32)
    cs = np.cos(ang).astype(np.float32).T
    sn = np.sin(ang).astype(np.float32).T
    c["c_ropec"] = np.ascontiguousarray(np.concatenate([cs, cs, cs, cs], axis=0))
    c["c_ropes"] = np.ascontiguousarray(np.concatenate([-sn, sn, -sn, sn], axis=0))
    rs = np.ones((128, 512), np.float32)
    rs[:, ::128] = 0.0
    c["c_reset"] = rs
    j = np.arange(128)[:, None]
    i = np.arange(128)[None, :]
    c["c_maskf"] = (j <= i).astype(np.float32).astype(ml_dtypes.bfloat16)
    c["c_maskb"] = (j >= i).astype(np.float32).astype(ml_dtypes.bfloat16)
    c["c_iota"] = np.tile(np.arange(512, dtype=np.float32)[None, :], (128, 1))
    tok = (np.arange(32)[None, :] * 128 + np.arange(128)[:, None])
    c["c_tokv"] = np.stack([(tok // 64).astype(np.float32), (tok % 64).astype(np.float32)], axis=-1)
    return c


def make_in_maps(inp):
    consts = host_constants()
    f = lambda a: np.ascontiguousarray(np.asarray(a, dtype=np.float32))
    shared = {
        "w_ada": f(inp["w_ada"][0]), "b_ada": f(inp["b_ada"][0]).reshape(1, -1), "w_in": f(inp["w_in"][0]),
        "w_gate2": f(inp["w_gate2"][0]), "b_gate": np.ascontiguousarray(f(inp["b_gate"][0]).reshape(2, 4, 128).transpose(2, 0, 1)),
        "gla_norm_g": f(inp["gla_norm_g"][0]).reshape(1, -1), "diff_lambda": f(inp["diff_lambda"][0]).reshape(1, -1),
        "diff_norm_g": f(inp["diff_norm_g"][0]).reshape(1, -1), "w_o": f(inp["w_o"][0]),
        "ln1_g": f(inp["ln1_g"][0]).reshape(1, -1), "ln1_b": f(inp["ln1_b"][0]).reshape(1, -1),
        "w_router": f(inp["w_router"][0]), "w1": f(inp["w1"][0]), "w3": f(inp["w3"][0]), "w2": f(inp["w2"][0]),
        "ln2_g": f(inp["ln2_g"][0]).reshape(1, -1), "ln2_b": f(inp["ln2_b"][0]).reshape(1, -1),
    }
    shared.update(consts)
    maps = []
    for core in range(8):
        b = core % 4
        m = dict(shared)
        m["x"] = f(inp["x"][b])
        m["ctx"] = f(inp["ctx"][b])
        m["cvec"] = np.ascontiguousarray(np.stack([np.asarray(inp["c"][b], np.float32), np.asarray(inp["c_ctx"], np.float32)]))
        maps.append(m)
    return maps


def kernel(**inputs):
    nc = build_program()
    in_maps = make_in_maps(inputs)
    in_maps = [{kk: v for kk, v in m.items() if kk in nc.used_inputs} for m in in_maps]
    res = run_bass_kernel_spmd(nc, in_maps, core_ids=list(range(8)))
    outs = [np.asarray(res.results[b]["out"]) for b in range(4)]
    return np.stack(outs, axis=0).astype(np.float32)
```

```python
import math
from contextlib import ExitStack

import numpy as np
import concourse.bass as bass
import concourse.mybir as mybir
from concourse.bass_utils import run_bass_kernel_spmd

F32 = mybir.dt.float32
F32R = mybir.dt.float32r
BF16 = mybir.dt.bfloat16
I32 = mybir.dt.int32
AF = mybir.ActivationFunctionType
ALU = mybir.AluOpType
AX = mybir.AxisListType

D = 2048
NLAT = 4096
NCTX = 256
NTOK = NLAT + NCTX
DIN = 6176
NE = 16
CAP = 512
ALPHA = 2.0 ** 0.25
LAM_INIT = 0.2
NDSEM = 80

STOP_AFTER = "Z"
DEBUG = False
DEBUG_NAMES = ()
B_KINDS = None
ROPE_STAGE = 9
C_HEADS = 8
N_FILL = 0


class Buf:
    def __init__(self, t=None, waw=True):
        self.t = t
        self.w = {}
        self.r = {}
        self.waw = waw


class K:
    def __init__(self, nc, es):
        self.nc = nc
        self.es = es
        self.E = {"pe": nc.tensor, "act": nc.scalar, "dve": nc.vector, "pool": nc.gpsimd, "sp": nc.sync}
        self.sem = {e: es.enter_context(nc.semaphore("s_" + e)) for e in self.E}
        self.seq = {e: 0 for e in self.E}
        self.known = {e: {} for e in self.E}
        self.dsem = [es.enter_context(nc.semaphore("d%d" % i)) for i in range(NDSEM)]
        self.duse = [0] * NDSEM
        self.dcount = 0
        self.dcount_sw = 0
        self.scopes = []
        self.n = 0

    def push(self):
        self.scopes.append(ExitStack())

    def pop(self):
        self.barrier()
        self.scopes.pop().close()

    def sb(self, name, shape, dtype):
        st = self.scopes[-1] if self.scopes else self.es
        return Buf(st.enter_context(self.nc.sbuf_tensor(name, list(shape), dtype)))

    def ps(self, name, shape, dtype):
        st = self.scopes[-1] if self.scopes else self.es
        return Buf(st.enter_context(self.nc.psum_tensor(name, list(shape), dtype)))

    def _semobj(self, key):
        return self.sem[key[1]] if key[0] == "e" else self.dsem[key[1]]

    def _wait(self, eng, key, val):
        if self.known[eng].get(key, 0) >= val:
            return
        self.E[eng].wait_ge(self._semobj(key), val)
        self.known[eng][key] = val

    def _deps(self, eng, reads, writes):
        for b in reads:
            for kk, v in b.w.items():
                if eng == "pe" and kk == ("e", "pe"):
                    continue
                self._wait(eng, kk, v)
        for b in writes:
            if b.waw:
                for kk, v in b.w.items():
                    if eng == "pe" and kk == ("e", "pe"):
                        continue
                    self._wait(eng, kk, v)
            for kk, v in b.r.items():
                if eng == "pe" and kk == ("e", "pe"):
                    continue
                self._wait(eng, kk, v)

    def _commit(self, key, val, reads, writes):
        for b in reads:
            b.r[key] = max(b.r.get(key, 0), val)
        for b in writes:
            if b.waw:
                b.w = {key: val}
                b.r = {}
            else:
                b.w[key] = max(b.w.get(key, 0), val)

    def op(self, eng, fn, reads=(), writes=()):
        self._deps(eng, reads, writes)
        inst = fn(self.E[eng])
        self.seq[eng] += 1
        inst.then_inc(self.sem[eng], 1)
        self._commit(("e", eng), self.seq[eng], reads, writes)
        self.n += 1

    def _dslot(self, q):
        half = NDSEM // 2
        if q == "pool":
            i = half + self.dcount_sw % half
            self.dcount_sw += 1
        else:
            i = self.dcount % half
            self.dcount += 1
        if self.duse[i] > 0:
            self._wait(q, ("d", i), self.duse[i] * 16)
        return i

    def dma(self, q, out, in_, reads=(), writes=(), **kw):
        self._deps(q, reads, writes)
        i = self._dslot(q)
        inst = self.E[q].dma_start(out=out, in_=in_, **kw)
        self.duse[i] += 1
        inst.then_inc(self.dsem[i], 16)
        self._commit(("d", i), self.duse[i] * 16, reads, writes)
        self.n += 1

    def idma(self, fn, reads=(), writes=()):
        q = "pool"
        self._deps(q, reads, writes)
        i = self._dslot(q)
        inst = fn(self.E[q])
        self.duse[i] += 1
        inst.then_inc(self.dsem[i], 16)
        self._commit(("d", i), self.duse[i] * 16, reads, writes)
        self.n += 1

    def barrier(self, engines=None):
        for e in (engines or list(self.E)):
            for f in self.E:
                if self.seq[f] > 0 and not (e == f == "pe"):
                    self._wait(e, ("e", f), self.seq[f])
            for i in range(NDSEM):
                if self.duse[i] > 0:
                    self._wait(e, ("d", i), self.duse[i] * 16)


def build_program():
    nc = bass.Bass("TRN2", target_bir_lowering=False)

    used_inputs = []

    class Lazy:
        def __init__(self, name, shape, dt=F32):
            self.name, self.shape, self.dt, self._ap = name, list(shape), dt, None

        @property
        def ap(self):
            if self._ap is None:
                self._ap = nc.dram_tensor(self.name, self.shape, self.dt, kind="ExternalInput").ap()
                used_inputs.append(self.name)
            return self._ap

    def din(name, shape, dt=F32):
        return Lazy(name, shape, dt)

    def dscr(name, shape, dt=F32):
        kind = "ExternalOutput" if (DEBUG and name in DEBUG_NAMES) else "Internal"
        return nc.dram_tensor(name, list(shape), dt, kind=kind).ap()

    x = din("x", [NLAT, D])
    ctx = din("ctx", [NCTX, D])
    cvec = din("cvec", [2, D])
    w_ada = din("w_ada", [D, 6 * D])
    b_ada = din("b_ada", [1, 6 * D])
    w_in = din("w_in", [D, DIN])
    w_gate2 = din("w_gate2", [2, 16, 512])
    b_gate = din("b_gate", [128, 2, 4])
    gla_norm_g = din("gla_norm_g", [1, 256])
    diff_lambda = din("diff_lambda", [1, 256])
    diff_norm_g = din("diff_norm_g", [1, 128])
    w_o = din("w_o", [D, D])
    ln1_g = din("ln1_g", [1, D])
    ln1_b = din("ln1_b", [1, D])
    w_router = din("w_router", [D, NE])
    w1 = din("w1", [NE, D, D])
    w3 = din("w3", [NE, D, D])
    w2 = din("w2", [NE, D, D])
    ln2_g = din("ln2_g", [1, D])
    ln2_b = din("ln2_b", [1, D])
    c_identf = din("c_identf", [128, 128])
    c_identb = din("c_identb", [128, 128], BF16)
    c_perm = din("c_perm", [128, 128], BF16)
    c_ropec = din("c_ropec", [128, NLAT])
    c_ropes = din("c_ropes", [128, NLAT])
    c_reset = din("c_reset", [128, 512])
    c_maskf = din("c_maskf", [128, 128], BF16)
    c_maskb = din("c_maskb", [128, 128], BF16)
    c_iota = din("c_iota", [128, 512])
    c_tokv = din("c_tokv", [128, 32, 2])
    out = nc.dram_tensor("out", [NLAT, D], F32, kind="ExternalOutput").ap()
    nc.used_inputs = used_inputs

    MOD = dscr("MOD", [2, 6 * D])
    AT = dscr("AT", [2, 4, 128, NTOK], BF16)
    BT = dscr("BT", [2, 4, 128, NTOK], BF16)
    ETOT = dscr("ETOT", [2, 128, 4, 34])
    VG = dscr("VG", [NTOK, 1024], BF16)
    RG = dscr("RG", [NLAT, 1024])
    QDT = dscr("QDT", [8, 128, NLAT], BF16)
    KDT = dscr("KDT", [8, 128, NTOK], BF16)
    VD = dscr("VD", [NTOK, 1024], BF16)
    OFS = dscr("OFS", [NLAT, 1024])
    AL = dscr("AL", [NLAT, D])
    X1 = dscr("X1", [NLAT, D])
    H = dscr("H", [NLAT, D])
    AFF = dscr("AFF", [NLAT, NE])
    FACC = dscr("FACC", [NLAT, D])
    ALT = dscr("ALT", [1024, NLAT])
    IDXD = dscr("IDXD", [128, NE, 4], I32)
    GATED = dscr("GATED", [128, NE, 4])
    bMOD, bAT, bBT, bETOT, bVG, bRG, bQDT, bKDT, bVD, bOFS, bAL, bX1, bH, bAFF, bFACC, bOUT = [
        Buf(None, waw=False) for _ in range(16)]
    bALT = Buf(None, waw=False)
    bFACCs = Buf(None, waw=True)

    with ExitStack() as es:
        k = K(nc, es)
        PS = [k.ps("psb%d" % i, [128, 512], F32) for i in range(8)]
        identf = k.sb("identf", [128, 128], F32)
        identb = k.sb("identb", [128, 128], BF16)
        k.dma("sp", identf.t[:], c_identf.ap, writes=[identf])
        k.dma("sp", identb.t[:], c_identb.ap, writes=[identb])
        modT = k.sb("modT", [128, 2, 96], F32)

        k.push()
        cT = k.sb("cT", [128, 16, 128], F32)
        k.op("pool", lambda e: e.memset(cT.t[:], 0.0), writes=[cT])
        with nc.allow_non_contiguous_dma(reason="tiny transposed vector load"):
            for r in range(2):
                k.dma("sp", cT.t[:, :, r], cvec.ap[r, :].rearrange("(c p) -> p c", p=128), writes=[cT])
        scT = k.sb("scT", [128, 16, 128], F32R)
        k.op("act", lambda e: e.activation(out=scT.t[:], in_=cT.t[:], func=AF.Silu), reads=[cT], writes=[scT])
        bada2 = k.sb("bada2", [2, 6 * D], F32)
        k.dma("sp", bada2.t[:], b_ada.ap.partition_broadcast(2), writes=[bada2])
        mod_sb = k.sb("mod_sb", [2, 6 * D], F32)
        slabs = [k.sb("aslab%d" % i, [128, 16, 512], F32R) for i in range(2)]
        for blk in range(24):
            slab = slabs[blk % 2]
            k.dma("pool", slab.t[:], w_ada.ap[:, blk * 512:(blk + 1) * 512].rearrange("(c p) f -> p c f", p=128),
                  writes=[slab])
            ps = PS[blk % 2]
            for c in range(16):
                k.op("pe", lambda e: e.matmul(ps.t[:], scT.t[:, c, :], slab.t[:, c, :], start=(c == 0), stop=(c == 15)),
                     reads=[scT, slab], writes=[ps])
            k.op("dve", lambda e: e.tensor_tensor(out=mod_sb.t[:, blk * 512:(blk + 1) * 512], in0=ps.t[0:2, :],
                                                  in1=bada2.t[:, blk * 512:(blk + 1) * 512], op=ALU.add),
                 reads=[ps, bada2], writes=[mod_sb])
        k.dma("sp", MOD, mod_sb.t[:], reads=[mod_sb], writes=[bMOD])
        with nc.allow_non_contiguous_dma(reason="small transposed reload of modulation vectors"):
            k.dma("sp", modT.t[:], MOD.rearrange("r (j p) -> p r j", p=128), reads=[bMOD], writes=[modT])
        for j0 in (16, 64):
            k.op("dve", lambda e: e.tensor_scalar(out=modT.t[:, :, j0:j0 + 16], in0=modT.t[:, :, j0:j0 + 16],
                                                  scalar1=1.0, scalar2=None, op0=ALU.add),
                 reads=[modT], writes=[modT])
        k.pop()
        if STOP_AFTER == "A":
            return finish(nc, k, out)


        k.push()
        Cblk = k.sb("Cblk", [128, 512], F32)
        Sblk = k.sb("Sblk", [128, 512], F32)
        qraw = [k.sb("qraw%d" % i, [128, 512], BF16) for i in range(2)]
        uT = k.sb("uT", [128, 16, 512], F32R)
        xt = [k.sb("xt%d" % i, [128, D], F32) for i in range(2)]
        slabs = [k.sb("bslab%d" % i, [128, 16, 512], F32R) for i in range(2)]
        wg2 = k.sb("wg2", [48, 512], F32)
        k.dma("sp", wg2.t[0:16, :], w_gate2.ap[0], writes=[wg2])
        k.dma("sp", wg2.t[32:48, :], w_gate2.ap[1], writes=[wg2])
        negbg = k.sb("negbg", [128, 2, 4], F32)
        k.dma("sp", negbg.t[:], b_gate.ap, writes=[negbg])
        k.op("dve", lambda e: e.tensor_scalar(out=negbg.t[:], in0=negbg.t[:], scalar1=-1.0, scalar2=None, op0=ALU.mult),
             reads=[negbg], writes=[negbg])
        perm = k.sb("perm", [128, 128], BF16)
        k.dma("sp", perm.t[:], c_perm.ap, writes=[perm])
        resetm = k.sb("resetm", [128, 512], F32)
        k.dma("sp", resetm.t[:], c_reset.ap, writes=[resetm])
        lrT = k.sb("lrT", [48, 512], F32)
        zf = xt[0]
        k.op("pool", lambda e: e.memset(zf.t[:], 0.0), writes=[zf])
        lrslab = k.sb("lrslab", [128, 16, 128], F32R)
        k.op("act", lambda e: e.activation(out=lrslab.t[:].rearrange("p c f -> p (c f)"), in_=zf.t[:], func=AF.Copy),
             reads=[zf], writes=[lrslab])
        with nc.allow_non_contiguous_dma(reason="small low-rank gate columns"):
            for d in range(2):
                k.dma("pool", lrslab.t[:, :, d * 32:d * 32 + 16],
                      w_in.ap[:, 3072 + d * 16:3072 + d * 16 + 16].rearrange("(c p) f -> p c f", p=128), writes=[lrslab])
        EA = k.sb("EA", [128, 2, 4, 512], BF16)
        EB = k.sb("EB", [128, 2, 4, 512], BF16)
        ABst = k.sb("ABst", [128, 2, 2, 4, 512], BF16)
        etst = k.sb("etst", [128, 2, 4, 4], F32)
        tmpE = [k.sb("tmpE%d" % i, [128, 512], F32) for i in range(2)]
        spt = [k.sb("spt%d" % i, [128, 512], F32) for i in range(2)]
        cumt = [k.sb("cumt%d" % i, [128, 512], F32) for i in range(2)]
        argt = [k.sb("argt%d" % i, [128, 512], F32) for i in range(2)]
        Vst = [k.sb("Vst%d" % i, [128, 4, 512], BF16) for i in range(2)]
        Rst = [k.sb("Rst0", [128, 4, 512], F32)] * 2
        QKst = [k.sb("QKst%d" % i, [128, 4, 512], BF16) for i in range(2)]
        t1 = tmpE
        t2 = spt
        SLABS = [("glr", 3072, 32), ("gq", 0, 512), ("gk", 512, 512), ("gv", 1024, 512), ("gv", 1536, 512),
                 ("gr", 2048, 512), ("gr", 2560, 512), ("dq", 3104, 512), ("dq", 3616, 512),
                 ("dk", 4128, 512), ("dk", 4640, 512), ("dv", 5152, 512), ("dv", 5664, 512)]
        cnt = {"x": 0, "slab": 0, "acc": 0, "g": 0, "st": 0, "r": 0}
        NBLK = 9 if STOP_AFTER != "B1" else 2
        for tb in range(NBLK):
            isctx = tb == 0
            NT = 256 if isctx else 512
            tok0 = 0 if isctx else 256 + (tb - 1) * 512
            lat0 = 0 if isctx else (tb - 1) * 512
            mr = 1 if isctx else 0
            src = ctx.ap if isctx else x.ap[lat0:lat0 + 512, :]
            nt = NT // 128
            for t in range(nt):
                xb = xt[cnt["x"] % 2]
                cnt["x"] += 1
                k.dma("sp", xb.t[:], src[t * 128:(t + 1) * 128, :], writes=[xb])
                for c in range(16):
                    pb = PS[c // 4]
                    k.op("pe", lambda e: e.transpose(pb.t[:, (c % 4) * 128:(c % 4 + 1) * 128], xb.t[:, c * 128:(c + 1) * 128],
                                                     identf.t[:]), reads=[xb, identf], writes=[pb])
                for c in range(16):
                    pb = PS[c // 4]
                    k.op("act", lambda e: e.activation(out=uT.t[:, c, t * 128:(t + 1) * 128],
                                                       in_=pb.t[:, (c % 4) * 128:(c % 4 + 1) * 128], func=AF.Identity,
                                                       scale=modT.t[:, mr, 16 + c:17 + c], bias=modT.t[:, mr, c:c + 1]),
                         reads=[pb, modT], writes=[uT])
            if not isctx:
                k.dma("sp", Cblk.t[:], c_ropec.ap[:, lat0:lat0 + 512], writes=[Cblk])
                k.dma("sp", Sblk.t[:], c_ropes.ap[:, lat0:lat0 + 512], writes=[Sblk])
            half = {}
            for (kind, c0, ncols) in SLABS:
                hf = half.get(kind, 0)
                half[kind] = hf + 1
                if isctx and kind in ("gr", "dq"):
                    continue
                if B_KINDS is not None and kind not in B_KINDS:
                    continue
                if kind != "glr":
                    slab = slabs[cnt["slab"] % 2]
                    cnt["slab"] += 1
                if kind == "glr":
                    slab = lrslab
                else:
                    k.dma("pool", slab.t[:], w_in.ap[:, c0:c0 + 512].rearrange("(c p) f -> p c f", p=128), writes=[slab])

                def acc_feat(j):
                    pa = PS[4 + cnt["acc"] % 2]
                    cnt["acc"] += 1
                    for c in range(16):
                        k.op("pe", lambda e: e.matmul(pa.t[:, :NT], slab.t[:, c, j * 128:(j + 1) * 128], uT.t[:, c, :NT],
                                                      start=(c == 0), stop=(c == 15)), reads=[slab, uT], writes=[pa])
                    return pa

                def acc_tok(t):
                    pa = PS[4 + cnt["acc"] % 2]
                    cnt["acc"] += 1
                    for c in range(16):
                        k.op("pe", lambda e: e.matmul(pa.t[:], uT.t[:, c, t * 128:(t + 1) * 128], slab.t[:, c, :],
                                                      start=(c == 0), stop=(c == 15)), reads=[slab, uT], writes=[pa])
                    return pa

                if kind == "glr":
                    pa = acc_feat(0)
                    k.op("act", lambda e: e.activation(out=lrT.t[:, :NT], in_=pa.t[0:48, :NT], func=AF.Copy),
                         reads=[pa], writes=[lrT])
                    nch = NT // 128
                    for d in range(2):
                        for j in range(4):
                            g = cnt["g"] % 2
                            cnt["g"] += 1
                            pz = PS[6 + g]
                            k.op("pe", lambda e: e.matmul(pz.t[:, :NT], wg2.t[d * 32:d * 32 + 16, j * 128:(j + 1) * 128],
                                                          lrT.t[d * 32:d * 32 + 16, :NT], start=True, stop=True),
                                 reads=[wg2, lrT], writes=[pz])
                            k.op("act", lambda e: e.activation(out=tmpE[g].t[:, :NT], in_=pz.t[:, :NT], func=AF.Exp,
                                                               scale=-1.0, bias=negbg.t[:, d, j:j + 1]),
                                 reads=[pz, negbg], writes=[tmpE[g]])
                            k.op("act", lambda e: e.activation(out=spt[g].t[:, :NT], in_=tmpE[g].t[:, :NT], func=AF.Ln,
                                                               scale=1.0, bias=1.0), reads=[tmpE[g]], writes=[spt[g]])
                            k.op("dve", lambda e: e.tensor_tensor_scan(out=cumt[g].t[:, :NT], data0=resetm.t[:, :NT],
                                                                       data1=spt[g].t[:, :NT], initial=0.0, op0=ALU.mult,
                                                                       op1=ALU.add), reads=[resetm, spt[g]], writes=[cumt[g]])
                            if d == 0:
                                arg = cumt[g]
                            else:
                                arg = argt[g]
                                k.op("dve", lambda e: e.tensor_tensor(out=arg.t[:, :NT], in0=cumt[g].t[:, :NT],
                                                                      in1=spt[g].t[:, :NT], op=ALU.subtract),
                                     reads=[cumt[g], spt[g]], writes=[arg])
                            sa = -1.0 / 16 if d == 0 else 1.0 / 16
                            k.op("act", lambda e: e.activation(out=EA.t[:, d, j, :NT], in_=arg.t[:, :NT], func=AF.Exp, scale=sa),
                                 reads=[arg], writes=[EA])
                            k.op("act", lambda e: e.activation(out=EB.t[:, d, j, :NT], in_=arg.t[:, :NT], func=AF.Exp, scale=-sa),
                                 reads=[arg], writes=[EB])
                            k.op("act", lambda e: e.activation(out=etst.t[:, d, j, 0:nch], in_=cumt[g].t[:, 127:NT:128],
                                                               func=AF.Exp, scale=-1.0 / 16), reads=[cumt[g]], writes=[etst])
                    ch0 = tok0 // 128
                    with nc.allow_non_contiguous_dma(reason="tiny per-chunk decay totals"):
                        for d in range(2):
                            k.dma("sp", ETOT[d, :, :, ch0:ch0 + nch], etst.t[:, d, :, 0:nch], reads=[etst], writes=[bETOT])
                elif kind in ("gq", "gk"):
                    isq = kind == "gq"
                    for j in range(4):
                        pa = acc_feat(j)
                        for d in range(2):
                            if isq:
                                k.op("dve", lambda e: e.scalar_tensor_tensor(out=ABst.t[:, 0, d, j, :NT], in0=pa.t[:, :NT],
                                                                             scalar=128.0 ** -0.5, in1=EA.t[:, d, j, :NT],
                                                                             op0=ALU.mult, op1=ALU.mult),
                                     reads=[pa, EA], writes=[ABst])
                            else:
                                k.op("dve", lambda e: e.tensor_tensor(out=ABst.t[:, 1, d, j, :NT], in0=pa.t[:, :NT],
                                                                      in1=EB.t[:, d, j, :NT], op=ALU.mult),
                                     reads=[pa, EB], writes=[ABst])
                    dst = AT if isq else BT
                    k.dma("sp", dst.rearrange("d j p n -> p d j n")[:, :, :, tok0:tok0 + NT],
                          ABst.t[:, 0 if isq else 1, :, :, :NT], reads=[ABst], writes=[bAT if isq else bBT])
                elif kind in ("gv", "dv", "gr"):
                    if kind == "gr":
                        st = Rst[cnt["r"] % 2]
                        cnt["r"] += 1
                    else:
                        st = Vst[cnt["st"] % 2]
                        cnt["st"] += 1
                    for t in range(nt):
                        pa = acc_tok(t)
                        fn = AF.Silu if kind == "gr" else AF.Copy
                        k.op("act", lambda e: e.activation(out=st.t[:, t, :], in_=pa.t[:], func=fn), reads=[pa], writes=[st])
                    if kind == "gr":
                        k.dma("sp", RG[lat0:lat0 + 512, hf * 512:(hf + 1) * 512].rearrange("(t p) f -> p t f", p=128),
                              st.t[:, 0:nt, :], reads=[st], writes=[bRG])
                    else:
                        dst, bd = (VG, bVG) if kind == "gv" else (VD, bVD)
                        k.dma("sp", dst[tok0:tok0 + NT, hf * 512:(hf + 1) * 512].rearrange("(t p) f -> p t f", p=128),
                              st.t[:, 0:nt, :], reads=[st], writes=[bd])
                elif kind in ("dq", "dk"):
                    st = QKst[cnt["st"] % 2]
                    cnt["st"] += 1
                    sc = 0.125 if kind == "dq" else 1.0
                    for jj in range(4):
                        pa = acc_feat(jj)
                        if isctx:
                            k.op("act", lambda e: e.activation(out=st.t[:, jj, :NT], in_=pa.t[:, :NT], func=AF.Copy),
                                 reads=[pa], writes=[st])
                            continue
                        g = cnt["g"] % 2
                        cnt["g"] += 1
                        k.op("act", lambda e: e.activation(out=qraw[g].t[:], in_=pa.t[:], func=AF.Copy), reads=[pa], writes=[qraw[g]])
                        pz = PS[6 + g]
                        if ROPE_STAGE >= 1:
                            k.op("pe", lambda e: e.matmul(pz.t[:], perm.t[:], qraw[g].t[:], start=True, stop=True),
                                 reads=[perm, qraw[g]], writes=[pz])
                        qf, pzf = argt[g], cumt[g]
                        if ROPE_STAGE >= 2:
                            k.op("act", lambda e: e.activation(out=qf.t[:], in_=pa.t[:], func=AF.Copy), reads=[pa], writes=[qf])
                            k.op("dve", lambda e: e.tensor_tensor(out=t1[g].t[:], in0=qf.t[:], in1=Cblk.t[:], op=ALU.mult),
                                 reads=[qf, Cblk], writes=[t1[g]])
                        if ROPE_STAGE >= 3:
                            k.op("act", lambda e: e.activation(out=pzf.t[:], in_=pz.t[:], func=AF.Copy), reads=[pz], writes=[pzf])
                            k.op("dve", lambda e: e.tensor_tensor(out=t2[g].t[:], in0=pzf.t[:], in1=Sblk.t[:], op=ALU.mult),
                                 reads=[pzf, Sblk], writes=[t2[g]])
                        if ROPE_STAGE >= 4:
                            k.op("dve", lambda e: e.tensor_tensor(out=t1[g].t[:], in0=t1[g].t[:], in1=t2[g].t[:], op=ALU.add),
                                 reads=[t1[g], t2[g]], writes=[t1[g]])
                            k.op("act", lambda e: e.activation(out=st.t[:, jj, :], in_=t1[g].t[:], func=AF.Copy, scale=sc),
                                 reads=[t1[g]], writes=[st])
                        else:
                            k.op("act", lambda e: e.activation(out=st.t[:, jj, :], in_=pa.t[:], func=AF.Copy), reads=[pa], writes=[st])
                    if kind == "dq":
                        k.dma("sp", QDT[hf * 4:hf * 4 + 4, :, lat0:lat0 + 512].rearrange("h p n -> p h n"), st.t[:],
                              reads=[st], writes=[bQDT])
                    else:
                        k.dma("sp", KDT[hf * 4:hf * 4 + 4, :, tok0:tok0 + NT].rearrange("h p n -> p h n"), st.t[:, :, :NT],
                              reads=[st], writes=[bKDT])
        k.pop()
        if STOP_AFTER in ("B", "B1"):
            return finish(nc, k, out)


        k.push()
        KT = k.sb("KT", [128, NTOK], BF16)
        Vh = k.sb("Vh", [128, 34, 128], BF16)
        onesb = k.sb("onesb", [128, 128], BF16)
        k.op("pool", lambda e: e.memset(onesb.t[:], 1.0), writes=[onesb])
        onesf = k.sb("onesf", [128, 128], F32)
        k.op("pool", lambda e: e.memset(onesf.t[:], 1.0), writes=[onesf])
        Pacc = [k.sb("Pacc%d" % i, [128, 512], F32) for i in range(2)]
        QT = [[k.sb("QT%d_%d" % (i, m), [128, 512], BF16) for m in range(2)] for i in range(2)]
        for i in range(2):
            for m in range(2):
                k.op("pool", lambda e: e.memset(QT[i][m].t[:], 0.0), writes=[QT[i][m]])
        PT = [k.sb("PT%d" % i, [128, 512], BF16) for i in range(3)]
        On = [k.sb("On%d" % i, [128, 512], F32) for i in range(2)]
        Rt = k.sb("Rt", [128, 512], F32)
        dd = k.sb("dd", [128, 512], F32)
        sqb = k.sb("sqb", [128, 512], BF16)
        rs = k.sb("rs", [128, 512], F32)
        ost = [k.sb("ost%d" % i, [128, 512], F32) for i in range(2)]
        dl = k.sb("dl", [128, 256], F32)
        k.dma("sp", dl.t[:], diff_lambda.ap.partition_broadcast(128), writes=[dl])
        prod = k.sb("prod", [128, 2, 64], F32)
        for i in range(2):
            k.op("dve", lambda e: e.tensor_tensor(out=prod.t[:, i, :], in0=dl.t[:, i * 128:i * 128 + 64],
                                                  in1=dl.t[:, i * 128 + 64:i * 128 + 128], op=ALU.mult), reads=[dl], writes=[prod])
        sums = k.sb("sums", [128, 2], F32)
        k.op("dve", lambda e: e.tensor_reduce(out=sums.t[:], in_=prod.t[:], axis=AX.X, op=ALU.add), reads=[prod], writes=[sums])
        exl = k.sb("exl", [128, 2], F32)
        k.op("act", lambda e: e.activation(out=exl.t[:], in_=sums.t[:], func=AF.Exp), reads=[sums], writes=[exl])
        neglam = k.sb("neglam", [128, 1], F32)
        k.op("dve", lambda e: e.tensor_tensor(out=neglam.t[:], in0=exl.t[:, 1:2], in1=exl.t[:, 0:1], op=ALU.subtract),
             reads=[exl], writes=[neglam])
        k.op("dve", lambda e: e.tensor_scalar(out=neglam.t[:], in0=neglam.t[:], scalar1=-LAM_INIT, scalar2=None, op0=ALU.add),
             reads=[neglam], writes=[neglam])
        g8c = k.sb("g8c", [128, 1], F32)
        with nc.allow_non_contiguous_dma(reason="tiny per-partition gain vector"):
            k.dma("sp", g8c.t[:], diff_norm_g.ap.rearrange("o (p u) -> (o p) u", u=1), writes=[g8c])
        k.op("dve", lambda e: e.tensor_scalar(out=g8c.t[:], in0=g8c.t[:], scalar1=1.0 - LAM_INIT, scalar2=None, op0=ALU.mult),
             reads=[g8c], writes=[g8c])
        cq = 0
        cp = 0
        cm = 0
        NH = C_HEADS if STOP_AFTER != "C1" else 1
        for h in range(NH):
            k.dma("sp", KT.t[:], KDT[h], reads=[bKDT], writes=[KT])
            k.dma("sp", Vh.t[:], VD[:, h * 128:(h + 1) * 128].rearrange("(t p) f -> p t f", p=128), reads=[bVD], writes=[Vh])
            for qb in range(8):
                QTb = QT[cq % 2]
                osb = ost[cq % 2]
                cq += 1
                for m in range(2):
                    k.dma("sp", QTb[m].t[64 * m:64 * m + 64, :], QDT[h, 64 * m:64 * m + 64, qb * 512:(qb + 1) * 512],
                          reads=[bQDT], writes=[QTb[m]])
                steps = [(m, kt) for m in range(2) for kt in range(34)]

                def emit_qk(i):
                    m, kt = steps[i]
                    pS = PS[i % 3]
                    k.op("pe", lambda e: e.matmul(pS.t[:], KT.t[:, kt * 128:(kt + 1) * 128], QTb[m].t[:], start=True, stop=True),
                         reads=[KT, QTb[m]], writes=[pS])

                emit_qk(0)
                emit_qk(1)
                for i, (m, kt) in enumerate(steps):
                    if i + 2 < len(steps):
                        emit_qk(i + 2)
                    if kt == 0:
                        pO = PS[3 + 2 * (cm % 2)]
                        pL = PS[4 + 2 * (cm % 2)]
                        cm += 1
                    pS = PS[i % 3]
                    PTb = PT[cp % 3]
                    cp += 1
                    for _f in range(N_FILL):
                        k.op("pe", lambda e: e.matmul(PS[7].t[:], onesb.t[:], KT.t[:, 0:512], start=True, stop=True),
                             reads=[onesb, KT], writes=[PS[7]])
                    k.op("act", lambda e: e.activation(out=PTb.t[:], in_=pS.t[:], func=AF.Exp), reads=[pS], writes=[PTb])
                    k.op("pe", lambda e: e.matmul(pO.t[:], Vh.t[:, kt, :], PTb.t[:], start=(kt == 0), stop=(kt == 33)),
                         reads=[PTb, Vh], writes=[pO])
                    if kt == 0:
                        k.op("dve", lambda e: e.tensor_copy(out=Pacc[m].t[:], in_=PTb.t[:]), reads=[PTb], writes=[Pacc[m]])
                    else:
                        k.op("dve", lambda e: e.tensor_tensor(out=Pacc[m].t[:], in0=Pacc[m].t[:], in1=PTb.t[:], op=ALU.add),
                             reads=[Pacc[m], PTb], writes=[Pacc[m]])
                    if kt == 33:
                        k.op("pe", lambda e: e.matmul(pL.t[:], onesf.t[:], Pacc[m].t[:], start=True, stop=True),
                             reads=[Pacc[m], onesf], writes=[pL])
                        k.op("act", lambda e: e.activation(out=On[m].t[:], in_=pO.t[:], func=AF.Copy), reads=[pO], writes=[On[m]])
                        k.op("dve", lambda e: e.reciprocal(out=Rt.t[:], in_=pL.t[:]), reads=[pL], writes=[Rt])
                        k.op("dve", lambda e: e.tensor_tensor(out=On[m].t[:], in0=On[m].t[:], in1=Rt.t[:], op=ALU.mult),
                             reads=[On[m], Rt], writes=[On[m]])
                k.op("dve", lambda e: e.scalar_tensor_tensor(out=dd.t[:], in0=On[1].t[:], scalar=neglam.t[:, 0:1], in1=On[0].t[:],
                                                             op0=ALU.mult, op1=ALU.add), reads=[On[0], On[1], neglam], writes=[dd])
                k.op("dve", lambda e: e.tensor_tensor(out=sqb.t[:], in0=dd.t[:], in1=dd.t[:], op=ALU.mult), reads=[dd], writes=[sqb])
                pR = PS[7]
                k.op("pe", lambda e: e.matmul(pR.t[:], onesb.t[:], sqb.t[:], start=True, stop=True), reads=[onesb, sqb], writes=[pR])
                k.op("dve", lambda e: e.tensor_scalar(out=rs.t[:], in0=pR.t[:], scalar1=1.0 / 128, scalar2=1e-6, op0=ALU.mult,
                                                      op1=ALU.add), reads=[pR], writes=[rs])
                k.op("act", lambda e: e.activation(out=rs.t[:], in_=rs.t[:], func=AF.Ln), reads=[rs], writes=[rs])
                k.op("act", lambda e: e.activation(out=rs.t[:], in_=rs.t[:], func=AF.Exp, scale=-0.5), reads=[rs], writes=[rs])
                k.op("dve", lambda e: e.scalar_tensor_tensor(out=osb.t[:], in0=dd.t[:], scalar=g8c.t[:, 0:1], in1=rs.t[:],
                                                             op0=ALU.mult, op1=ALU.mult), reads=[dd, g8c, rs], writes=[osb])
                k.dma("sp", ALT[h * 128:(h + 1) * 128, qb * 512:(qb + 1) * 512], osb.t[:], reads=[osb], writes=[bALT])
        k.pop()
        if STOP_AFTER in ("C", "C1"):
            return finish(nc, k, out)

        k.push()
        etot = k.sb("etot", [128, 2, 4, 34], F32)
        for d in range(2):
            k.dma("sp", etot.t[:, d], ETOT[d], reads=[bETOT], writes=[etot])
        masks = [k.sb("mask%d" % d, [128, 128], BF16) for d in range(2)]
        k.dma("sp", masks[0].t[:], c_maskf.ap, writes=[masks[0]])
        k.dma("sp", masks[1].t[:], c_maskb.ap, writes=[masks[1]])
        gn = k.sb("gn", [128, 256], F32)
        k.dma("sp", gn.t[:], gla_norm_g.ap.partition_broadcast(128), writes=[gn])
        S32 = [k.sb("S32_%d" % j, [128, 256], F32) for j in range(4)]
        Sb = [k.sb("Sb_%d" % j, [128, 256], BF16) for j in range(4)]
        Ab = [k.sb("Ab%d" % i, [128, 4, 128], BF16) for i in range(2)]
        Bb = [k.sb("Bb%d" % i, [128, 4, 128], BF16) for i in range(2)]
        Vg = [k.sb("Vg%d" % i, [128, 1024], BF16) for i in range(2)]
        OFb = [k.sb("OFb%d" % i, [128, 1024], F32) for i in range(2)]
        Rb = [k.sb("Rb%d" % i, [128, 1024], F32) for i in range(2)]
        OFst = [k.sb("OFst%d" % i, [128, 1024], F32) for i in range(2)]
        gl = [k.sb("gl%d" % i, [128, 1024], F32) for i in range(2)]
        Btok = [k.sb("Btok%d" % i, [128, 128], BF16) for i in range(2)]
        attT = [k.sb("attT%d" % i, [128, 128], BF16) for i in range(2)]
        ste = [k.sb("ste%d" % i, [128, 256], F32) for i in range(2)]
        osb2 = [k.sb("osb2_%d" % i, [128, 256], F32) for i in range(2)]
        junk2 = k.sb("junk2", [128, 256], F32)
        ssq2 = k.sb("ssq2", [128, 4], F32)
        rstd2 = k.sb("rstd2", [128, 4], F32)
        cs = 0
        cx = 0
        orders = [list(range(34)), [1, 0] + list(range(33, 1, -1))]
        if STOP_AFTER == "D1":
            orders = [o[:6] for o in orders]
        seqD = [(d, ci) for d in range(2) for ci in orders[d]]

        def d_load(idx):
            d, ci = seqD[idx]
            b = idx % 2
            tok0 = ci * 128
            lat0 = tok0 - 256
            k.dma("sp", Ab[b].t[:], AT[d, :, :, tok0:tok0 + 128].rearrange("j p n -> p j n"), reads=[bAT], writes=[Ab[b]])
            k.dma("sp", Bb[b].t[:], BT[d, :, :, tok0:tok0 + 128].rearrange("j p n -> p j n"), reads=[bBT], writes=[Bb[b]])
            k.dma("sp", Vg[b].t[:], VG[tok0:tok0 + 128, :], reads=[bVG], writes=[Vg[b]])
            if d == 1 and ci >= 2:
                k.dma("sp", OFb[b].t[:], OFS[lat0:lat0 + 128, :], reads=[bOFS], writes=[OFb[b]])
                k.dma("sp", Rb[b].t[:], RG[lat0:lat0 + 128, :], reads=[bRG], writes=[Rb[b]])

        d_load(0)
        for d in range(2):
            for j in range(4):
                k.op("pool", lambda e: e.memset(S32[j].t[:], 0.0), writes=[S32[j]])
                k.op("pool", lambda e: e.memset(Sb[j].t[:], 0.0), writes=[Sb[j]])
            for ci in orders[d]:
                isctx = ci < 2
                tok0 = ci * 128
                lat0 = tok0 - 256
                b = cs % 2
                cs += 1
                if cs < len(seqD):
                    d_load(cs)
                for j in range(4):
                    x2 = cx % 2
                    cx += 1
                    et = etot.t[:, d, j, ci:ci + 1]
                    pT = PS[x2]
                    k.op("pe", lambda e: e.transpose(pT.t[:].bitcast(BF16)[:, 0:128], Bb[b].t[:, j, :], identb.t[:]),
                         reads=[Bb[b], identb], writes=[pT])
                    k.op("act", lambda e: e.activation(out=Btok[x2].t[:], in_=pT.t[:].bitcast(BF16)[:, 0:128], func=AF.Copy),
                         reads=[pT], writes=[Btok[x2]])
                    if d == 1:
                        k.op("dve", lambda e: e.tensor_scalar(out=S32[j].t[:], in0=S32[j].t[:], scalar1=et, scalar2=None,
                                                              op0=ALU.mult), reads=[S32[j], etot], writes=[S32[j]])
                        k.op("act", lambda e: e.activation(out=Sb[j].t[:], in_=S32[j].t[:], func=AF.Copy),
                             reads=[S32[j]], writes=[Sb[j]])
                    if not isctx:
                        pA = PS[2 + x2]
                        k.op("pe", lambda e: e.matmul(pA.t[:, 0:128], Bb[b].t[:, j, :], Ab[b].t[:, j, :], start=True, stop=True),
                             reads=[Ab[b], Bb[b]], writes=[pA])
                        k.op("dve", lambda e: e.tensor_tensor(out=attT[x2].t[:], in0=pA.t[:, 0:128], in1=masks[d].t[:], op=ALU.mult),
                             reads=[pA, masks[d]], writes=[attT[x2]])
                        pO = PS[4 + x2]
                        k.op("pe", lambda e: e.matmul(pO.t[:, 0:256], attT[x2].t[:], Vg[b].t[:, j * 256:(j + 1) * 256],
                                                      start=True, stop=False), reads=[attT[x2], Vg[b]], writes=[pO])
                        k.op("pe", lambda e: e.matmul(pO.t[:, 0:256], Ab[b].t[:, j, :], Sb[j].t[:], start=False, stop=True),
                             reads=[Ab[b], Sb[j]], writes=[pO])
                        if d == 0:
                            k.op("act", lambda e: e.activation(out=OFst[b].t[:, j * 256:(j + 1) * 256], in_=pO.t[:, 0:256],
                                                               func=AF.Copy), reads=[pO], writes=[OFst[b]])
                        else:
                            k.op("act", lambda e: e.activation(out=osb2[x2].t[:], in_=pO.t[:, 0:256], func=AF.Copy),
                                 reads=[pO], writes=[osb2[x2]])
                            k.op("dve", lambda e: e.tensor_tensor(out=gl[b].t[:, j * 256:(j + 1) * 256], in0=osb2[x2].t[:],
                                                                  in1=OFb[b].t[:, j * 256:(j + 1) * 256], op=ALU.add),
                                 reads=[osb2[x2], OFb[b]], writes=[gl[b]])
                            k.op("act", lambda e: e.activation(out=junk2.t[:], in_=gl[b].t[:, j * 256:(j + 1) * 256], func=AF.Square,
                                                               accum_out=ssq2.t[:, j:j + 1]), reads=[gl[b]], writes=[junk2, ssq2])
                    pS2 = PS[6 + x2]
                    k.op("pe", lambda e: e.matmul(pS2.t[:, 0:256], Btok[x2].t[:], Vg[b].t[:, j * 256:(j + 1) * 256],
                                                  start=True, stop=True), reads=[Btok[x2], Vg[b]], writes=[pS2])
                    if d == 0:
                        k.op("act", lambda e: e.activation(out=ste[x2].t[:], in_=pS2.t[:, 0:256], func=AF.Copy, scale=et),
                             reads=[pS2, etot], writes=[ste[x2]])
                        k.op("dve", lambda e: e.scalar_tensor_tensor(out=S32[j].t[:], in0=S32[j].t[:], scalar=et, in1=ste[x2].t[:],
                                                                     op0=ALU.mult, op1=ALU.add),
                             reads=[S32[j], ste[x2], etot], writes=[S32[j]])
                        k.op("act", lambda e: e.activation(out=Sb[j].t[:], in_=S32[j].t[:], func=AF.Copy),
                             reads=[S32[j]], writes=[Sb[j]])
                    else:
                        k.op("act", lambda e: e.activation(out=ste[x2].t[:], in_=pS2.t[:, 0:256], func=AF.Copy),
                             reads=[pS2], writes=[ste[x2]])
                        k.op("dve", lambda e: e.tensor_tensor(out=S32[j].t[:], in0=S32[j].t[:], in1=ste[x2].t[:], op=ALU.add),
                             reads=[S32[j], ste[x2]], writes=[S32[j]])
                if not isctx:
                    if d == 0:
                        k.dma("sp", OFS[lat0:lat0 + 128, :], OFst[b].t[:], reads=[OFst[b]], writes=[bOFS])
                    else:
                        k.op("dve", lambda e: e.tensor_scalar(out=rstd2.t[:], in0=ssq2.t[:], scalar1=1.0 / 256, scalar2=1e-6,
                                                              op0=ALU.mult, op1=ALU.add), reads=[ssq2], writes=[rstd2])
                        k.op("act", lambda e: e.activation(out=rstd2.t[:], in_=rstd2.t[:], func=AF.Sqrt), reads=[rstd2], writes=[rstd2])
                        k.op("dve", lambda e: e.reciprocal(out=rstd2.t[:], in_=rstd2.t[:]), reads=[rstd2], writes=[rstd2])
                        for j in range(4):
                            k.op("dve", lambda e: e.scalar_tensor_tensor(out=gl[b].t[:, j * 256:(j + 1) * 256],
                                                                         in0=gl[b].t[:, j * 256:(j + 1) * 256],
                                                                         scalar=rstd2.t[:, j:j + 1], in1=gn.t[:], op0=ALU.mult,
                                                                         op1=ALU.mult), reads=[gl[b], rstd2, gn], writes=[gl[b]])
                        k.op("pool", lambda e: e.tensor_tensor(out=gl[b].t[:], in0=gl[b].t[:], in1=Rb[b].t[:], op=ALU.mult),
                             reads=[gl[b], Rb[b]], writes=[gl[b]])
                        k.dma("sp", AL[lat0:lat0 + 128, 0:1024], gl[b].t[:], reads=[gl[b]], writes=[bAL])
        k.pop()
        if STOP_AFTER in ("D", "D1"):
            return finish(nc, k, out)


        def bcast_tile(name, src_ap):
            t = k.sb(name, [128, D], F32)
            k.dma("sp", t.t[:], src_ap.partition_broadcast(128), writes=[t])
            return t

        def layer_norm(y, gbc, bbc, outb, st, junkb):
            k.op("act", lambda e: e.activation(out=junkb.t[:], in_=y.t[:], func=AF.Copy, accum_out=st.t[:, 0:1]),
                 reads=[y], writes=[junkb, st])
            k.op("act", lambda e: e.activation(out=junkb.t[:], in_=y.t[:], func=AF.Square, accum_out=st.t[:, 1:2]),
                 reads=[y], writes=[junkb, st])
            k.op("dve", lambda e: e.tensor_scalar(out=st.t[:, 0:2], in0=st.t[:, 0:2], scalar1=1.0 / D, scalar2=None, op0=ALU.mult),
                 reads=[st], writes=[st])
            k.op("dve", lambda e: e.tensor_tensor(out=st.t[:, 2:3], in0=st.t[:, 0:1], in1=st.t[:, 0:1], op=ALU.mult),
                 reads=[st], writes=[st])
            k.op("dve", lambda e: e.tensor_tensor(out=st.t[:, 3:4], in0=st.t[:, 1:2], in1=st.t[:, 2:3], op=ALU.subtract),
                 reads=[st], writes=[st])
            k.op("dve", lambda e: e.tensor_scalar(out=st.t[:, 3:4], in0=st.t[:, 3:4], scalar1=1e-5, scalar2=None, op0=ALU.add),
                 reads=[st], writes=[st])
            k.op("act", lambda e: e.activation(out=st.t[:, 3:4], in_=st.t[:, 3:4], func=AF.Sqrt), reads=[st], writes=[st])
            k.op("dve", lambda e: e.reciprocal(out=st.t[:, 4:5], in_=st.t[:, 3:4]), reads=[st], writes=[st])
            k.op("dve", lambda e: e.scalar_tensor_tensor(out=st.t[:, 5:6], in0=st.t[:, 0:1], scalar=-1.0, in1=st.t[:, 4:5],
                                                         op0=ALU.mult, op1=ALU.mult), reads=[st], writes=[st])
            k.op("act", lambda e: e.activation(out=outb.t[:], in_=y.t[:], func=AF.Identity, scale=st.t[:, 4:5], bias=st.t[:, 5:6]),
                 reads=[y, st], writes=[outb])
            k.op("dve", lambda e: e.tensor_tensor(out=outb.t[:], in0=outb.t[:], in1=gbc.t[:], op=ALU.mult),
                 reads=[outb, gbc], writes=[outb])
            k.op("pool", lambda e: e.tensor_tensor(out=outb.t[:], in0=outb.t[:], in1=bbc.t[:], op=ALU.add),
                 reads=[outb, bbc], writes=[outb])

        affall = k.sb("affall", [128, 32, NE], F32)
        k.push()
        g1bc = bcast_tile("g1bc", MOD[0:1, 2 * D:3 * D])
        l1g = bcast_tile("l1g", ln1_g.ap)
        l1b = bcast_tile("l1b", ln1_b.ap)
        aT = k.sb("aT", [128, 16, 512], F32R)
        oslab = [k.sb("oslab%d" % i, [128, 16, 256], F32R) for i in range(2)]
        osb = k.sb("osbE", [128, 4, D], F32)
        alt = [k.sb("alt%d" % i, [128, D], F32) for i in range(2)]
        xe = k.sb("xe", [128, D], F32)
        x1t = [k.sb("x1t%d" % i, [128, D], F32) for i in range(2)]
        junkE = k.sb("junkE", [128, D], F32)
        stE = k.sb("stE", [128, 8], F32)
        hT = k.sb("hT", [128, 16, 128], F32R)
        wr = k.sb("wr", [128, 16, NE], F32R)
        k.dma("pool", wr.t[:], w_router.ap.rearrange("(c p) e -> p c e", p=128), writes=[wr])
        sm = k.sb("sm", [128, 4], F32)
        ee = k.sb("ee", [128, NE], F32)
        ca = 0
        cso = 0
        NBE = 8 if STOP_AFTER != "E1" else 1
        for tb in range(NBE):
            k.dma("pool", aT.t[:, 8:16, :], ALT[:, tb * 512:(tb + 1) * 512].rearrange("(c p) n -> p c n", p=128),
                  reads=[bALT], writes=[aT])
            for t in range(4):
                al = alt[ca % 2]
                ca += 1
                r0 = tb * 512 + t * 128
                k.dma("sp", al.t[:, 0:1024], AL[r0:r0 + 128, 0:1024], reads=[bAL], writes=[al])
                for c in range(8):
                    pb = PS[c // 4]
                    k.op("pe", lambda e: e.transpose(pb.t[:, (c % 4) * 128:(c % 4 + 1) * 128], al.t[:, c * 128:(c + 1) * 128],
                                                     identf.t[:]), reads=[al, identf], writes=[pb])
                for q4 in range(2):
                    pb = PS[q4]
                    k.op("act", lambda e: e.activation(out=aT.t[:, q4 * 4:q4 * 4 + 4, t * 128:(t + 1) * 128],
                                                       in_=pb.t[:].rearrange("p (c n) -> p c n", c=4), func=AF.Copy),
                         reads=[pb], writes=[aT])
            for cb in range(8):
                sl = oslab[cso % 2]
                cso += 1
                k.dma("pool", sl.t[:], w_o.ap[:, cb * 256:(cb + 1) * 256].rearrange("(c p) f -> p c f", p=128), writes=[sl])
                for t in range(4):
                    pa = PS[4 + (t % 4)]
                    for c in range(16):
                        k.op("pe", lambda e: e.matmul(pa.t[:, 0:256], aT.t[:, c, t * 128:(t + 1) * 128], sl.t[:, c, :],
                                                      start=(c == 0), stop=(c == 15)), reads=[aT, sl], writes=[pa])
                    k.op("act", lambda e: e.activation(out=osb.t[:, t, cb * 256:(cb + 1) * 256], in_=pa.t[:, 0:256], func=AF.Copy),
                         reads=[pa], writes=[osb])
            for t in range(4):
                r0 = tb * 512 + t * 128
                tile_i = tb * 4 + t
                x1 = x1t[tile_i % 2]
                k.dma("sp", xe.t[:], x.ap[r0:r0 + 128, :], writes=[xe])
                k.op("dve", lambda e: e.tensor_tensor(out=osb.t[:, t, :], in0=osb.t[:, t, :], in1=g1bc.t[:], op=ALU.mult),
                     reads=[osb, g1bc], writes=[osb])
                k.op("dve", lambda e: e.scalar_tensor_tensor(out=xe.t[:], in0=xe.t[:], scalar=ALPHA, in1=osb.t[:, t, :],
                                                             op0=ALU.mult, op1=ALU.add), reads=[xe, osb], writes=[xe])
                layer_norm(xe, l1g, l1b, x1, stE, junkE)
                k.dma("sp", X1[r0:r0 + 128, :], x1.t[:], reads=[x1], writes=[bX1])
                for c in range(16):
                    pb = PS[c // 4]
                    k.op("pe", lambda e: e.transpose(pb.t[:, (c % 4) * 128:(c % 4 + 1) * 128], x1.t[:, c * 128:(c + 1) * 128],
                                                     identf.t[:]), reads=[x1, identf], writes=[pb])
                for c in range(16):
                    pb = PS[c // 4]
                    k.op("act", lambda e: e.activation(out=hT.t[:, c, :], in_=pb.t[:, (c % 4) * 128:(c % 4 + 1) * 128],
                                                       func=AF.Identity, scale=modT.t[:, 0, 64 + c:65 + c],
                                                       bias=modT.t[:, 0, 48 + c:49 + c]), reads=[pb, modT], writes=[hT])
                pl = PS[4]
                for c in range(16):
                    k.op("pe", lambda e: e.matmul(pl.t[:, 0:NE], hT.t[:, c, :], wr.t[:, c, :], start=(c == 0), stop=(c == 15)),
                         reads=[hT, wr], writes=[pl])
                k.op("dve", lambda e: e.tensor_reduce(out=sm.t[:, 0:1], in_=pl.t[:, 0:NE], axis=AX.X, op=ALU.max),
                     reads=[pl], writes=[sm])
                k.op("dve", lambda e: e.tensor_scalar(out=sm.t[:, 1:2], in0=sm.t[:, 0:1], scalar1=-1.0, scalar2=None, op0=ALU.mult),
                     reads=[sm], writes=[sm])
                k.op("act", lambda e: e.activation(out=ee.t[:], in_=pl.t[:, 0:NE], func=AF.Exp, bias=sm.t[:, 1:2],
                                                   accum_out=sm.t[:, 2:3]), reads=[pl, sm], writes=[ee, sm])
                k.op("dve", lambda e: e.reciprocal(out=sm.t[:, 3:4], in_=sm.t[:, 2:3]), reads=[sm], writes=[sm])
                k.op("dve", lambda e: e.tensor_scalar(out=affall.t[:, tile_i, :], in0=ee.t[:], scalar1=sm.t[:, 3:4], scalar2=None,
                                                      op0=ALU.mult), reads=[ee, sm], writes=[affall])
        k.pop()
        if DEBUG and "AFF" in DEBUG_NAMES:
            k.dma("sp", AFF.rearrange("(t p) e -> p t e", p=128), affall.t[:], reads=[affall], writes=[bAFF])
        if STOP_AFTER in ("E", "E1"):
            return finish(nc, k, out)

        idxI = k.sb("idxI", [128, NE, 4], I32)
        gateS = k.sb("gateS", [128, NE, 4], F32)
        k.push()
        affT = k.sb("affT", [16, NLAT], F32)
        for g8i in range(8):
            pb = PS[g8i % 2]
            for t4 in range(4):
                t = g8i * 4 + t4
                k.op("pe", lambda e: e.transpose(pb.t[0:16, t4 * 128:(t4 + 1) * 128], affall.t[:, t, :], identf.t[:]),
                     reads=[affall, identf], writes=[pb])
            k.op("act", lambda e: e.activation(out=affT.t[:, g8i * 512:(g8i + 1) * 512], in_=pb.t[0:16, :], func=AF.Copy),
                 reads=[pb], writes=[affT])
        bs = k.sb("bs", [16, 8], F32)
        junkT = k.sb("junkT", [16, NLAT], F32)
        k.op("dve", lambda e: e.memset(bs.t[:], 0.0), writes=[bs])
        k.op("dve", lambda e: e.memset(bs.t[:, 1:2], 1.0), reads=[bs], writes=[bs])
        for it in range(30):
            k.op("dve", lambda e: e.tensor_scalar(out=bs.t[:, 5:6], in0=bs.t[:, 1:2], scalar1=0.5, scalar2=None, op0=ALU.mult),
                 reads=[bs], writes=[bs])
            k.op("dve", lambda e: e.scalar_tensor_tensor(out=bs.t[:, 2:3], in0=bs.t[:, 0:1], scalar=0.5, in1=bs.t[:, 5:6],
                                                         op0=ALU.mult, op1=ALU.add), reads=[bs], writes=[bs])
            k.op("dve", lambda e: e.tensor_scalar(out=junkT.t[:], in0=affT.t[:], scalar1=bs.t[:, 2:3], scalar2=0.0, op0=ALU.is_ge,
                                                  op1=ALU.add, accum_out=bs.t[:, 3:4]), reads=[affT, bs], writes=[junkT, bs])
            k.op("dve", lambda e: e.tensor_scalar(out=bs.t[:, 4:5], in0=bs.t[:, 3:4], scalar1=float(CAP), scalar2=None,
                                                  op0=ALU.is_ge), reads=[bs], writes=[bs])
            k.op("dve", lambda e: e.tensor_tensor(out=bs.t[:, 5:6], in0=bs.t[:, 2:3], in1=bs.t[:, 0:1], op=ALU.subtract),
                 reads=[bs], writes=[bs])
            k.op("dve", lambda e: e.tensor_tensor(out=bs.t[:, 6:7], in0=bs.t[:, 1:2], in1=bs.t[:, 2:3], op=ALU.subtract),
                 reads=[bs], writes=[bs])
            k.op("dve", lambda e: e.scalar_tensor_tensor(out=bs.t[:, 0:1], in0=bs.t[:, 5:6], scalar=bs.t[:, 4:5], in1=bs.t[:, 0:1],
                                                         op0=ALU.mult, op1=ALU.add), reads=[bs], writes=[bs])
            k.op("dve", lambda e: e.scalar_tensor_tensor(out=bs.t[:, 1:2], in0=bs.t[:, 6:7], scalar=bs.t[:, 4:5], in1=bs.t[:, 2:3],
                                                         op0=ALU.mult, op1=ALU.add), reads=[bs], writes=[bs])
        Mt = k.sb("Mt", [16, NLAT], F32)
        k.op("dve", lambda e: e.tensor_scalar(out=Mt.t[:], in0=affT.t[:], scalar1=bs.t[:, 0:1], scalar2=None, op0=ALU.is_ge),
             reads=[affT, bs], writes=[Mt])
        k.op("dve", lambda e: e.memset(junkT.t[:], 1.0), writes=[junkT])
        cumM = k.sb("cumM", [16, NLAT], F32)
        k.op("dve", lambda e: e.tensor_tensor_scan(out=cumM.t[:], data0=junkT.t[:], data1=Mt.t[:], initial=0.0, op0=ALU.mult,
                                                   op1=ALU.add), reads=[junkT, Mt], writes=[cumM])
        k.op("dve", lambda e: e.scalar_tensor_tensor(out=cumM.t[:], in0=Mt.t[:], scalar=-8193.0, in1=cumM.t[:], op0=ALU.mult,
                                                     op1=ALU.add), reads=[Mt, cumM], writes=[cumM])
        k.op("dve", lambda e: e.tensor_scalar(out=cumM.t[:], in0=cumM.t[:], scalar1=8192.0, scalar2=None, op0=ALU.add),
             reads=[cumM], writes=[cumM])
        slotTM = k.sb("slotTM", [128, 32, NE], F32)
        pb = PS[2]
        for t in range(32):
            k.op("pe", lambda e: e.transpose(pb.t[:, t * 16:(t + 1) * 16], cumM.t[:, t * 128:(t + 1) * 128], identf.t[0:16, 0:16]),
                 reads=[cumM, identf], writes=[pb])
        k.op("act", lambda e: e.activation(out=slotTM.t[:].rearrange("p t e -> p (t e)"), in_=pb.t[:], func=AF.Copy),
             reads=[pb], writes=[slotTM])
        vals = k.sb("vals", [128, 32, NE, 4], F32)
        tokv = k.sb("tokv", [128, 32, 2], F32)
        k.dma("sp", tokv.t[:], c_tokv.ap, writes=[tokv])
        for e_ in range(NE):
            k.op("pool", lambda e: e.tensor_copy(out=vals.t[:, :, e_, 0:2], in_=tokv.t[:]), reads=[tokv], writes=[vals])
        k.op("pool", lambda e: e.tensor_copy(out=vals.t[:, :, :, 2], in_=affall.t[:]), reads=[affall], writes=[vals])
        k.op("pool", lambda e: e.tensor_copy(out=vals.t[:, :, :, 3], in_=affall.t[:]), reads=[affall], writes=[vals])
        iot = k.sb("iot", [128, 512], F32)
        k.dma("sp", iot.t[:], c_iota.ap, writes=[iot])
        Sel = [k.sb("Sel%d" % i, [128, 512], F32) for i in range(3)]
        res = k.sb("resF", [128, 4, 4], F32)
        idf = k.sb("idf", [128, 4], F32)
        csel = 0
        for e_ in range(NE):
            for t in range(32):
                sl = Sel[csel % 3]
                csel += 1
                k.op("dve", lambda e: e.tensor_scalar(out=sl.t[:], in0=iot.t[:], scalar1=slotTM.t[:, t, e_:e_ + 1], scalar2=None,
                                                      op0=ALU.is_equal), reads=[iot, slotTM], writes=[sl])
                for st in range(4):
                    k.op("pe", lambda e: e.matmul(PS[4 + st].t[:, 0:4], sl.t[:, st * 128:(st + 1) * 128], vals.t[:, t, e_, :],
                                                  start=(t == 0), stop=(t == 31)), reads=[sl, vals], writes=[PS[4 + st]])
            for st in range(4):
                k.op("act", lambda e: e.activation(out=res.t[:, st, :], in_=PS[4 + st].t[:, 0:4], func=AF.Copy),
                     reads=[PS[4 + st]], writes=[res])
            k.op("dve", lambda e: e.scalar_tensor_tensor(out=idf.t[:], in0=res.t[:, :, 0], scalar=64.0, in1=res.t[:, :, 1],
                                                         op0=ALU.mult, op1=ALU.add), reads=[res], writes=[idf])
            k.op("dve", lambda e: e.tensor_copy(out=idxI.t[:, e_, :], in_=idf.t[:]), reads=[idf], writes=[idxI])
            k.op("dve", lambda e: e.tensor_copy(out=gateS.t[:, e_, :], in_=res.t[:, :, 2]), reads=[res], writes=[gateS])
        k.pop()
        if DEBUG and "IDXD" in DEBUG_NAMES:
            k.dma("sp", IDXD, idxI.t[:], reads=[idxI], writes=[bAFF])
            k.dma("sp", GATED, gateS.t[:], reads=[gateS], writes=[bAFF])
        if STOP_AFTER == "F":
            return finish(nc, k, out)

        k.push()
        zt = k.sb("zt", [128, D], F32)
        k.op("pool", lambda e: e.memset(zt.t[:], 0.0), writes=[zt])
        for t in range(32):
            k.dma("sp", FACC[t * 128:(t + 1) * 128, :], zt.t[:], reads=[zt], writes=[bFACC])
        xsT = k.sb("xsT", [128, 16, 512], F32R)
        hidT = k.sb("hidT", [128, 16, 512], F32R)
        gsl = [k.sb("gsl%d" % i, [128, 16, 512], F32R) for i in range(2)]
        xg = k.sb("xg", [128, D], F32)
        yst = [k.sb("yst%d" % i, [128, D], F32) for i in range(4)]
        s1 = k.sb("s1", [128, 4, 512], F32)
        t3 = [k.sb("t3_%d" % i, [128, 512], F32) for i in range(2)]
        cg = [0]
        cpp = [0]
        NEX = NE if STOP_AFTER != "G1" else 1

        def prep_slot_tile(e_, st):
            k.idma(lambda e: e.indirect_dma_start(out=xg.t[:], out_offset=None, in_=X1,
                                                  in_offset=bass.IndirectOffsetOnAxis(ap=idxI.t[:, e_, st:st + 1], axis=0)),
                   reads=[bX1, idxI], writes=[xg])
            for c in range(16):
                pb = PS[c // 4]
                k.op("pe", lambda e: e.transpose(pb.t[:, (c % 4) * 128:(c % 4 + 1) * 128], xg.t[:, c * 128:(c + 1) * 128],
                                                 identf.t[:]), reads=[xg, identf], writes=[pb])
            for c in range(16):
                pb = PS[c // 4]
                k.op("act", lambda e: e.activation(out=xsT.t[:, c, st * 128:(st + 1) * 128],
                                                   in_=pb.t[:, (c % 4) * 128:(c % 4 + 1) * 128], func=AF.Identity,
                                                   scale=modT.t[:, 0, 64 + c:65 + c], bias=modT.t[:, 0, 48 + c:49 + c]),
                     reads=[pb, modT], writes=[xsT])

        def load_slab(wsrc, e_, cb):
            sl = gsl[cg[0] % 2]
            cg[0] += 1
            k.dma("pool", sl.t[:], wsrc.ap[e_, :, cb * 512:(cb + 1) * 512].rearrange("(c p) f -> p c f", p=128), writes=[sl])
            return sl

        for st in range(4):
            prep_slot_tile(0, st)
        pre = [load_slab(w1, 0, 0), load_slab(w3, 0, 0)]
        for e_ in range(NEX):
            for fb in range(4):
                for wi, wsrc in enumerate((w1, w3)):
                    if fb == 0:
                        sl = pre[wi]
                    else:
                        sl = load_slab(wsrc, e_, fb)
                    for fc in range(4):
                        pa = PS[4 + cpp[0] % 4]
                        cpp[0] += 1
                        for c in range(16):
                            k.op("pe", lambda e: e.matmul(pa.t[:], sl.t[:, c, fc * 128:(fc + 1) * 128], xsT.t[:, c, :],
                                                          start=(c == 0), stop=(c == 15)), reads=[sl, xsT], writes=[pa])
                        if wi == 0:
                            k.op("act", lambda e: e.activation(out=s1.t[:, fc, :], in_=pa.t[:], func=AF.Silu), reads=[pa], writes=[s1])
                        else:
                            tt = t3[fc % 2]
                            k.op("act", lambda e: e.activation(out=tt.t[:], in_=pa.t[:], func=AF.Copy), reads=[pa], writes=[tt])
                            k.op("dve", lambda e: e.tensor_tensor(out=hidT.t[:, fb * 4 + fc, :], in0=tt.t[:], in1=s1.t[:, fc, :],
                                                                  op=ALU.mult), reads=[tt, s1], writes=[hidT])
            for db in range(4):
                sl = load_slab(w2, e_, db)
                for st in range(4):
                    pa = PS[4 + cpp[0] % 4]
                    cpp[0] += 1
                    for c in range(16):
                        k.op("pe", lambda e: e.matmul(pa.t[:], hidT.t[:, c, st * 128:(st + 1) * 128], sl.t[:, c, :],
                                                      start=(c == 0), stop=(c == 15)), reads=[sl, hidT], writes=[pa])
                    k.op("act", lambda e: e.activation(out=yst[st].t[:, db * 512:(db + 1) * 512], in_=pa.t[:], func=AF.Copy,
                                                       scale=gateS.t[:, e_, st:st + 1]), reads=[pa, gateS], writes=[yst[st]])
                if e_ + 1 < NEX:
                    prep_slot_tile(e_ + 1, db)
            if e_ + 1 < NEX:
                pre = [load_slab(w1, e_ + 1, 0), load_slab(w3, e_ + 1, 0)]
            for st in range(4):
                k.idma(lambda e: e.indirect_dma_start(out=FACC, out_offset=bass.IndirectOffsetOnAxis(ap=idxI.t[:, e_, st:st + 1], axis=0),
                                                      in_=yst[st].t[:], in_offset=None, compute_op=ALU.add),
                       reads=[yst[st], idxI, bFACCs, bFACC], writes=[bFACCs])
        k.pop()
        if STOP_AFTER in ("G", "G1"):
            return finish(nc, k, out)

        k.push()
        g2bc = bcast_tile("g2bc", MOD[0:1, 5 * D:6 * D])
        l2g = bcast_tile("l2g", ln2_g.ap)
        l2b = bcast_tile("l2b", ln2_b.ap)
        xh = [k.sb("xh%d" % i, [128, D], F32) for i in range(2)]
        fh = [k.sb("fh%d" % i, [128, D], F32) for i in range(2)]
        oh = [k.sb("oh%d" % i, [128, D], F32) for i in range(2)]
        junkH = k.sb("junkH", [128, D], F32)
        stH = k.sb("stH", [128, 8], F32)
        for t in range(32):
            b = t % 2
            k.dma("sp", xh[b].t[:], X1[t * 128:(t + 1) * 128, :], reads=[bX1], writes=[xh[b]])
            k.dma("sp", fh[b].t[:], FACC[t * 128:(t + 1) * 128, :], reads=[bFACC, bFACCs], writes=[fh[b]])
            k.op("dve", lambda e: e.tensor_tensor(out=fh[b].t[:], in0=fh[b].t[:], in1=g2bc.t[:], op=ALU.mult),
                 reads=[fh[b], g2bc], writes=[fh[b]])
            k.op("dve", lambda e: e.scalar_tensor_tensor(out=xh[b].t[:], in0=xh[b].t[:], scalar=ALPHA, in1=fh[b].t[:],
                                                         op0=ALU.mult, op1=ALU.add), reads=[xh[b], fh[b]], writes=[xh[b]])
            layer_norm(xh[b], l2g, l2b, oh[b], stH, junkH)
            k.dma("sp", out[t * 128:(t + 1) * 128, :], oh[b].t[:], reads=[oh[b]], writes=[bOUT])
        k.pop()

        finish(nc, k, out)
    return nc


def finish(nc, k, out):
    k.barrier(["sp"])
    return nc


_PROGRAM = None


def host_constants():
    import ml_dtypes
    c = {}
    c["c_identf"] = np.eye(128, dtype=np.float32)
    c["c_identb"] = np.eye(128, dtype=np.float32).astype(ml_dtypes.bfloat16)
    perm = np.zeros((128, 128), np.float32)
    for m in range(128):
        s = m + 32 if (m % 64) < 32 else m - 32
        perm[s, m] = 1.0
    c["c_perm"] = perm.astype(ml_dtypes.bfloat16)
    n = np.arange(NLAT)
    row = (n // 64).astype(np.float32)
    col = (n % 64).astype(np.float32)
    inv = (np.float32(10000.0) ** (-np.arange(16, dtype=np.float32) / np.float32(16))).astype(np.float32)
    ang = np.concatenate([row[:, None] * inv[None, :], col[:, None] * inv[None, :]], axis=-1).astype(np.float32)
    cs = np.cos(ang).astype(np.float32).T
    sn = np.sin(ang).astype(np.float32).T
    c["c_ropec"] = np.ascontiguousarray(np.concatenate([cs, cs, cs, cs], axis=0))
    c["c_ropes"] = np.ascontiguousarray(np.concatenate([-sn, sn, -sn, sn], axis=0))
    rs = np.ones((128, 512), np.float32)
    rs[:, ::128] = 0.0
    c["c_reset"] = rs
    j = np.arange(128)[:, None]
    i = np.arange(128)[None, :]
    c["c_maskf"] = (j <= i).astype(np.float32).astype(ml_dtypes.bfloat16)
    c["c_maskb"] = (j >= i).astype(np.float32).astype(ml_dtypes.bfloat16)
    c["c_iota"] = np.tile(np.arange(512, dtype=np.float32)[None, :], (128, 1))
    tok = (np.arange(32)[None, :] * 128 + np.arange(128)[:, None])
    c["c_tokv"] = np.stack([(tok // 64).astype(np.float32), (tok % 64).astype(np.float32)], axis=-1)
    return c


def make_in_maps(inp):
    consts = host_constants()
    f = lambda a: np.ascontiguousarray(np.asarray(a, dtype=np.float32))
    shared = {
        "w_ada": f(inp["w_ada"][0]), "b_ada": f(inp["b_ada"][0]).reshape(1, -1), "w_in": f(inp["w_in"][0]),
        "w_gate2": f(inp["w_gate2"][0]), "b_gate": np.ascontiguousarray(f(inp["b_gate"][0]).reshape(2, 4, 128).transpose(2, 0, 1)),
        "gla_norm_g": f(inp["gla_norm_g"][0]).reshape(1, -1), "diff_lambda": f(inp["diff_lambda"][0]).reshape(1, -1),
        "diff_norm_g": f(inp["diff_norm_g"][0]).reshape(1, -1), "w_o": f(inp["w_o"][0]),
        "ln1_g": f(inp["ln1_g"][0]).reshape(1, -1), "ln1_b": f(inp["ln1_b"][0]).reshape(1, -1),
        "w_router": f(inp["w_router"][0]), "w1": f(inp["w1"][0]), "w3": f(inp["w3"][0]), "w2": f(inp["w2"][0]),
        "ln2_g": f(inp["ln2_g"][0]).reshape(1, -1), "ln2_b": f(inp["ln2_b"][0]).reshape(1, -1),
    }
    shared.update(consts)
    maps = []
    for core in range(8):
        b = core % 4
        m = dict(shared)
        m["x"] = f(inp["x"][b])
        m["ctx"] = f(inp["ctx"][b])
        m["cvec"] = np.ascontiguousarray(np.stack([np.asarray(inp["c"][b], np.float32), np.asarray(inp["c_ctx"], np.float32)]))
        maps.append(m)
    return maps


def kernel(**inputs):
    nc = build_program()
    in_maps = make_in_maps(inputs)
    in_maps = [{kk: v for kk, v in m.items() if kk in nc.used_inputs} for m in in_maps]
    res = run_bass_kernel_spmd(nc, in_maps, core_ids=list(range(8)))
    outs = [np.asarray(res.results[b]["out"]) for b in range(4)]
    return np.stack(outs, axis=0).astype(np.float32)
```
